# Optimizing a Trainium2 kernel written in Bass

```python
import math
import jax
import jax.numpy as jnp
from jax import lax
import numpy as np

D_MODEL = 1024
BATCH = 2
SEQ = 8192
DEPTH = 1

N_HEADS = 8
HEAD_DIM = 64
ATTN_WIDTH = N_HEADS * HEAD_DIM
ROPE_THETA = 10000.0
MOBA_BLOCK = 256
MOBA_TOPK = 3
Q_CHUNK = 64
SSM_GROUP_SIZE = 16
SSM_GROUPS = 32
SSM_WIDTH = SSM_GROUPS * SSM_GROUP_SIZE
SSM_STATE = 64
DT_MIN = 0.001
DT_MAX = 0.1
IN_WIDTH = SSM_WIDTH + 3 * ATTN_WIDTH + 2 * D_MODEL
N_EXPERTS = 32
TOP_K = 4
D_FF = D_MODEL
SWIGLU_ALPHA = 1.702
SWIGLU_LIMIT = 7.0
MOE_BLOCK = 128
NORM_EPS = 1e-6
NEG_INF = -1e30

kernel_name = "hybrid_s5_moba_moe_block"


def rms_norm(t, g):
    tf = t.astype(jnp.float32)
    tf = tf * lax.rsqrt(jnp.mean(tf * tf, axis=-1, keepdims=True) + NORM_EPS)
    return tf * g.astype(jnp.float32)


def apply_rope(t, positions):
    half = HEAD_DIM // 2
    inv_freq = ROPE_THETA ** (-jnp.arange(half, dtype=jnp.float32) / half)
    ang = positions.astype(jnp.float32)[..., None] * inv_freq
    cos = jnp.cos(ang)[:, :, None, :]
    sin = jnp.sin(ang)[:, :, None, :]
    tf = t.astype(jnp.float32)
    t1, t2 = tf[..., :half], tf[..., half:]
    return jnp.concatenate([t1 * cos - t2 * sin, t2 * cos + t1 * sin], axis=-1)


def _linear_recurrence(left, right):
    a_l, b_l = left
    a_r, b_r = right
    return a_r * a_l, a_r * b_l + b_r


def s5_mixer(u, lam_re, lam_im, log_dt, b_re, b_im, c_re, c_im, d_skip, w_glu):
    bsz, seq, _ = u.shape
    uf = u.astype(jnp.float32)
    ug = uf.reshape(bsz, seq, SSM_GROUPS, SSM_GROUP_SIZE)
    lam = lax.complex(lam_re.astype(jnp.float32), lam_im.astype(jnp.float32))
    dt = jnp.exp(log_dt.astype(jnp.float32))[:, None]
    lam_bar = jnp.exp(lam * dt)
    b_mat = lax.complex(b_re.astype(jnp.float32), b_im.astype(jnp.float32))
    c_mat = lax.complex(c_re.astype(jnp.float32), c_im.astype(jnp.float32))
    b_bar = ((lam_bar - 1.0) / lam)[..., None] * b_mat
    bu = jnp.einsum('gpc,bsgc->bsgp', b_bar, ug.astype(jnp.complex64))
    a = jnp.broadcast_to(lam_bar, bu.shape)
    _, states = lax.associative_scan(_linear_recurrence, (a, bu), axis=1)
    y = jnp.einsum('gcp,bsgp->bsgc', c_mat, states).real.reshape(bsz, seq, SSM_WIDTH)
    y = y + d_skip.astype(jnp.float32) * uf
    y = jax.nn.gelu(y)
    y = y * jax.nn.sigmoid(y @ w_glu.astype(jnp.float32))
    return y


def moba_attention(q, k, v):
    bsz, nh, seq, hd = q.shape
    qf, kf, vf = q.astype(jnp.float32), k.astype(jnp.float32), v.astype(jnp.float32)
    nb = -(-seq // MOBA_BLOCK)
    pad = nb * MOBA_BLOCK - seq
    kb = jnp.pad(kf, ((0, 0), (0, 0), (0, pad), (0, 0))).reshape(bsz, nh, nb, MOBA_BLOCK, hd)
    vb = jnp.pad(vf, ((0, 0), (0, 0), (0, pad), (0, 0))).reshape(bsz, nh, nb, MOBA_BLOCK, hd)
    k_mean = jnp.mean(kb, axis=3)
    n_sel = min(MOBA_TOPK, nb)
    scale = HEAD_DIM ** -0.5
    n_chunks = seq // Q_CHUNK
    q_chunks = qf.reshape(bsz, nh, n_chunks, Q_CHUNK, hd).transpose(2, 0, 1, 3, 4)
    gather_blocks = jax.vmap(jax.vmap(lambda blocks, idx: blocks[idx]))
    block_ids = jnp.arange(nb)
    offs = jnp.arange(MOBA_BLOCK)

    def chunk_attend(args):
        ci, qi = args
        q_pos = ci * Q_CHUNK + jnp.arange(Q_CHUNK)
        q_blk = q_pos // MOBA_BLOCK
        own = (ci * Q_CHUNK) // MOBA_BLOCK
        gate = jnp.einsum('bhqd,bhnd->bhqn', qi, k_mean)
        gate = jnp.where(block_ids[None, :] < q_blk[:, None], gate, NEG_INF)
        _, sel = lax.top_k(gate, n_sel)
        sel_ok = sel < q_blk[:, None]
        k_sel = gather_blocks(kb, sel)
        v_sel = gather_blocks(vb, sel)
        s_sel = jnp.einsum('bhqd,bhqnld->bhqnl', qi, k_sel) * scale
        s_sel = jnp.where(sel_ok[..., None], s_sel, NEG_INF).reshape(bsz, nh, Q_CHUNK, n_sel * MOBA_BLOCK)
        k_own = lax.dynamic_index_in_dim(kb, own, axis=2, keepdims=False)
        v_own = lax.dynamic_index_in_dim(vb, own, axis=2, keepdims=False)
        s_own = jnp.einsum('bhqd,bhld->bhql', qi, k_own) * scale
        k_pos = own * MOBA_BLOCK + offs
        s_own = jnp.where(k_pos[None, :] <= q_pos[:, None], s_own, NEG_INF)
        p = jax.nn.softmax(jnp.concatenate([s_sel, s_own], axis=-1), axis=-1)
        p_sel = p[..., :n_sel * MOBA_BLOCK].reshape(bsz, nh, Q_CHUNK, n_sel, MOBA_BLOCK)
        p_own = p[..., n_sel * MOBA_BLOCK:]
        return (jnp.einsum('bhqnl,bhqnld->bhqd', p_sel, v_sel)
                + jnp.einsum('bhql,bhld->bhqd', p_own, v_own))

    out = lax.map(chunk_attend, (jnp.arange(n_chunks), q_chunks))
    return out.transpose(1, 2, 0, 3, 4).reshape(bsz, nh, seq, hd)


def moe_ffn(h, router_w, router_b, w_gate, b_gate, w_up, b_up, w_down, b_down):
    bsz, seq, d = h.shape
    n_tok = bsz * seq
    hf = h.reshape(n_tok, d).astype(jnp.float32)
    logits = hf @ router_w.astype(jnp.float32) + router_b.astype(jnp.float32)
    top_val, top_idx = lax.top_k(logits, TOP_K)
    top_w = jax.nn.softmax(top_val, axis=-1)
    n_assign = n_tok * TOP_K
    eid = top_idx.reshape(n_assign)
    order = jnp.argsort(eid)
    s_eid = eid[order]
    s_tok = order // TOP_K
    s_w = top_w.reshape(n_assign)[order]
    counts = jnp.bincount(eid, length=N_EXPERTS)
    starts = jnp.cumsum(counts) - counts
    padded = (counts + MOE_BLOCK - 1) // MOE_BLOCK * MOE_BLOCK
    p_ends = jnp.cumsum(padded)
    p_starts = p_ends - padded
    dest = p_starts[s_eid] + jnp.arange(n_assign) - starts[s_eid]
    n_blocks = -(-n_assign // MOE_BLOCK) + N_EXPERTS
    n_rows = n_blocks * MOE_BLOCK
    row_tok = jnp.full((n_rows,), n_tok, dtype=jnp.int32).at[dest].set(s_tok.astype(jnp.int32))
    row_w = jnp.zeros((n_rows,), jnp.float32).at[dest].set(s_w)
    blk_exp = jnp.minimum(jnp.searchsorted(p_ends, jnp.arange(n_blocks) * MOE_BLOCK, side='right'),
                          N_EXPERTS - 1)
    h_pad = jnp.concatenate([hf, jnp.zeros((1, d), jnp.float32)], axis=0)
    xs = h_pad[row_tok].reshape(n_blocks, MOE_BLOCK, d)

    def expert_block(args):
        xb, e = args
        g = xb @ w_gate[e].astype(jnp.float32) + b_gate[e].astype(jnp.float32)
        u = xb @ w_up[e].astype(jnp.float32) + b_up[e].astype(jnp.float32)
        g = jnp.minimum(g, SWIGLU_LIMIT)
        u = jnp.clip(u, -SWIGLU_LIMIT, SWIGLU_LIMIT)
        act = g * jax.nn.sigmoid(SWIGLU_ALPHA * g) * (u + 1.0)
        return act @ w_down[e].astype(jnp.float32) + b_down[e].astype(jnp.float32)

    yb = lax.map(expert_block, (xs, blk_exp)).reshape(n_rows, d)
    y = jnp.zeros((n_tok + 1, d), jnp.float32).at[row_tok].add(yb * row_w[:, None])
    return y[:n_tok].reshape(bsz, seq, d)


def setup_inputs(seed: int = 0) -> dict:
    key = jax.random.key(seed)
    ks = jax.random.split(key, 32)
    f32 = jnp.float32
    nrm = lambda k, shape, s: jax.random.normal(k, shape, f32) * s
    L, D, G, P, HG, E, F = DEPTH, D_MODEL, SSM_GROUPS, SSM_STATE, SSM_GROUP_SIZE, N_EXPERTS, D_FF
    x = jax.random.normal(ks[0], (BATCH, SEQ, D), f32)
    c = jax.random.normal(ks[1], (BATCH, D), f32)
    offset = jax.random.randint(ks[2], (BATCH, 1), 0, 1024, dtype=jnp.int32)
    positions = (jnp.arange(SEQ, dtype=jnp.int32)[None, :] + offset).astype(jnp.int32)
    lam_im0 = jnp.pi * jnp.arange(P, dtype=f32)
    return {
        'x': x,
        'c': c,
        'positions': positions,
        'ada_w': nrm(ks[3], (L, D, 6 * D), 0.5 * D ** -0.5),
        'ada_b': nrm(ks[4], (L, 6 * D), 0.01),
        'mix_pre_g': 1.0 + nrm(ks[5], (L, D), 0.05),
        'mix_post_g': 1.0 + nrm(ks[6], (L, D), 0.05),
        'ffn_pre_g': 1.0 + nrm(ks[7], (L, D), 0.05),
        'ffn_post_g': 1.0 + nrm(ks[8], (L, D), 0.05),
        'w_in': nrm(ks[9], (L, D, IN_WIDTH), D ** -0.5),
        'ssm_lam_re': -0.5 + nrm(ks[10], (L, G, P), 0.01),
        'ssm_lam_im': lam_im0 + nrm(ks[11], (L, G, P), 0.01),
        'ssm_log_dt': jax.random.uniform(ks[12], (L, G), f32, math.log(DT_MIN), math.log(DT_MAX)),
        'ssm_b_re': nrm(ks[13], (L, G, P, HG), (2 * HG) ** -0.5),
        'ssm_b_im': nrm(ks[14], (L, G, P, HG), (2 * HG) ** -0.5),
        'ssm_c_re': nrm(ks[15], (L, G, HG, P), (2 * P) ** -0.5),
        'ssm_c_im': nrm(ks[16], (L, G, HG, P), (2 * P) ** -0.5),
        'ssm_d': nrm(ks[17], (L, SSM_WIDTH), 1.0),
        'ssm_w_glu': nrm(ks[18], (L, SSM_WIDTH, SSM_WIDTH), SSM_WIDTH ** -0.5),
        'w_ssm_branch': nrm(ks[19], (L, SSM_WIDTH, D), SSM_WIDTH ** -0.5),
        'w_attn_branch': nrm(ks[20], (L, ATTN_WIDTH, D), ATTN_WIDTH ** -0.5),
        'w_out': nrm(ks[21], (L, D, D), D ** -0.5),
        'router_w': nrm(ks[22], (L, D, E), D ** -0.5),
        'router_b': nrm(ks[23], (L, E), 0.01),
        'w_gate': nrm(ks[24], (L, E, D, F), D ** -0.5),
        'b_gate': nrm(ks[25], (L, E, F), 0.01),
        'w_up': nrm(ks[26], (L, E, D, F), D ** -0.5),
        'b_up': nrm(ks[27], (L, E, F), 0.01),
        'w_down': nrm(ks[28], (L, E, F, D), F ** -0.5),
        'b_down': nrm(ks[29], (L, E, D), 0.01),
    }


def reference(x, c, positions, ada_w, ada_b, mix_pre_g, mix_post_g, ffn_pre_g, ffn_post_g,
              w_in, ssm_lam_re, ssm_lam_im, ssm_log_dt, ssm_b_re, ssm_b_im, ssm_c_re, ssm_c_im,
              ssm_d, ssm_w_glu, w_ssm_branch, w_attn_branch, w_out,
              router_w, router_b, w_gate, b_gate, w_up, b_up, w_down, b_down):
    bsz, seq, _ = x.shape
    dtype = x.dtype
    cond = jax.nn.silu(c.astype(jnp.float32))
    splits = [SSM_WIDTH, SSM_WIDTH + ATTN_WIDTH, SSM_WIDTH + 2 * ATTN_WIDTH,
              SSM_WIDTH + 3 * ATTN_WIDTH, SSM_WIDTH + 3 * ATTN_WIDTH + D_MODEL]
    for l in range(DEPTH):
        ada = cond @ ada_w[l].astype(jnp.float32) + ada_b[l].astype(jnp.float32)
        sh_m, sc_m, gt_m, sh_f, sc_f, gt_f = [a[:, None, :] for a in jnp.split(ada, 6, axis=-1)]

        h = (rms_norm(x, mix_pre_g[l]) * (1.0 + sc_m) + sh_m).astype(dtype)
        proj = h @ w_in[l]
        u_s, q, k, v, g_s, g_a = jnp.split(proj, splits, axis=-1)
        y_s = s5_mixer(u_s, ssm_lam_re[l], ssm_lam_im[l], ssm_log_dt[l], ssm_b_re[l], ssm_b_im[l],
                       ssm_c_re[l], ssm_c_im[l], ssm_d[l], ssm_w_glu[l])
        q = apply_rope(q.reshape(bsz, seq, N_HEADS, HEAD_DIM), positions).transpose(0, 2, 1, 3)
        k = apply_rope(k.reshape(bsz, seq, N_HEADS, HEAD_DIM), positions).transpose(0, 2, 1, 3)
        v = v.reshape(bsz, seq, N_HEADS, HEAD_DIM).transpose(0, 2, 1, 3)
        y_a = moba_attention(q, k, v).transpose(0, 2, 1, 3).reshape(bsz, seq, ATTN_WIDTH)
        merged = (jax.nn.sigmoid(g_s.astype(jnp.float32)) * (y_s @ w_ssm_branch[l].astype(jnp.float32))
                  + jax.nn.sigmoid(g_a.astype(jnp.float32)) * (y_a @ w_attn_branch[l].astype(jnp.float32)))
        mix = merged @ w_out[l].astype(jnp.float32)
        x = (x.astype(jnp.float32) + gt_m * rms_norm(mix, mix_post_g[l])).astype(dtype)

        h2 = (rms_norm(x, ffn_pre_g[l]) * (1.0 + sc_f) + sh_f).astype(dtype)
        ff = moe_ffn(h2, router_w[l], router_b[l], w_gate[l], b_gate[l], w_up[l], b_up[l],
                     w_down[l], b_down[l])
        x = (x.astype(jnp.float32) + gt_f * rms_norm(ff, ffn_post_g[l])).astype(dtype)
    return x
```

```python
import numpy as np
from contextlib import ExitStack
import ml_dtypes
import concourse.bass as bass
import concourse.mybir as mybir
from concourse.bass_utils import run_bass_kernel_spmd

F32 = mybir.dt.float32
BF16 = mybir.dt.bfloat16
I32 = mybir.dt.int32
ALU = mybir.AluOpType
AF = mybir.ActivationFunctionType
AX = mybir.AxisListType
PI = float(np.pi)
TWO_PI = float(2 * np.pi)
PI_LO = 3.1415925
TWO_PI_LO = 6.283185
SEM_CAP = 30000
NEG = -30000.0


class Tracker:
    def __init__(self, nc):
        self.nc = nc
        self.engs = {"pe": nc.tensor, "act": nc.scalar, "dve": nc.vector,
                     "pool": nc.gpsimd, "sp": nc.sync}
        self.cur_sem = {}
        self.cnt = {}
        self.nsem = 0
        for e in ("pe", "act", "dve", "pool"):
            self._new_epoch(e)
        self.seen = {e: {} for e in self.engs}
        self.lastw = {}
        self.reads = {}
        self.NS = 8
        self.dq = {}
        for q in ("sp", "pool"):
            sems = [self._alloc(f"dq_{q}_{i}") for i in range(self.NS)]
            self.dq[q] = {"sems": sems, "i": 0, "last": {}}

    def _alloc(self, name):
        self.nsem += 1
        return self.nc.alloc_semaphore(name)

    def _new_epoch(self, e):
        self.cur_sem[e] = self._alloc(f"c_{e}_{self.nsem}")
        self.cnt[e] = 0

    def _wait(self, e, ev):
        if ev is None:
            return
        sem, val, src = ev
        if src == "pe" and e == "pe":
            return
        k = id(sem)
        old = self.seen[e].get(k)
        if old is not None and old >= val:
            return
        self.seen[e][k] = val
        self.engs[e].wait_ge(sem, val)

    def _deps(self, e, reads, writes):
        for r in reads:
            self._wait(e, self.lastw.get(r))
        for w in writes:
            self._wait(e, self.lastw.get(w))
            for ev in self.reads.get(w, ()):
                self._wait(e, ev)

    def _commit(self, ev, reads, writes):
        for r in reads:
            lst = self.reads.setdefault(r, [])
            lst.append(ev)
            if len(lst) > 24:
                d = {}
                for s, v, src in lst:
                    if id(s) not in d or d[id(s)][1] < v:
                        d[id(s)] = (s, v, src)
                self.reads[r] = list(d.values())
        for w in writes:
            self.lastw[w] = ev
            self.reads[w] = []

    def op(self, e, fn, reads=(), writes=()):
        self._deps(e, reads, writes)
        if self.cnt[e] >= SEM_CAP:
            self._new_epoch(e)
        ins = fn(self.engs[e])
        self.cnt[e] += 1
        ins.then_inc(self.cur_sem[e], 1)
        ev = (self.cur_sem[e], self.cnt[e], e)
        self._commit(ev, reads, writes)
        return ev

    def dma(self, q, out, in_, reads=(), writes=(), **kw):
        d = self.dq[q]
        i = d["i"]
        d["i"] += 1
        slot = i % self.NS
        sem = d["sems"][slot]
        prev = 16 * (i // self.NS)
        if prev + 16 > 2 * SEM_CAP:
            d["sems"] = [self._alloc(f"dq_{q}_{i}_{k}") for k in range(self.NS)]
            d["i"] = 1
            i = 0
            slot = 0
            sem = d["sems"][0]
            prev = 0
        if prev > 0:
            self._wait(q, (sem, prev, "dma"))
        self._deps(q, reads, writes)
        ins = self.engs[q].dma_start(out=out, in_=in_, **kw)
        ins.then_inc(sem, 16)
        ev = (sem, prev + 16, "dma")
        self._commit(ev, reads, writes)
        d["last"][id(sem)] = ev
        return ev

    def dma_fn(self, q, fn, reads=(), writes=()):
        d = self.dq[q]
        i = d["i"]
        d["i"] += 1
        slot = i % self.NS
        sem = d["sems"][slot]
        prev = 16 * (i // self.NS)
        if prev + 16 > 2 * SEM_CAP:
            d["sems"] = [self._alloc(f"dq_{q}_{i}_{k}") for k in range(self.NS)]
            d["i"] = 1
            slot = 0
            sem = d["sems"][0]
            prev = 0
        if prev > 0:
            self._wait(q, (sem, prev, "dma"))
        self._deps(q, reads, writes)
        ins = fn(self.engs[q])
        ins.then_inc(sem, 16)
        ev = (sem, prev + 16, "dma")
        self._commit(ev, reads, writes)
        d["last"][id(sem)] = ev
        return ev

    def _all_events(self):
        evs = []
        for e in ("pe", "act", "dve", "pool"):
            if self.cnt[e] > 0:
                evs.append((self.cur_sem[e], self.cnt[e], "bar"))
        for q, d in self.dq.items():
            for ev in d["last"].values():
                evs.append((ev[0], ev[1], "bar"))
        return evs

    def barrier(self):
        evs = self._all_events()
        for e in self.engs:
            for ev in evs:
                self._wait(e, ev)
        self.lastw.clear()
        self.reads.clear()

    def finish(self, eng="sp"):
        for ev in self._all_events():
            self._wait(eng, ev)


def build_program():
    nc = bass.Bass("TRN2", target_bir_lowering=False)

    def din(name, shape, dt=F32):
        return nc.dram_tensor(name, list(shape), dt, kind="ExternalInput").ap()

    def dscr(name, shape, dt):
        return nc.dram_tensor(name, list(shape), dt, kind="Internal").ap()

    x_loc = din("x_loc", [8192, 1024])
    tmask_d = din("tmask", [128, 64])
    pos_d = din("pos_loc", [1, 8192], I32)
    c_b = din("c_b", [128, 8])
    ada_w = din("ada_w", [1024, 6144])
    ada_b = din("ada_b", [1, 6144])
    gains = din("gains", [1, 4096])
    w_in = din("w_in", [1024, 4096])
    cst_d = din("cst", [128, 16])
    ident_d = din("ident", [128, 128])
    iota_d = din("iota", [1, 1024])
    lre_d = din("lre", [128, 32])
    lim_d = din("lim", [128, 32])
    ldt_d = din("ldt", [1, 32])
    bl1_d = din("bl1", [128, 32 * 128])
    bl2_d = din("bl2", [128, 32 * 128])
    cr_d = din("cr", [128, 512])
    ci_d = din("ci", [128, 512])
    dsk_d = din("dsk", [128, 4])
    wglu_d = din("w_glu", [512, 512])
    wsb_d = din("w_sb", [512, 1024])
    wab_d = din("w_ab", [512, 1024])
    wout_d = din("w_out", [1024, 1024])
    bonehot_d = din("bonehot", [32, 8192], BF16)
    dm_d = din("dmask", [128, 2048], BF16)
    vb_d = din("vbias", [1, 8 * 96])
    vv_d = din("vvalid", [1, 8 * 96])
    vo_d = din("vown", [1, 8 * 96])
    rw_d = din("router_w", [1024, 32])
    rb_d = din("router_b", [1, 32])
    wg_d = din("w_gate", [32, 1024, 1024])
    wu_d = din("w_up", [32, 1024, 1024])
    wd_d = din("w_down", [32, 1024, 1024])
    bg_d = din("b_gate", [128, 32 * 8])
    bu_d = din("b_up", [128, 32 * 8])
    bd_d = din("b_down", [32, 1024])
    ltri_d = din("ltri", [128, 128], BF16)
    eoff_d = din("eoff", [1, 32])
    out_d = nc.dram_tensor("out", [2048, 1024], F32, kind="ExternalOutput").ap()
    CAP = 1024
    XD = dscr("XD", [32 * CAP, 1024], BF16)
    YD = dscr("YD", [32 * CAP, 1024], BF16)

    UT = dscr("UT", [512, 8192], BF16)
    KT = dscr("KT", [512, 8192], BF16)
    VV = dscr("VV", [8192, 512], BF16)
    QT = dscr("QT", [512, 2048], BF16)
    GST = dscr("GST", [1024, 2048], BF16)
    GAT = dscr("GAT", [1024, 2048], BF16)
    YST = dscr("YST", [512, 2048], BF16)
    OAT = dscr("OAT", [512, 2048], BF16)
    X1 = dscr("X1", [2048, 1024], F32)
    H2T = dscr("H2T", [1024, 2048], BF16)

    T = Tracker(nc)
    BCREG = nc.gpsimd.to_reg(32 * 1024 - 1)
    _cnt = [0]

    def _u(n):
        _cnt[0] += 1
        return f'sb{_cnt[0]}_{n}'
    op = T.op
    dma = T.dma
    uid = [0]

    def mm(out, lhsT, rhs, start, stop, reads, writes):
        return op("pe", lambda e: e.matmul(out, lhsT=lhsT, rhs=rhs, start=start, stop=stop),
                  reads=reads, writes=writes)

    def act(out, in_, func, reads, writes, eng="act", **kw):
        return op(eng, lambda e: e.activation(out=out, in_=in_, func=func, **kw), reads=reads, writes=writes)

    def tt(eng, out, a, b, o, reads, writes):
        return op(eng, lambda e: e.tensor_tensor(out=out, in0=a, in1=b, op=o), reads=reads, writes=writes)

    def ts(eng, out, a, s1, s2, o0, o1, reads, writes):
        if o1 is None:
            return op(eng, lambda e: e.tensor_scalar(out=out, in0=a, scalar1=s1, scalar2=None, op0=o0),
                      reads=reads, writes=writes)
        return op(eng, lambda e: e.tensor_scalar(out=out, in0=a, scalar1=s1, scalar2=s2, op0=o0, op1=o1),
                  reads=reads, writes=writes)

    def stt(eng, out, a, s, b, o0, o1, reads, writes):
        return op(eng, lambda e: e.scalar_tensor_tensor(out=out, in0=a, scalar=s, in1=b, op0=o0, op1=o1),
                  reads=reads, writes=writes)

    def cp(eng, out, a, reads, writes):
        return op(eng, lambda e: e.tensor_copy(out=out, in_=a), reads=reads, writes=writes)


    MAGIC = 12582912.0
    INV2PI = float(1.0 / (2 * np.pi))

    def reduce_angle(x, kx, k, kk, r, kr, ab, kab):
        ts("dve", k, x, INV2PI, MAGIC, ALU.mult, ALU.add, [kx], [kk])
        ts("dve", k, k, -MAGIC, None, ALU.add, None, [kk], [kk])
        stt("dve", r, k, -TWO_PI, x, ALU.mult, ALU.add, [kk, kx], [kr])
        ts("dve", r, r, PI_LO, -PI_LO, ALU.min, ALU.max, [kr], [kr])
        act(ab, r, AF.Abs, [kr], [kab])

    with ExitStack() as gs:
        def gsb(name, shape, dt=F32):
            return gs.enter_context(nc.sbuf_tensor(_u(name), shape, dt))
        G2 = gsb("G2", [128, 1024])
        ident = gsb("ident", [128, 128])
        identb = gsb("identb", [128, 128], BF16)
        CST = gsb("CST", [128, 16])
        tmask = gsb("tmaskt", [128, 64])
        RWT = gsb("RWT", [128, 16, 32])
        IDX = gsb("IDX", [128, 512], I32)
        aes = ExitStack()
        PRM = aes.enter_context(nc.sbuf_tensor(_u("PRM"), [128, 5, 1024], F32))
        dma("sp", ident[:], ident_d, writes=["ident"])
        dma("sp", CST[:], cst_d, writes=["CST"])
        dma("sp", tmask[:], tmask_d, writes=["tmask"])
        cp("dve", identb[:], ident[:], ["ident"], ["identb"])
        SGNR = CST[:, 1:2]
        HALFPI = CST[:, 2:3]
        INVF = CST[:, 3:4]
        MLO = CST[:, 4:5]
        NMHI = CST[:, 5:6]
        SHALF = CST[:, 6:7]
        NEG1 = CST[:, 7:8]
        NMLO = CST[:, 8:9]
        ONE9 = CST[:, 9:10]
        N2PI = CST[:, 10:11]
        SC1 = CST[:, 11:12]
        SHALF1 = CST[:, 12:13]
        MAGICC = CST[:, 13:14]

        with ExitStack() as es:
            def sb(name, shape, dt=F32):
                return es.enter_context(nc.sbuf_tensor(_u(name), shape, dt))
            ct = sb("ct", [128, 8]); cs = sb("cs", [128, 8])
            CB = sb("CB", [128, 8, 128], BF16)
            AWb = sb("AWb", [128, 8, 512], BF16)
            ADAB = sb("ADAB", [128, 6144]); ADA = sb("ADA", [128, 6144])
            GB = sb("GB", [128, 4096])
            pa = es.enter_context(nc.psum_tensor(_u("pa"), [128, 512], F32))
            dma("sp", ct[:], c_b, writes=["ct"])
            dma("pool", ADAB[:], ada_b.to_broadcast([128, 6144]), writes=["ADAB"])
            dma("pool", GB[:], gains.to_broadcast([128, 4096]), writes=["GB"])
            act(cs[:], ct[:], AF.Silu, ["ct"], ["cs"])
            for kc in range(8):
                cp("dve", CB[:, kc, :], cs[:, kc:kc + 1].to_broadcast([128, 128]), ["cs"], ["CB"])
            for n in range(12):
                dma("pool", AWb[:], ada_w[:, n * 512:(n + 1) * 512].rearrange("(k p) n -> p k n", p=128),
                    writes=["AWb"])
                for kc in range(8):
                    mm(pa[:], CB[:, kc, :], AWb[:, kc, :], kc == 0, kc == 7, ["CB", "AWb"], ["pa"])
                tt("dve", ADA[:, n * 512:(n + 1) * 512], pa[:], ADAB[:, n * 512:(n + 1) * 512], ALU.add,
                   ["pa", "ADAB"], ["ADA"])
            stt("dve", PRM[:, 0, :], ADA[:, 1024:2048], 1.0, GB[:, 0:1024], ALU.add, ALU.mult, ["ADA", "GB"], ["PRM"])
            cp("dve", PRM[:, 1, :], ADA[:, 0:1024], ["ADA"], ["PRM"])
            tt("dve", PRM[:, 2, :], ADA[:, 2048:3072], GB[:, 1024:2048], ALU.mult, ["ADA", "GB"], ["PRM"])
            stt("dve", PRM[:, 3, :], ADA[:, 4096:5120], 1.0, GB[:, 2048:3072], ALU.add, ALU.mult, ["ADA", "GB"], ["PRM"])
            cp("dve", PRM[:, 4, :], ADA[:, 3072:4096], ["ADA"], ["PRM"])
            tt("dve", G2[:], ADA[:, 5120:6144], GB[:, 3072:4096], ALU.mult, ["ADA", "GB"], ["G2"])
            T.barrier()

        def norm_mod(es_sb, xt, key_x, a_idx, sh_idx, maskcol, hb, key_hb, tmp, tmp2, ss, rs, sq):
            act(sq[:], xt, AF.Square, [key_x], ["sq", "ss"], accum_out=ss[:])
            ts("dve", rs[:], ss[:], 1.0 / 1024, 1e-6, ALU.mult, ALU.add, ["ss"], ["rs"])
            act(rs[:], rs[:], AF.Sqrt, ["rs"], ["rs"])
            op("dve", lambda e: e.reciprocal(out=rs[:], in_=rs[:]), reads=["rs"], writes=["rs"])
            stt("dve", tmp[:], xt, rs[:, 0:1], PRM[:, a_idx, :], ALU.mult, ALU.mult, [key_x, "rs", "PRM"], ["tmp"])
            tt("dve", tmp2[:], tmp[:], PRM[:, sh_idx, :], ALU.add, ["tmp", "PRM"], ["tmp2"])
            if maskcol is None:
                act(hb, tmp2[:], AF.Copy, ["tmp2"], [key_hb])
            else:
                act(hb, tmp2[:], AF.Copy, ["tmp2", "tmask"], [key_hb], scale=maskcol)

        with ExitStack() as es:
            def sb(name, shape, dt=F32):
                return es.enter_context(nc.sbuf_tensor(_u(name), shape, dt))
            def ps(name, shape, dt=F32):
                return es.enter_context(nc.psum_tensor(_u(name), shape, dt))
            WB = sb("WB", [128, 8, 5120], BF16)
            xt = sb("xt", [128, 1024]); tmp = sb("tmp", [128, 1024]); tmp2 = sb("tmp2", [128, 1024])
            sq = sb("sq", [128, 1024], BF16)
            ss = sb("ss", [128, 1]); rs = sb("rs", [128, 1])
            hb = sb("hb", [128, 1024], BF16)
            hT = sb("hT", [128, 8, 512], BF16)
            posi = sb("posi", [128, 512], I32); posf = sb("posf", [128, 512])
            a1 = sb("a1", [128, 512]); a2 = sb("a2", [128, 512])
            cosT = sb("cosT", [128, 512]); sinS = sb("sinS", [128, 512])
            m1 = sb("m1", [128, 512]); m2 = sb("m2", [128, 512])
            ob = [sb(f"ob{i}", [128, 512], BF16) for i in range(3)]
            pT = ps("pT", [128, 8, 128], BF16)
            pk = ps("pk", [128, 512]); pks = ps("pks", [128, 512])
            pm = [ps(f"pm{i}", [128, 512]) for i in range(2)]

            blocks = [(0, 0, False), (512, 1024, False), (1024, 1024, True), (1536, 1536, False),
                      (2048, 512, False), (2560, 512, True), (3072, 2048, False), (3584, 2560, False),
                      (4096, 3072, False), (4608, 3584, False)]
            for dst, src, swp in blocks:
                if not swp:
                    dma("pool", WB[:, :, dst:dst + 512], w_in[:, src:src + 512].rearrange("(k p) n -> p k n", p=128),
                        writes=["WB"])
                else:
                    srcv = w_in[:, src:src + 512].rearrange("(k p) (h t j) -> p k h t j", p=128, h=8, t=2, j=32)
                    dstv = WB[:, :, dst:dst + 512].rearrange("p k (h t j) -> p k h t j", h=8, t=2, j=32)
                    for kc in range(8):
                        dma("pool", dstv[:, kc, :, 0, :], srcv[:, kc, :, 1, :], writes=["WB"])
                        dma("pool", dstv[:, kc, :, 1, :], srcv[:, kc, :, 0, :], writes=["WB"])

            T.barrier()
            obi = [0]

            def emit(src_ps, key_ps, dst_ap, func=AF.Copy, **kw):
                o = ob[obi[0] % 3]
                k = f"ob{obi[0] % 3}"
                obi[0] += 1
                act(o[:], src_ps, func, [key_ps], [k], **kw)
                dma("sp", dst_ap, o[:], reads=[k])

            for c in range(16):
                own = c >= 12
                for j in range(4):
                    t = 4 * c + j
                    dma("sp", xt[:], x_loc[t * 128:(t + 1) * 128, :], writes=["xt"])
                    norm_mod(sb, xt[:], "xt", 0, 1, tmask[:, t:t + 1], hb[:], "hb", tmp, tmp2, ss, rs, sq)
                    for kc in range(8):
                        op("pe", lambda e: e.transpose(out=pT[:, kc, :], in_=hb[:, kc * 128:(kc + 1) * 128],
                                                       identity=identb[:]),
                           reads=["hb", "identb"], writes=["pT"])
                    cp("dve", hT[:, :, j * 128:(j + 1) * 128], pT[:], ["pT"], ["hT"])
                dma("pool", posi[:], pos_d[0:1, c * 512:(c + 1) * 512].to_broadcast([128, 512]), writes=["posi"])
                cp("dve", posf[:], posi[:], ["posi"], ["posf"])
                ts("dve", a1[:], posf[:], INVF, None, ALU.mult, None, ["posf", "CST"], ["a1"])
                reduce_angle(a1[:], "a1", a2[:], "a2", m1[:], "m1", m2[:], "m2")
                act(sinS[:], m1[:], AF.Sin, ["m1", "CST"], ["sinS"], scale=SGNR)
                act(cosT[:], m2[:], AF.Sin, ["m2", "CST"], ["cosT"], scale=NEG1, bias=HALFPI)

                def rope_proj(c0, c0s, dst, scale):
                    for cb in range(4):
                        for kc in range(8):
                            mm(pk[:], WB[:, kc, c0 + cb * 128:c0 + (cb + 1) * 128], hT[:, kc, :], kc == 0, kc == 7,
                               ["WB", "hT"], ["pk"])
                        for kc in range(8):
                            mm(pks[:], WB[:, kc, c0s + cb * 128:c0s + (cb + 1) * 128], hT[:, kc, :], kc == 0, kc == 7,
                               ["WB", "hT"], ["pks"])
                        tt("dve", m1[:], pk[:], cosT[:], ALU.mult, ["pk", "cosT"], ["m1"])
                        tt("dve", m2[:], pks[:], sinS[:], ALU.mult, ["pks", "sinS"], ["m2"])
                        tt("dve", m1[:], m1[:], m2[:], ALU.add, ["m1", "m2"], ["m1"])
                        emit(m1[:], "m1", dst(cb), scale=scale)

                rope_proj(512, 1024, lambda cb: KT[cb * 128:(cb + 1) * 128, c * 512:(c + 1) * 512], 1.0)
                for cb in range(4):
                    p = pm[cb % 2]; kp = f"pm{cb % 2}"
                    for kc in range(8):
                        mm(p[:], WB[:, kc, cb * 128:(cb + 1) * 128], hT[:, kc, :], kc == 0, kc == 7, ["WB", "hT"], [kp])
                    emit(p[:], kp, UT[cb * 128:(cb + 1) * 128, c * 512:(c + 1) * 512])
                for j in range(4):
                    p = pm[j % 2]; kp = f"pm{j % 2}"
                    for kc in range(8):
                        mm(p[:], hT[:, kc, j * 128:(j + 1) * 128], WB[:, kc, 1536:2048], kc == 0, kc == 7, ["WB", "hT"], [kp])
                    emit(p[:], kp, VV[(4 * c + j) * 128:(4 * c + j + 1) * 128, :])
                if own:
                    co = c - 12
                    rope_proj(2048, 2560, lambda cb: QT[cb * 128:(cb + 1) * 128, co * 512:(co + 1) * 512], 0.125)
                    for gi, (c0, dstT) in enumerate(((3072, GST), (4096, GAT))):
                        for db in range(8):
                            p = pm[db % 2]; kp = f"pm{db % 2}"
                            for kc in range(8):
                                mm(p[:], WB[:, kc, c0 + db * 128:c0 + (db + 1) * 128], hT[:, kc, :], kc == 0, kc == 7,
                                   ["WB", "hT"], [kp])
                            emit(p[:], kp, dstT[db * 128:(db + 1) * 128, co * 512:(co + 1) * 512], func=AF.Sigmoid)
            T.barrier()

        SEG = 1024
        NSEG = 8192 // SEG
        with ExitStack() as es:
            def sb(name, shape, dt=F32):
                return es.enter_context(nc.sbuf_tensor(_u(name), shape, dt))
            Y = sb("Y", [128, 4, 2048])
            LA = sb("LA", [128, 32, 128], BF16); LB = sb("LB", [128, 32, 128], BF16)
            BL1b = sb("BL1b", [128, 32, 128], BF16); BL2b = sb("BL2b", [128, 32, 128], BF16)
            TH = sb("TH", [128, 32]); RHO = sb("RHO", [128, 32]); CAR = sb("CAR", [128, 32])
            PHf = sb("PHf", [128, 32]); OFFT = sb("OFFT", [128, 8, 32]); NOFF = sb("NOFF", [128, 8, 32])
            SB1 = sb("SB1", [128, 8, 32]); SB2 = sb("SB2", [128, 8, 32])
            DSK = sb("DSK", [128, 4])
            dma("sp", DSK[:], dsk_d, writes=["DSK"])
            with ExitStack() as e2:
                def sb2(name, shape, dt=F32):
                    return e2.enter_context(nc.sbuf_tensor(_u(name), shape, dt))
                LRE = sb2("LRE", [128, 32]); LIM = sb2("LIM", [128, 32]); LDT = sb2("LDT", [128, 32])
                CR = sb2("CR", [128, 32, 16]); CI = sb2("CI", [128, 32, 16])
                w = [sb2(f"w{i}", [128, 32]) for i in range(10)]
                c1 = sb2("c1", [128, 32, 16]); c2 = sb2("c2", [128, 32, 16])
                cpr = sb2("cpr", [128, 32, 16]); cpi = sb2("cpi", [128, 32, 16])
                for src, dstb, kb in ((bl1_d, BL1b, "BL1b"), (bl2_d, BL2b, "BL2b")):
                    dma("pool", dstb[:].rearrange("p g m -> p (g m)"), src, writes=[kb])
                dma("sp", LRE[:], lre_d, writes=["LRE"])
                dma("sp", LIM[:], lim_d, writes=["LIM"])
                dma("pool", LDT[:], ldt_d.to_broadcast([128, 32]), writes=["LDT"])
                dma("sp", CR[:].rearrange("p g c -> p (g c)"), cr_d, writes=["CR"])
                dma("sp", CI[:].rearrange("p g c -> p (g c)"), ci_d, writes=["CI"])
                K = "tb"
                dt_ = w[0]
                act(dt_[:], LDT[:], AF.Exp, ["LDT"], [K])
                tt("dve", TH[:], LIM[:], dt_[:], ALU.mult, ["LIM", K], ["TH"])
                tt("dve", w[1][:], LRE[:], dt_[:], ALU.mult, ["LRE", K], [K])
                act(RHO[:], w[1][:], AF.Exp, [K], ["RHO"])
                reduce_angle(TH[:], "TH", w[2][:], K, w[3][:], K, w[9][:], K)
                act(w[4][:], w[3][:], AF.Sin, [K, "CST"], [K], scale=ONE9)
                act(w[5][:], w[9][:], AF.Sin, [K, "CST"], [K], scale=NEG1, bias=HALFPI)
                tt("dve", w[6][:], RHO[:], w[5][:], ALU.mult, ["RHO", K], [K])
                ts("dve", w[6][:], w[6][:], -1.0, None, ALU.add, None, [K], [K])
                tt("dve", w[7][:], RHO[:], w[4][:], ALU.mult, ["RHO", K], [K])
                tt("dve", w[8][:], LRE[:], LRE[:], ALU.mult, ["LRE"], [K])
                tt("dve", w[9][:], LIM[:], LIM[:], ALU.mult, ["LIM"], [K])
                tt("dve", w[8][:], w[8][:], w[9][:], ALU.add, [K], [K])
                op("dve", lambda e: e.reciprocal(out=w[8][:], in_=w[8][:]), reads=[K], writes=[K])
                tt("dve", w[0][:], w[6][:], LRE[:], ALU.mult, [K, "LRE"], [K])
                tt("dve", w[1][:], w[7][:], LIM[:], ALU.mult, [K, "LIM"], [K])
                tt("dve", w[0][:], w[0][:], w[1][:], ALU.add, [K], [K])
                tt("dve", w[2][:], w[0][:], w[8][:], ALU.mult, [K], [K])
                tt("dve", w[0][:], w[7][:], LRE[:], ALU.mult, [K, "LRE"], [K])
                tt("dve", w[1][:], w[6][:], LIM[:], ALU.mult, [K, "LIM"], [K])
                tt("dve", w[0][:], w[0][:], w[1][:], ALU.subtract, [K], [K])
                tt("dve", w[3][:], w[0][:], w[8][:], ALU.mult, [K], [K])
                qrb = w[2][:, :].unsqueeze(2).to_broadcast([128, 32, 16])
                qib = w[3][:, :].unsqueeze(2).to_broadcast([128, 32, 16])
                tt("dve", c1[:], CR[:], qrb, ALU.mult, ["CR", K], [K])
                tt("dve", c2[:], CI[:], qib, ALU.mult, ["CI", K], [K])
                tt("dve", cpr[:], c1[:], c2[:], ALU.subtract, [K], [K])
                tt("dve", c1[:], CR[:], qib, ALU.mult, ["CR", K], [K])
                tt("dve", c2[:], CI[:], qrb, ALU.mult, ["CI", K], [K])
                tt("dve", cpi[:], c1[:], c2[:], ALU.add, [K], [K])
                ts("dve", c1[:], cpr[:], MLO, None, ALU.mult, None, [K, "CST"], [K])
                stt("dve", c1[:], cpi[:], NMHI, c1[:], ALU.mult, ALU.add, [K, "CST"], [K])
                ts("dve", c2[:], cpi[:], NMLO, None, ALU.mult, None, [K, "CST"], [K])
                stt("dve", c2[:], cpr[:], NMHI, c2[:], ALU.mult, ALU.add, [K, "CST"], [K])
                op("pool", lambda e: e.memset(LA[:], 0.0), writes=["LA"])
                op("pool", lambda e: e.memset(LB[:], 0.0), writes=["LB"])
                for src, dst, kd in ((c1, LA, "LA"), (c2, LB, "LB")):
                    dv = dst[:].rearrange("p (a r) (s c) -> p a r s c", r=8, s=8, c=16)
                    sv = src[:].rearrange("p (a r) c -> p a r c", r=8)
                    for r in range(8):
                        cp("dve", dv[:, :, r, r, :], sv[:, :, r, :], [K], [kd])
                op("dve", lambda e: e.memset(CAR[:], 0.0), writes=["CAR"])
                ts("dve", w[0][:], TH[:], INV2PI, MAGIC, ALU.mult, ALU.add, ["TH"], [K])
                ts("dve", w[0][:], w[0][:], -MAGIC, None, ALU.add, None, [K], [K])
                stt("dve", PHf[:], TH[:], INV2PI, w[0][:], ALU.mult, ALU.subtract, ["TH", K], ["PHf"])
                for sg in range(8):
                    ts("dve", w[1][:], PHf[:], float(sg * 1024), None, ALU.mult, None, ["PHf"], [K])
                    ts("dve", w[2][:], w[1][:], MAGIC, None, ALU.add, None, [K], [K])
                    stt("dve", OFFT[:, sg, :], w[2][:], -MAGIC, w[1][:], ALU.add, ALU.subtract, [K], ["OFFT"])
                    ts("dve", OFFT[:, sg, :], OFFT[:, sg, :], -1.0, None, ALU.mult, None, ["OFFT"], ["OFFT"])
                ts("dve", NOFF[:].rearrange("p a g -> p (a g)"), OFFT[:].rearrange("p a g -> p (a g)"), -1.0, None,
                   ALU.mult, None, ["OFFT"], ["NOFF"])
                ts("dve", SB2[:].rearrange("p a g -> p (a g)"), OFFT[:].rearrange("p a g -> p (a g)"),
                   TWO_PI, None, ALU.mult, None, ["OFFT"], ["SB2"])
                ts("dve", SB1[:].rearrange("p a g -> p (a g)"), SB2[:].rearrange("p a g -> p (a g)"), SHALF1, None,
                   ALU.mult, None, ["SB2", "CST"], ["SB1"])
                T.barrier()

            with ExitStack() as e2:
                def sb2(name, shape, dt=F32):
                    return e2.enter_context(nc.sbuf_tensor(_u(name), shape, dt))
                def ps2(name, shape, dt=F32):
                    return e2.enter_context(nc.psum_tensor(_u(name), shape, dt))
                UTb = sb2("UTb", [128, 8192], BF16)
                IOT = sb2("IOT", [128, SEG])
                UB = sb2("UB", [128, SEG]); U_ = sb2("U_", [128, SEG]); KK = sb2("KK", [128, SEG]); NF = sb2("NF", [128, SEG]); AB = sb2("AB", [128, SEG])
                COS2 = sb2("COS2", [128, SEG]); SINS = sb2("SINS", [128, SEG]); SIN2 = sb2("SIN2", [128, SEG])
                m2 = sb2("sm2", [128, 512])
                W = sb2("W", [128, SEG]); RB = sb2("RB", [128, SEG]); Z = sb2("Z", [128, SEG])
                A1b = sb2("A1b", [128, SEG], BF16); A2b = sb2("A2b", [128, SEG], BF16)
                py = [ps2(f"py{i}", [128, 512]) for i in range(4)]
                p1 = [ps2(f"p1{i}", [128, 512]) for i in range(2)]
                p2 = [ps2(f"p2{i}", [128, 512]) for i in range(2)]
                dma("pool", IOT[:], iota_d.to_broadcast([128, SEG]), writes=["IOT"])
                ZT = sb2("ZT", [128, 4096], BF16)
                op("pool", lambda e: e.memset(ZT[:], 0.0), writes=["ZT"])
                xdv = XD.rearrange("(p i) d -> p (i d)", p=128)
                for zi in range(64):
                    dma("sp", xdv[:, zi * 4096:(zi + 1) * 4096], ZT[:], reads=["ZT"], writes=[f"XDz{zi}"])
                pi_ = 0
                for cbk in range(4):
                    dma("sp", UTb[:], UT[cbk * 128:(cbk + 1) * 128, :], writes=["UTb"])
                    for gg in range(8):
                        g = cbk * 8 + gg
                        base = min((gg // 2) * 32, 64)
                        kr = 32 if gg // 2 < 3 else 64
                        cp("dve", RB[:], RHO[:, g:g + 1].to_broadcast([128, SEG]), ["RHO"], ["RB"])
                        ts("dve", UB[:], IOT[:], PHf[:, g:g + 1], None, ALU.mult, None, ["IOT", "PHf"], ["UB"])
                        for seg in range(NSEG):
                            ownseg = seg >= NSEG - 2048 // SEG
                            act(U_[:], UB[:], AF.Identity, ["UB", "OFFT"], ["U_"], bias=OFFT[:, seg, g:g + 1])
                            ts("dve", KK[:], U_[:], MAGIC, None, ALU.add, None, ["U_"], ["KK"])
                            stt("dve", NF[:], KK[:], -MAGIC, U_[:], ALU.add, ALU.subtract, ["KK", "U_"], ["NF"])
                            act(AB[:], NF[:], AF.Abs, ["NF"], ["AB"])
                            act(COS2[:], AB[:], AF.Sin, ["AB", "CST"], ["COS2"], scale=N2PI, bias=HALFPI)
                            act(SINS[:], NF[:], AF.Sin, ["NF", "CST"], ["SINS"], scale=SC1)
                            if ownseg:
                                act(SIN2[:], NF[:], AF.Sin, ["NF", "CST"], ["SIN2"], scale=N2PI)
                            for ch in range(SEG // 512):
                                tok0 = seg * SEG + ch * 512
                                q1 = p1[pi_ % 2]; q2 = p2[pi_ % 2]; k1 = f"p1{pi_ % 2}"; k2 = f"p2{pi_ % 2}"
                                pi_ += 1
                                mm(q1[:], BL1b[base:base + kr, g, :], UTb[base:base + kr, tok0:tok0 + 512], True, True,
                                   ["BL1b", "UTb"], [k1])
                                mm(q2[:], BL2b[base:base + kr, g, :], UTb[base:base + kr, tok0:tok0 + 512], True, True,
                                   ["BL2b", "UTb"], [k2])
                                wv = W[:, ch * 512:(ch + 1) * 512]
                                tt("dve", wv, q1[:], COS2[:, ch * 512:(ch + 1) * 512], ALU.mult, [k1, "COS2"], ["W"])
                                tt("dve", m2[:], q2[:], SINS[:, ch * 512:(ch + 1) * 512], ALU.mult, [k2, "SINS"], ["m2"])
                                tt("dve", wv, wv, m2[:], ALU.add, ["W", "m2"], ["W"])
                            op("dve", lambda e: e.tensor_tensor_scan(out=Z[:], data0=RB[:], data1=W[:],
                                                                      initial=CAR[:, g:g + 1], op0=ALU.mult, op1=ALU.add),
                               reads=["RB", "W", "CAR"], writes=["Z"])
                            cp("dve", CAR[:, g:g + 1], Z[:, SEG - 1:SEG], ["Z"], ["CAR"])
                            if ownseg:
                                tt("dve", A1b[:], Z[:], COS2[:], ALU.mult, ["Z", "COS2"], ["A1b"])
                                tt("dve", A2b[:], Z[:], SIN2[:], ALU.mult, ["Z", "SIN2"], ["A2b"])
                                so = seg - (NSEG - 2048 // SEG)
                                for ch in range(SEG // 512):
                                    yi = so * (SEG // 512) + ch
                                    mm(py[yi][:], LA[:, g, :], A1b[:, ch * 512:(ch + 1) * 512], gg == 0, False,
                                       ["LA", "A1b"], [f"py{yi}"])
                                    mm(py[yi][:], LB[:, g, :], A2b[:, ch * 512:(ch + 1) * 512], False, gg == 7,
                                       ["LB", "A2b"], [f"py{yi}"])
                    for yi in range(4):
                        stt("dve", Y[:, cbk, yi * 512:(yi + 1) * 512], UTb[:, 6144 + yi * 512:6144 + (yi + 1) * 512],
                            DSK[:, cbk:cbk + 1], py[yi][:], ALU.mult, ALU.add, ["UTb", "DSK", f"py{yi}"], ["Y"])
                T.barrier()

            with ExitStack() as e2:
                def sb2(name, shape, dt=F32):
                    return e2.enter_context(nc.sbuf_tensor(_u(name), shape, dt))
                def ps2(name, shape, dt=F32):
                    return e2.enter_context(nc.psum_tensor(_u(name), shape, dt))
                WGb = sb2("WGb", [128, 4, 512], BF16)
                t1 = sb2("gt1", [128, 2048]); t2 = sb2("gt2", [128, 2048])
                YGb = sb2("YGb", [128, 4, 2048], BF16)
                sgl = sb2("sgl", [128, 512]); ysb = [sb2(f"ysb{i}", [128, 512], BF16) for i in range(2)]
                pg = [ps2(f"pgl{i}", [128, 512]) for i in range(2)]
                dma("pool", WGb[:], wglu_d.rearrange("(k p) n -> p k n", p=128), writes=["WGb"])
                for cbk in range(4):
                    yv = Y[:, cbk, :]
                    act(t1[:], yv, AF.Square, ["Y"], ["t1"])
                    ts("dve", t1[:], t1[:], 0.044715, 1.0, ALU.mult, ALU.add, ["t1"], ["t1"])
                    tt("dve", t1[:], t1[:], yv, ALU.mult, ["t1", "Y"], ["t1"])
                    act(t2[:], t1[:], AF.Sigmoid, ["t1"], ["t2"], scale=1.5957691216057308)
                    tt("dve", yv, yv, t2[:], ALU.mult, ["Y", "t2"], ["Y"])
                    act(YGb[:, cbk, :], yv, AF.Copy, ["Y"], ["YGb"])
                i = 0
                for cbo in range(4):
                    for ch in range(4):
                        p = pg[i % 2]; kp = f"pgl{i % 2}"; o = ysb[i % 2]; ko = f"ysb{i % 2}"
                        i += 1
                        for kc in range(4):
                            mm(p[:], WGb[:, kc, cbo * 128:(cbo + 1) * 128], YGb[:, kc, ch * 512:(ch + 1) * 512],
                               kc == 0, kc == 3, ["WGb", "YGb"], [kp])
                        act(sgl[:], p[:], AF.Sigmoid, [kp], ["sgl"])
                        tt("dve", o[:], Y[:, cbo, ch * 512:(ch + 1) * 512], sgl[:], ALU.mult, ["Y", "sgl"], [ko])
                        dma("sp", YST[cbo * 128:(cbo + 1) * 128, ch * 512:(ch + 1) * 512], o[:], reads=[ko])
                T.barrier()

        with ExitStack() as es:
            def sb(name, shape, dt=F32):
                return es.enter_context(nc.sbuf_tensor(_u(name), shape, dt))
            def ps(name, shape, dt=F32):
                return es.enter_context(nc.psum_tensor(_u(name), shape, dt))
            KA = sb("KA", [128, 8192], BF16)
            VA = sb("VA", [128, 64, 65], BF16)
            QA = sb("QA", [128, 2048], BF16)
            DM = sb("DM", [128, 4, 512], BF16)
            VB = sb("VB", [128, 8, 96]); VVd = sb("VVd", [128, 8, 96]); VO = sb("VO", [128, 8, 96])
            KM = sb("KM", [64, 32]); KMb = sb("KMb", [64, 32], BF16)
            gt_ = sb("gt", [128, 96]); top8 = sb("top8", [128, 8]); sel = sb("sel", [128, 96])
            pt = [sb(f"pt{i}", [128, 512], BF16) for i in range(3)]
            rrow = sb("rrow", [128, 512]); rhi = sb("rhi", [128, 512], BF16); rlo = sb("rlo", [128, 512], BF16)
            rtmp = sb("rtmp", [128, 512])
            onesb = sb("onesb", [128, 64], BF16)
            bc = sb("bc", [64, 512]); oab = sb("oab", [64, 512], BF16)
            pS = [ps(f"pS{i}", [128, 512]) for i in range(3)]
            pO = [ps(f"pO{i}", [128, 512]) for i in range(2)]
            pG = ps("pG", [128, 96]); pB = ps("pB", [128, 128]); pO2 = ps("pO2", [64, 512])
            dma("sp", KA[64:96, :], bonehot_d, writes=["KAoh"])
            dma("sp", DM[:].rearrange("p a n -> p (a n)"), dm_d, writes=["DM"])
            dma("pool", VB[:].rearrange("p a n -> p (a n)"), vb_d.to_broadcast([128, 768]), writes=["VB"])
            dma("pool", VVd[:].rearrange("p a n -> p (a n)"), vv_d.to_broadcast([128, 768]), writes=["VVd"])
            dma("pool", VO[:].rearrange("p a n -> p (a n)"), vo_d.to_broadcast([128, 768]), writes=["VO"])
            op("pool", lambda e: e.memset(VA[:, :, 64:65], 1.0), writes=["VA1"])
            op("pool", lambda e: e.memset(onesb[:], 1.0), writes=["onesb"])
            op("pool", lambda e: e.memset(gt_[:], 0.0), writes=["gt"])
            itb = [0]
            for h in range(8):
                dma("sp", KA[0:64, :], KT[h * 64:(h + 1) * 64, :], writes=["KA"])
                dma("sp", QA[0:64, :], QT[h * 64:(h + 1) * 64, :], writes=["QA"])
                dma("pool", VA[:, :, 0:64], VV[:, h * 64:(h + 1) * 64].rearrange("(t p) d -> p t d", p=128),
                    writes=["VA"])
                op("dve", lambda e: e.tensor_reduce(out=KM[:], in_=KA[0:64, :].rearrange("p (n l) -> p n l", l=256),
                                                    axis=AX.X, op=ALU.add), reads=["KA"], writes=["KM"])
                cp("dve", KMb[:], KM[:], ["KM"], ["KMb"])
                for qt in range(16):
                    qb = qt // 2
                    mm(pG[:, 64:96], QA[0:64, qt * 128:(qt + 1) * 128], KMb[:], True, True, ["QA", "KMb"], ["pG"])
                    tt("dve", gt_[:, 64:96], pG[:, 64:96], VB[:, qb, 64:96], ALU.add, ["pG", "VB"], ["gt"])
                    op("dve", lambda e: e.max(out=top8[:], in_=gt_[:, 64:96]), reads=["gt"], writes=["top8"])
                    ts("dve", sel[:, 64:96], gt_[:, 64:96], top8[:, 2:3], None, ALU.is_ge, None, ["gt", "top8"], ["sel"])
                    tt("dve", sel[:, 64:96], sel[:, 64:96], VVd[:, qb, 64:96], ALU.mult, ["sel", "VVd"], ["sel"])
                    tt("dve", sel[:, 64:96], sel[:, 64:96], VO[:, qb, 64:96], ALU.add, ["sel", "VO"], ["sel"])
                    ts("dve", gt_[:, 64:96], sel[:, 64:96], -1.0, -NEG, ALU.add, ALU.mult, ["sel"], ["gt"])
                    op("pe", lambda e: e.transpose(out=pB[0:96, :], in_=gt_[:, 0:96], identity=ident[:]),
                       reads=["gt", "ident"], writes=["pB"])
                    cp("dve", QA[64:96, qt * 128:(qt + 1) * 128], pB[64:96, :], ["pB"], ["QAb"])
                for G in range(4):
                    cnt = 52 + 4 * G
                    po = pO[G % 2]; kpo = f"pO{G % 2}"

                    def qk(kt):
                        i3 = (itb[0] + kt) % 3
                        mm(pS[i3][:], KA[0:96, kt * 128:(kt + 1) * 128], QA[0:96, G * 512:(G + 1) * 512], True, True,
                           ["KA", "KAoh", "QA", "QAb"], [f"pS{i3}"])
                    qk(0)
                    qk(1)
                    for kt in range(cnt):
                        i3 = (itb[0] + kt) % 3
                        s_ = pS[i3]; ks = f"pS{i3}"; p_ = pt[i3]; kp = f"pt{i3}"
                        if kt + 2 < cnt:
                            qk(kt + 2)
                        act(p_[:], s_[:], AF.Exp, [ks], [kp])
                        di = kt - (cnt - 4)
                        if di >= 0:
                            tt("dve", p_[:], p_[:], DM[:, di, :], ALU.mult, [kp, "DM"], [kp])
                        mm(po[0:65, :], VA[:, kt, 0:65], p_[:], kt == 0, kt == cnt - 1, ["VA", "VA1", kp], [kpo])
                    itb[0] += cnt
                    cp("dve", rrow[64:65, :], po[64:65, :], [kpo], ["rrow"])
                    op("dve", lambda e: e.reciprocal(out=rrow[64:65, :], in_=rrow[64:65, :]), reads=["rrow"], writes=["rrow"])
                    cp("dve", rhi[64:65, :], rrow[64:65, :], ["rrow"], ["rhi"])
                    cp("dve", rtmp[64:65, :], rhi[64:65, :], ["rhi"], ["rtmp"])
                    tt("dve", rtmp[64:65, :], rrow[64:65, :], rtmp[64:65, :], ALU.subtract, ["rrow", "rtmp"], ["rtmp"])
                    cp("dve", rlo[64:65, :], rtmp[64:65, :], ["rtmp"], ["rlo"])
                    mm(pO2[:], onesb[64:65, 0:64], rhi[64:65, :], True, False, ["onesb", "rhi"], ["pO2"])
                    mm(pO2[:], onesb[64:65, 0:64], rlo[64:65, :], False, True, ["onesb", "rlo"], ["pO2"])
                    cp("dve", bc[:], pO2[:], ["pO2"], ["bc"])
                    tt("dve", oab[:], po[0:64, :], bc[:], ALU.mult, [kpo, "bc"], ["oab"])
                    dma("sp", OAT[h * 64:(h + 1) * 64, G * 512:(G + 1) * 512], oab[:], reads=["oab"])
            T.barrier()

        with ExitStack() as es:
            def sb(name, shape, dt=F32):
                return es.enter_context(nc.sbuf_tensor(_u(name), shape, dt))
            def ps(name, shape, dt=F32):
                return es.enter_context(nc.psum_tensor(_u(name), shape, dt))
            Wsb = sb("Wsb", [128, 4, 1024], BF16); Wab = sb("Wab", [128, 4, 1024], BF16)
            Wo = sb("Wo", [128, 8, 1024], BF16)
            ys = sb("ys", [128, 4, 512], BF16); oa = sb("oa", [128, 4, 512], BF16)
            gsT = sb("gsT", [128, 8, 512], BF16); gaT = sb("gaT", [128, 8, 512], BF16)
            MT = sb("MT", [128, 8, 512], BF16)
            b1 = sb("b1", [128, 512]); b2 = sb("b2", [128, 512])
            xt = sb("xt2", [128, 1024]); x1 = sb("x1", [128, 1024]); tmp = sb("tmpE", [128, 1024]); tmp2 = sb("tmp2E", [128, 1024])
            sq = sb("sqE", [128, 1024], BF16); ss = sb("ssE", [128, 1]); ss2 = sb("ss2E", [128, 1]); rs = sb("rsE", [128, 1])
            hbs = [sb(f"hbE{i}", [128, 1024], BF16) for i in range(2)]; h2T = sb("h2T", [128, 8, 512], BF16)
            RWb = sb("RWb", [128, 8, 32], BF16); RBb = sb("RBb", [128, 32]); LT = sb("LT", [128, 128], BF16)
            ONESb = sb("ONESb", [128, 128], BF16); CNT = sb("CNT", [128, 32]); EOFF = sb("EOFF", [128, 32])
            mb = sb("mb", [128, 32], BF16); posf = sb("posfE", [128, 32]); idxf = sb("idxf", [128, 32])
            lg = sb("lg", [128, 32]); top8 = sb("top8F", [128, 8]); msk = sb("msk", [128, 32]); ex = sb("ex", [128, 32])
            nmx = sb("nmx", [128, 1]); sm = sb("sm", [128, 1])
            pl = ps("pl", [128, 32]); pp = ps("pp", [128, 32]); pc = ps("pc", [128, 32])
            dma("pool", RWb[:], rw_d.rearrange("(k p) n -> p k n", p=128), writes=["RWb"])
            dma("pool", RBb[:], rb_d.to_broadcast([128, 32]), writes=["RBb"])
            dma("pool", EOFF[:], eoff_d.to_broadcast([128, 32]), writes=["EOFF"])
            dma("sp", LT[:], ltri_d, writes=["LT"])
            op("pool", lambda e: e.memset(ONESb[:], 1.0), writes=["ONESb"])
            op("pool", lambda e: e.memset(CNT[:], 0.0), writes=["CNT"])
            pb1 = ps("pb1", [128, 512]); pb2 = ps("pb2", [128, 512])
            pmx = ps("pmx", [128, 1024]); pT = ps("pTE", [128, 8, 128], BF16)
            for (src, dst, kd, nk) in ((wsb_d, Wsb, "Wsb", 4), (wab_d, Wab, "Wab", 4), (wout_d, Wo, "Wo", 8)):
                dma("pool", dst[:], src.rearrange("(k p) n -> p k n", p=128), writes=[kd])
            for c in range(4):
                cs_ = slice(c * 512, (c + 1) * 512)
                dma("sp", ys[:], YST[:, cs_].rearrange("(k p) n -> p k n", p=128), writes=["ys"])
                dma("sp", oa[:], OAT[:, cs_].rearrange("(k p) n -> p k n", p=128), writes=["oa"])
                dma("pool", gsT[:], GST[:, cs_].rearrange("(k p) n -> p k n", p=128), writes=["gsT"])
                dma("pool", gaT[:], GAT[:, cs_].rearrange("(k p) n -> p k n", p=128), writes=["gaT"])
                for db in range(8):
                    for kc in range(4):
                        mm(pb1[:], Wsb[:, kc, db * 128:(db + 1) * 128], ys[:, kc, :], kc == 0, kc == 3, ["Wsb", "ys"], ["pb1"])
                    for kc in range(4):
                        mm(pb2[:], Wab[:, kc, db * 128:(db + 1) * 128], oa[:, kc, :], kc == 0, kc == 3, ["Wab", "oa"], ["pb2"])
                    tt("dve", b1[:], pb1[:], gsT[:, db, :], ALU.mult, ["pb1", "gsT"], ["b1"])
                    tt("dve", b2[:], pb2[:], gaT[:, db, :], ALU.mult, ["pb2", "gaT"], ["b2"])
                    tt("dve", MT[:, db, :], b1[:], b2[:], ALU.add, ["b1", "b2"], ["MT"])
                for j in range(4):
                    t = 4 * c + j
                    for half in range(2):
                        for kc in range(8):
                            mm(pmx[:, half * 512:(half + 1) * 512], MT[:, kc, j * 128:(j + 1) * 128],
                               Wo[:, kc, half * 512:(half + 1) * 512], kc == 0, kc == 7, ["MT", "Wo"], ["pmx"])
                    dma("sp", xt[:], x_loc[6144 + t * 128:6144 + (t + 1) * 128, :], writes=["xt"])
                    act(sq[:, 0:512], pmx[:, 0:512], AF.Square, ["pmx"], ["sq", "ss"], accum_out=ss[:])
                    act(sq[:, 512:1024], pmx[:, 512:1024], AF.Square, ["pmx"], ["sq", "ss2"], accum_out=ss2[:])
                    tt("dve", ss[:], ss[:], ss2[:], ALU.add, ["ss", "ss2"], ["ss"])
                    ts("dve", rs[:], ss[:], 1.0 / 1024, 1e-6, ALU.mult, ALU.add, ["ss"], ["rs"])
                    act(rs[:], rs[:], AF.Sqrt, ["rs"], ["rs"])
                    op("dve", lambda e: e.reciprocal(out=rs[:], in_=rs[:]), reads=["rs"], writes=["rs"])
                    cp("dve", tmp[:], pmx[:], ["pmx"], ["tmp"])
                    stt("dve", tmp[:], tmp[:], rs[:, 0:1], PRM[:, 2, :], ALU.mult, ALU.mult, ["tmp", "rs", "PRM"], ["tmp"])
                    tt("dve", x1[:], tmp[:], xt[:], ALU.add, ["tmp", "xt"], ["x1"])
                    dma("sp", X1[t * 128:(t + 1) * 128, :], x1[:], reads=["x1"])
                    hb = hbs[t % 2]; khb = f"hb{t % 2}"
                    norm_mod(sb, x1[:], "x1", 3, 4, None, hb[:], khb, tmp, tmp2, ss, rs, sq)
                    for kc in range(8):
                        op("pe", lambda e: e.transpose(out=pT[:, kc, :], in_=hb[:, kc * 128:(kc + 1) * 128],
                                                       identity=identb[:]),
                           reads=[khb, "identb"], writes=["pT"])
                    cp("dve", h2T[:, :, j * 128:(j + 1) * 128], pT[:], ["pT"], ["h2T"])
                    for kc in range(8):
                        mm(pl[:], h2T[:, kc, j * 128:(j + 1) * 128], RWb[:, kc, :], kc == 0, kc == 7, ["h2T", "RWb"], ["pl"])
                    tt("dve", lg[:], pl[:], RBb[:], ALU.add, ["pl", "RBb"], ["lg"])
                    op("dve", lambda e: e.max(out=top8[:], in_=lg[:]), reads=["lg"], writes=["top8"])
                    ts("dve", msk[:], lg[:], top8[:, 3:4], None, ALU.is_ge, None, ["lg", "top8"], ["msk"])
                    ts("dve", nmx[:], top8[:, 0:1], -1.0, None, ALU.mult, None, ["top8"], ["nmx"])
                    act(ex[:], lg[:], AF.Exp, ["lg", "nmx"], ["ex"], bias=nmx[:, 0:1])
                    tt("dve", ex[:], ex[:], msk[:], ALU.mult, ["ex", "msk"], ["ex"])
                    op("dve", lambda e: e.reduce_sum(out=sm[:], in_=ex[:], axis=AX.X), reads=["ex"], writes=["sm"])
                    op("dve", lambda e: e.reciprocal(out=sm[:], in_=sm[:]), reads=["sm"], writes=["sm"])
                    ts("dve", RWT[:, t, :], ex[:], sm[:, 0:1], None, ALU.mult, None, ["ex", "sm"], ["RWT"])
                    cp("dve", mb[:], msk[:], ["msk"], ["mb"])
                    mm(pp[:], LT[:], mb[:], True, True, ["LT", "mb"], ["pp"])
                    mm(pc[:], ONESb[:], mb[:], True, True, ["ONESb", "mb"], ["pc"])
                    tt("dve", posf[:], pp[:], CNT[:], ALU.add, ["pp", "CNT"], ["posf"])
                    tt("dve", CNT[:], CNT[:], pc[:], ALU.add, ["CNT", "pc"], ["CNT"])
                    ts("dve", idxf[:], posf[:], float(CAP), None, ALU.is_lt, None, ["posf"], ["idxf"])
                    tt("dve", msk[:], msk[:], idxf[:], ALU.mult, ["msk", "idxf"], ["msk"])
                    tt("dve", idxf[:], posf[:], EOFF[:], ALU.add, ["posf", "EOFF"], ["idxf"])
                    tt("dve", idxf[:], idxf[:], msk[:], ALU.mult, ["idxf", "msk"], ["idxf"])
                    ts("dve", idxf[:], idxf[:], 40000.0, None, ALU.add, None, ["idxf"], ["idxf"])
                    cp("dve", IDX[:, t * 32:(t + 1) * 32], idxf[:], ["idxf"], [f"IDX{t}"])
                    for e_ in range(32):
                        T.dma_fn("pool", lambda g, e_=e_, hb=hb: g.indirect_dma_start(
                            out=XD[:, :], out_offset=bass.IndirectOffsetOnAxis(ap=IDX[:, t * 32 + e_:t * 32 + e_ + 1], axis=0),
                            in_=hb[:, :], in_offset=None, bounds_check=BCREG, oob_is_err=False),
                            reads=[khb, f"IDX{t}"], writes=[f"XD{e_}"])
                dma("sp", H2T[:, cs_].rearrange("(k p) n -> p k n", p=128), h2T[:], reads=["h2T"])
            T.barrier()

        aes.close()
        with ExitStack() as es:
            def sb(name, shape, dt=F32):
                return es.enter_context(nc.sbuf_tensor(_u(name), shape, dt))
            def ps(name, shape, dt=F32):
                return es.enter_context(nc.psum_tensor(_u(name), shape, dt))
            acc = sb("acc", [128, 16, 1024])
            Wg = [sb(f"Wg{i}", [128, 8, 1024], BF16) for i in range(2)]
            Wu = [sb(f"Wu{i}", [128, 8, 1024], BF16) for i in range(2)]
            Wds = [sb(f"Wd{i}", [128, 8, 1024], BF16) for i in range(2)]
            XT1 = sb("XT0", [128, 8, 512], BF16)
            XT = [XT1, XT1]
            xs = [sb(f"xs{i}", [128, 1024], BF16) for i in range(2)]
            BG = sb("BG", [128, 32, 8]); BU = sb("BU", [128, 32, 8])
            BD = sb("BD", [128, 1024], BF16)
            aT = sb("aT", [128, 8, 512], BF16)
            gb = [sb("g_0", [128, 512])] * 2; ub = [sb("u_0", [128, 512])] * 2
            sb_ = [sb("s_0", [128, 512])] * 2; u0b = [sb("u0_0", [128, 512])] * 2
            yo = [sb(f"yo{i}", [128, 512], BF16) for i in range(2)]
            tg = [sb(f"tg{i}", [128, 1024], BF16) for i in range(2)]
            ssF = sb("ssF", [128, 1]); ss2F = sb("ss2F", [128, 1])
            pgt = [ps(f"pgt{i}", [128, 512]) for i in range(2)]
            put = [ps(f"put{i}", [128, 512]) for i in range(2)]
            pdn = [ps(f"pdn{i}", [128, 512]) for i in range(2)]
            pT = ps("pTF", [128, 8, 128], BF16)
            dma("sp", BG[:].rearrange("p e k -> p (e k)"), bg_d, writes=["BG"])
            dma("sp", BU[:].rearrange("p e k -> p (e k)"), bu_d, writes=["BU"])
            op("pool", lambda e: e.memset(acc[:], 0.0), writes=["acc"])
            for i in range(2):
                op("pool", lambda e, i=i: e.memset(tg[i][:], 0.0), writes=[f"tg{i}"])

            def load_gu(e):
                sl = e % 2
                dma("pool", Wg[sl][:], wg_d[e].rearrange("(k p) n -> p k n", p=128), writes=[f"Wg{sl}"])
                dma("pool", Wu[sl][:], wu_d[e].rearrange("(k p) n -> p k n", p=128), writes=[f"Wu{sl}"])

            def load_d(e):
                dma("pool", Wds[e % 2][:], wd_d[e].rearrange("(k p) n -> p k n", p=128), writes=[f"Wd{e % 2}"])

            def load_bd(e):
                dma("pool", BD[:], bd_d[e:e + 1, :].to_broadcast([128, 1024]), writes=["BD"])

            xi = [0]

            def build_xt(e, c):
                for st in range(4):
                    x_ = xs[xi[0] % 2]; kx = f"xs{xi[0] % 2}"
                    xi[0] += 1
                    r0 = e * CAP + c * 512 + st * 128
                    dma("sp", x_[:], XD[r0:r0 + 128, :], writes=[kx])
                    for kc in range(8):
                        op("pe", lambda en: en.transpose(out=pT[:, kc, :], in_=x_[:, kc * 128:(kc + 1) * 128],
                                                        identity=identb[:]),
                           reads=[kx, "identb"], writes=["pTF"])
                    act(XT[c][:, :, st * 128:(st + 1) * 128], pT[:], AF.Copy, ["pTF"], ["XT0"])

            def GU(e, c):
                sl = e % 2
                h_ = XT[c]; kh = "XT0"

                def tail(fc):
                    i2 = 0
                    ts("dve", ub[i2][:], u0b[i2][:], -7.0, 7.0, ALU.max, ALU.min, [f"u0_{i2}"], [f"u_{i2}"])
                    stt("dve", aT[:, fc, :], ub[i2][:], 1.0, sb_[i2][:], ALU.add, ALU.mult,
                        [f"u_{i2}", f"s_{i2}"], ["aT"])

                for fc in range(8):
                    i2 = fc % 2
                    pg_ = pgt[i2]; kg = f"pgt{i2}"; pu_ = put[i2]; ku = f"put{i2}"
                    for kc in range(8):
                        mm(pg_[:], Wg[sl][:, kc, fc * 128:(fc + 1) * 128], h_[:, kc, :], kc == 0, kc == 7,
                           [f"Wg{sl}", kh], [kg])
                    for kc in range(8):
                        mm(pu_[:], Wu[sl][:, kc, fc * 128:(fc + 1) * 128], h_[:, kc, :], kc == 0, kc == 7,
                           [f"Wu{sl}", kh], [ku])
                    ts("dve", gb[0][:], pg_[:], BG[:, e, fc:fc + 1], 7.0, ALU.add, ALU.min, [kg, "BG"], ["g_0"])
                    act(u0b[0][:], pu_[:], AF.Identity, [ku, "BU"], ["u0_0"], bias=BU[:, e, fc:fc + 1])
                    act(sb_[0][:], gb[0][:], AF.Silu, ["g_0"], ["s_0"], scale=1.702)
                    tail(fc)

            yi = [0]

            def DN(e, c):
                for j in range(4):
                    r0 = e * CAP + c * 512 + j * 128
                    for half in range(2):
                        pd_ = pdn[half]; kd = f"pdn{half}"
                        for fc in range(8):
                            mm(pd_[:], aT[:, fc, j * 128:(j + 1) * 128], Wds[e % 2][:, fc, half * 512:(half + 1) * 512],
                               fc == 0, fc == 7, ["aT", f"Wd{e % 2}"], [kd])
                        y_ = yo[yi[0] % 2]; ky = f"yo{yi[0] % 2}"
                        yi[0] += 1
                        stt("dve", y_[:], pd_[:], 1.0 / 1.702, BD[:, half * 512:(half + 1) * 512], ALU.mult, ALU.add,
                            [kd, "BD"], [ky])
                        dma("sp", YD[r0:r0 + 128, half * 512:(half + 1) * 512], y_[:], reads=[ky],
                            writes=[f"YD{e}_{c * 8 + j * 2 + half}"])

            gi = [0]

            def combine(e):
                for i in range(16):
                    t_ = tg[gi[0] % 2]; kt_ = f"tg{gi[0] % 2}"
                    gi[0] += 1
                    T.dma_fn("pool", lambda g, t_=t_, i=i: g.indirect_dma_start(
                        out=t_[:, :], out_offset=None, in_=YD[:, :],
                        in_offset=bass.IndirectOffsetOnAxis(ap=IDX[:, i * 32 + e:i * 32 + e + 1], axis=0),
                        bounds_check=BCREG, oob_is_err=False),
                        reads=[f"YD{e}_{m}" for m in range(16)], writes=[kt_])
                    stt("dve", acc[:, i, :], t_[:], RWT[:, i, e:e + 1], acc[:, i, :], ALU.mult, ALU.add,
                        [kt_, "acc"], ["acc"])

            load_gu(0)
            load_d(0)
            load_bd(0)
            load_gu(1)
            load_d(1)
            build_xt(0, 0)
            for e in range(32):
                GU(e, 0)
                build_xt(e, 1)
                DN(e, 0)
                GU(e, 1)
                if e + 2 < 32:
                    load_gu(e + 2)
                if e + 1 < 32:
                    build_xt(e + 1, 0)
                DN(e, 1)
                if e + 2 < 32:
                    load_d(e + 2)
                if e + 1 < 32:
                    load_bd(e + 1)
                combine(e)
            T.barrier()
            xv = Wg[0][:].rearrange("p k n -> p (k n)").bitcast(F32)
            sqv = Wu[0][:].rearrange("p k n -> p (k n)")
            for t in range(16):
                x1v = xv[:, (t % 2) * 2048:(t % 2) * 2048 + 1024]; ov = xv[:, (t % 2) * 2048 + 1024:(t % 2) * 2048 + 2048]
                kx = f"x1v{t % 2}"; ko = f"ov{t % 2}"
                dma("sp", x1v, X1[t * 128:(t + 1) * 128, :], writes=[kx])
                act(sqv[:, 0:1024], acc[:, t, :], AF.Square, ["acc"], ["sqv", "ssF"], accum_out=ssF[:])
                ts("dve", ss2F[:], ssF[:], 1.0 / 1024, 1e-6, ALU.mult, ALU.add, ["ssF"], ["ss2F"])
                act(ss2F[:], ss2F[:], AF.Sqrt, ["ss2F"], ["ss2F"])
                op("dve", lambda e: e.reciprocal(out=ss2F[:], in_=ss2F[:]), reads=["ss2F"], writes=["ss2F"])
                stt("dve", ov, acc[:, t, :], ss2F[:, 0:1], G2[:], ALU.mult, ALU.mult, ["acc", "ss2F", "G2"], [ko])
                tt("dve", ov, ov, x1v, ALU.add, [ko, kx], [ko])
                dma("sp", out_d[t * 128:(t + 1) * 128, :], ov, reads=[ko])
            T.finish("sp")
    return nc


_NC = None


def _bf16(a):
    return a.astype(ml_dtypes.bfloat16)


def kernel(**inp):
    global _NC
    f32 = np.float32
    x = np.asarray(inp["x"], f32)
    c = np.asarray(inp["c"], f32)
    positions = np.asarray(inp["positions"]).astype(np.int32)
    sq = lambda k: np.asarray(inp[k])[0]
    if _NC is None:
        _NC = build_program()
    nc = _NC
    gains = np.concatenate([sq("mix_pre_g"), sq("mix_post_g"), sq("ffn_pre_g"), sq("ffn_post_g")])[None, :].astype(f32)
    p = np.arange(128)
    inv_freq = (10000.0 ** (-np.arange(32, dtype=np.float32) / np.float32(32))).astype(f32)
    cst = np.zeros((128, 16), f32)
    sgn_r = np.where((p % 64) < 32, -1.0, 1.0).astype(f32)
    shalf = np.where(p < 64, 1.0, -1.0).astype(f32)
    cst[:, 1] = sgn_r
    cst[:, 2] = np.pi / 2
    cst[:, 3] = inv_freq[p % 32]
    cst[:, 4] = (p < 64)
    cst[:, 5] = -(p >= 64).astype(f32)
    cst[:, 6] = shalf * 0.999999
    cst[:, 7] = -1.0
    cst[:, 8] = -(p < 64).astype(f32)
    cst[:, 9] = 1.0
    cst[:, 10] = -TWO_PI_LO
    cst[:, 11] = -TWO_PI_LO * shalf
    cst[:, 13] = 12582912.0
    cst[:, 12] = shalf
    ident = np.eye(128, dtype=f32)
    iota = np.arange(1024, dtype=f32)[None, :]
    lre = np.concatenate([sq("ssm_lam_re").T, sq("ssm_lam_re").T], 0).astype(f32)
    lim = np.concatenate([sq("ssm_lam_im").T, sq("ssm_lam_im").T], 0).astype(f32)
    ldt = sq("ssm_log_dt")[None, :].astype(f32)
    bre = sq("ssm_b_re"); bim = sq("ssm_b_im")
    bl1 = np.zeros((128, 32, 128), f32); bl2 = np.zeros((128, 32, 128), f32)
    for g in range(32):
        r0 = ((g % 8) // 2) * 32 + (g % 2) * 16
        bl1[r0:r0 + 16, g, 0:64] = bre[g].T; bl1[r0:r0 + 16, g, 64:128] = bim[g].T
        bl2[r0:r0 + 16, g, 0:64] = bim[g].T; bl2[r0:r0 + 16, g, 64:128] = bre[g].T
    cre = sq("ssm_c_re"); cim = sq("ssm_c_im")
    crp = np.transpose(cre, (2, 0, 1)); cip = np.transpose(cim, (2, 0, 1))
    cr = np.concatenate([crp, crp], 0).reshape(128, 512).astype(f32)
    ci = np.concatenate([cip, cip], 0).reshape(128, 512).astype(f32)
    dsk = sq("ssm_d").reshape(4, 128).T.copy().astype(f32)
    bonehot = np.zeros((32, 8192), f32)
    for n in range(32):
        bonehot[n, n * 256:(n + 1) * 256] = 1.0
    bonehot = _bf16(bonehot)
    dmask = np.ones((128, 4, 512), f32)
    kk = np.arange(128)[:, None]; qq = np.arange(128)[None, :]
    for i in range(4):
        for j in range(4):
            if i // 2 == j // 2:
                if i == j:
                    dmask[:, i, j * 128:(j + 1) * 128] = (kk <= qq)
                elif i > j:
                    dmask[:, i, j * 128:(j + 1) * 128] = 0.0
    dmask = _bf16(dmask.reshape(128, 2048))
    bg = np.transpose(sq("b_gate").reshape(32, 8, 128), (2, 0, 1)).reshape(128, 256).astype(f32)
    bu = np.transpose(sq("b_up").reshape(32, 8, 128), (2, 0, 1)).reshape(128, 256).astype(f32)
    ltri = _bf16((np.arange(128)[:, None] < np.arange(128)[None, :]).astype(f32))
    eoff = (np.arange(32, dtype=f32) * 1024.0 - 40000.0)[None, :]
    shared = {
        "ltri": ltri, "eoff": eoff,
        "ada_w": sq("ada_w"), "ada_b": sq("ada_b")[None, :], "gains": gains, "w_in": sq("w_in"),
        "cst": cst, "ident": ident, "iota": iota, "lre": lre, "lim": lim, "ldt": ldt,
        "bl1": bl1.reshape(128, 4096), "bl2": bl2.reshape(128, 4096), "cr": cr, "ci": ci, "dsk": dsk,
        "w_glu": sq("ssm_w_glu"), "w_sb": sq("w_ssm_branch"), "w_ab": sq("w_attn_branch"), "w_out": sq("w_out"),
        "bonehot": bonehot, "dmask": dmask, "router_w": sq("router_w"), "router_b": sq("router_b")[None, :],
        "w_gate": sq("w_gate"), "w_up": sq("w_up"), "w_down": sq("w_down"),
        "b_gate": bg, "b_up": bu, "b_down": sq("b_down"),
    }
    shared = {k: np.ascontiguousarray(v) for k, v in shared.items()}
    in_maps = []
    for core in range(8):
        b, j = core // 4, core % 4
        nprev = j * 2048
        x_loc = np.zeros((8192, 1024), f32)
        x_loc[6144 - nprev:] = x[b, :nprev + 2048]
        tm = np.zeros(8192, f32); tm[6144 - nprev:] = 1.0
        pos = np.zeros(8192, np.int32); pos[6144 - nprev:] = positions[b, :nprev + 2048]
        ninv = (3 - j) * 8
        vb = np.zeros((8, 96), f32); vv = np.zeros((8, 96), f32); vo = np.zeros((8, 96), f32)
        for qb in range(8):
            n = np.arange(32)
            valid = (n >= ninv) & (n < 24 + qb)
            vb[qb, 64:96] = np.where(valid, 0.0, -1e30)
            vv[qb, 64:96] = valid
            vo[qb, 64 + 24 + qb] = 1.0
        m = dict(shared)
        m.update({
            "x_loc": x_loc, "tmask": np.ascontiguousarray(tm.reshape(64, 128).T),
            "pos_loc": pos[None, :], "c_b": np.ascontiguousarray(c[b].reshape(8, 128).T),
            "vbias": vb.reshape(1, 768), "vvalid": vv.reshape(1, 768), "vown": vo.reshape(1, 768),
        })
        in_maps.append(m)
    res = run_bass_kernel_spmd(nc, in_maps, core_ids=list(range(8)))
    out = np.zeros((2, 8192, 1024), f32)
    for core in range(8):
        b, j = core // 4, core % 4
        out[b, j * 2048:(j + 1) * 2048] = res.results[core]["out"]
    return out
```

```python
import numpy as np
from contextlib import ExitStack
import ml_dtypes
import concourse.bass as bass
import concourse.mybir as mybir
from concourse.bass_utils import run_bass_kernel_spmd

F32 = mybir.dt.float32
BF16 = mybir.dt.bfloat16
I32 = mybir.dt.int32
ALU = mybir.AluOpType
AF = mybir.ActivationFunctionType
AX = mybir.AxisListType
PI = float(np.pi)
TWO_PI = float(2 * np.pi)
PI_LO = 3.1415925
TWO_PI_LO = 6.283185
SEM_CAP = 30000
NEG = -30000.0


class Tracker:
    def __init__(self, nc):
        self.nc = nc
        self.engs = {"pe": nc.tensor, "act": nc.scalar, "dve": nc.vector,
                     "pool": nc.gpsimd, "sp": nc.sync}
        self.cur_sem = {}
        self.cnt = {}
        self.nsem = 0
        for e in ("pe", "act", "dve", "pool"):
            self._new_epoch(e)
        self.seen = {e: {} for e in self.engs}
        self.lastw = {}
        self.reads = {}
        self.NS = 8
        self.dq = {}
        for q in ("sp", "pool"):
            sems = [self._alloc(f"dq_{q}_{i}") for i in range(self.NS)]
            self.dq[q] = {"sems": sems, "i": 0, "last": {}}

    def _alloc(self, name):
        self.nsem += 1
        return self.nc.alloc_semaphore(name)

    def _new_epoch(self, e):
        self.cur_sem[e] = self._alloc(f"c_{e}_{self.nsem}")
        self.cnt[e] = 0

    def _wait(self, e, ev):
        if ev is None:
            return
        sem, val, src = ev
        if src == "pe" and e == "pe":
            return
        k = id(sem)
        old = self.seen[e].get(k)
        if old is not None and old >= val:
            return
        self.seen[e][k] = val
        self.engs[e].wait_ge(sem, val)

    def _deps(self, e, reads, writes):
        for r in reads:
            self._wait(e, self.lastw.get(r))
        for w in writes:
            self._wait(e, self.lastw.get(w))
            for ev in self.reads.get(w, ()):
                self._wait(e, ev)

    def _commit(self, ev, reads, writes):
        for r in reads:
            lst = self.reads.setdefault(r, [])
            lst.append(ev)
            if len(lst) > 24:
                d = {}
                for s, v, src in lst:
                    if id(s) not in d or d[id(s)][1] < v:
                        d[id(s)] = (s, v, src)
                self.reads[r] = list(d.values())
        for w in writes:
            self.lastw[w] = ev
            self.reads[w] = []

    def op(self, e, fn, reads=(), writes=()):
        self._deps(e, reads, writes)
        if self.cnt[e] >= SEM_CAP:
            self._new_epoch(e)
        ins = fn(self.engs[e])
        self.cnt[e] += 1
        ins.then_inc(self.cur_sem[e], 1)
        ev = (self.cur_sem[e], self.cnt[e], e)
        self._commit(ev, reads, writes)
        return ev

    def dma(self, q, out, in_, reads=(), writes=(), **kw):
        d = self.dq[q]
        i = d["i"]
        d["i"] += 1
        slot = i % self.NS
        sem = d["sems"][slot]
        prev = 16 * (i // self.NS)
        if prev + 16 > 2 * SEM_CAP:
            d["sems"] = [self._alloc(f"dq_{q}_{i}_{k}") for k in range(self.NS)]
            d["i"] = 1
            i = 0
            slot = 0
            sem = d["sems"][0]
            prev = 0
        if prev > 0:
            self._wait(q, (sem, prev, "dma"))
        self._deps(q, reads, writes)
        ins = self.engs[q].dma_start(out=out, in_=in_, **kw)
        ins.then_inc(sem, 16)
        ev = (sem, prev + 16, "dma")
        self._commit(ev, reads, writes)
        d["last"][id(sem)] = ev
        return ev

    def dma_fn(self, q, fn, reads=(), writes=()):
        d = self.dq[q]
        i = d["i"]
        d["i"] += 1
        slot = i % self.NS
        sem = d["sems"][slot]
        prev = 16 * (i // self.NS)
        if prev + 16 > 2 * SEM_CAP:
            d["sems"] = [self._alloc(f"dq_{q}_{i}_{k}") for k in range(self.NS)]
            d["i"] = 1
            slot = 0
            sem = d["sems"][0]
            prev = 0
        if prev > 0:
            self._wait(q, (sem, prev, "dma"))
        self._deps(q, reads, writes)
        ins = fn(self.engs[q])
        ins.then_inc(sem, 16)
        ev = (sem, prev + 16, "dma")
        self._commit(ev, reads, writes)
        d["last"][id(sem)] = ev
        return ev

    def _all_events(self):
        evs = []
        for e in ("pe", "act", "dve", "pool"):
            if self.cnt[e] > 0:
                evs.append((self.cur_sem[e], self.cnt[e], "bar"))
        for q, d in self.dq.items():
            for ev in d["last"].values():
                evs.append((ev[0], ev[1], "bar"))
        return evs

    def barrier(self):
        evs = self._all_events()
        for e in self.engs:
            for ev in evs:
                self._wait(e, ev)
        self.lastw.clear()
        self.reads.clear()

    def finish(self, eng="sp"):
        for ev in self._all_events():
            self._wait(eng, ev)


def build_program():
    nc = bass.Bass("TRN2", target_bir_lowering=False)

    def din(name, shape, dt=F32):
        return nc.dram_tensor(name, list(shape), dt, kind="ExternalInput").ap()

    def dscr(name, shape, dt):
        return nc.dram_tensor(name, list(shape), dt, kind="Internal").ap()

    x_loc = din("x_loc", [8192, 1024])
    tmask_d = din("tmask", [128, 64])
    pos_d = din("pos_loc", [1, 8192], I32)
    c_b = din("c_b", [128, 8])
    ada_w = din("ada_w", [1024, 6144])
    ada_b = din("ada_b", [1, 6144])
    gains = din("gains", [1, 4096])
    w_in = din("w_in", [1024, 4096])
    cst_d = din("cst", [128, 16])
    ident_d = din("ident", [128, 128])
    iota_d = din("iota", [1, 1024])
    lre_d = din("lre", [128, 32])
    lim_d = din("lim", [128, 32])
    ldt_d = din("ldt", [1, 32])
    bl1_d = din("bl1", [128, 32 * 128])
    bl2_d = din("bl2", [128, 32 * 128])
    cr_d = din("cr", [128, 512])
    ci_d = din("ci", [128, 512])
    dsk_d = din("dsk", [128, 4])
    wglu_d = din("w_glu", [512, 512])
    wsb_d = din("w_sb", [512, 1024])
    wab_d = din("w_ab", [512, 1024])
    wout_d = din("w_out", [1024, 1024])
    bonehot_d = din("bonehot", [32, 8192], BF16)
    dm_d = din("dmask", [128, 2048], BF16)
    vb_d = din("vbias", [1, 8 * 96])
    vv_d = din("vvalid", [1, 8 * 96])
    vo_d = din("vown", [1, 8 * 96])
    rw_d = din("router_w", [1024, 32])
    rb_d = din("router_b", [1, 32])
    wg_d = din("w_gate", [32, 1024, 1024])
    wu_d = din("w_up", [32, 1024, 1024])
    wd_d = din("w_down", [32, 1024, 1024])
    bg_d = din("b_gate", [128, 32 * 8])
    bu_d = din("b_up", [128, 32 * 8])
    bd_d = din("b_down", [32, 1024])
    ltri_d = din("ltri", [128, 128], BF16)
    eoff_d = din("eoff", [1, 32])
    out_d = nc.dram_tensor("out", [2048, 1024], F32, kind="ExternalOutput").ap()
    CAP = 1024
    XD = dscr("XD", [32 * CAP, 1024], BF16)
    YD = dscr("YD", [32 * CAP, 1024], BF16)

    UT = dscr("UT", [512, 8192], BF16)
    KT = dscr("KT", [512, 8192], BF16)
    VV = dscr("VV", [8192, 512], BF16)
    QT = dscr("QT", [512, 2048], BF16)
    GST = dscr("GST", [1024, 2048], BF16)
    GAT = dscr("GAT", [1024, 2048], BF16)
    YST = dscr("YST", [512, 2048], BF16)
    OAT = dscr("OAT", [512, 2048], BF16)
    X1 = dscr("X1", [2048, 1024], F32)
    H2T = dscr("H2T", [1024, 2048], BF16)

    T = Tracker(nc)
    BCREG = nc.gpsimd.to_reg(32 * 1024 - 1)
    _cnt = [0]

    def _u(n):
        _cnt[0] += 1
        return f'sb{_cnt[0]}_{n}'
    op = T.op
    dma = T.dma
    uid = [0]

    def mm(out, lhsT, rhs, start, stop, reads, writes):
        return op("pe", lambda e: e.matmul(out, lhsT=lhsT, rhs=rhs, start=start, stop=stop),
                  reads=reads, writes=writes)

    def act(out, in_, func, reads, writes, eng="act", **kw):
        return op(eng, lambda e: e.activation(out=out, in_=in_, func=func, **kw), reads=reads, writes=writes)

    def tt(eng, out, a, b, o, reads, writes):
        return op(eng, lambda e: e.tensor_tensor(out=out, in0=a, in1=b, op=o), reads=reads, writes=writes)

    def ts(eng, out, a, s1, s2, o0, o1, reads, writes):
        if o1 is None:
            return op(eng, lambda e: e.tensor_scalar(out=out, in0=a, scalar1=s1, scalar2=None, op0=o0),
                      reads=reads, writes=writes)
        return op(eng, lambda e: e.tensor_scalar(out=out, in0=a, scalar1=s1, scalar2=s2, op0=o0, op1=o1),
                  reads=reads, writes=writes)

    def stt(eng, out, a, s, b, o0, o1, reads, writes):
        return op(eng, lambda e: e.scalar_tensor_tensor(out=out, in0=a, scalar=s, in1=b, op0=o0, op1=o1),
                  reads=reads, writes=writes)

    def cp(eng, out, a, reads, writes):
        return op(eng, lambda e: e.tensor_copy(out=out, in_=a), reads=reads, writes=writes)


    MAGIC = 12582912.0
    INV2PI = float(1.0 / (2 * np.pi))

    def reduce_angle(x, kx, k, kk, r, kr, ab, kab):
        ts("dve", k, x, INV2PI, MAGIC, ALU.mult, ALU.add, [kx], [kk])
        ts("dve", k, k, -MAGIC, None, ALU.add, None, [kk], [kk])
        stt("dve", r, k, -TWO_PI, x, ALU.mult, ALU.add, [kk, kx], [kr])
        ts("dve", r, r, PI_LO, -PI_LO, ALU.min, ALU.max, [kr], [kr])
        act(ab, r, AF.Abs, [kr], [kab])

    with ExitStack() as gs:
        def gsb(name, shape, dt=F32):
            return gs.enter_context(nc.sbuf_tensor(_u(name), shape, dt))
        G2 = gsb("G2", [128, 1024])
        ident = gsb("ident", [128, 128])
        identb = gsb("identb", [128, 128], BF16)
        CST = gsb("CST", [128, 16])
        tmask = gsb("tmaskt", [128, 64])
        RWT = gsb("RWT", [128, 16, 32])
        IDX = gsb("IDX", [128, 512], I32)
        aes = ExitStack()
        PRM = aes.enter_context(nc.sbuf_tensor(_u("PRM"), [128, 5, 1024], F32))
        dma("sp", ident[:], ident_d, writes=["ident"])
        dma("sp", CST[:], cst_d, writes=["CST"])
        dma("sp", tmask[:], tmask_d, writes=["tmask"])
        cp("dve", identb[:], ident[:], ["ident"], ["identb"])
        SGNR = CST[:, 1:2]
        HALFPI = CST[:, 2:3]
        INVF = CST[:, 3:4]
        MLO = CST[:, 4:5]
        NMHI = CST[:, 5:6]
        SHALF = CST[:, 6:7]
        NEG1 = CST[:, 7:8]
        NMLO = CST[:, 8:9]
        ONE9 = CST[:, 9:10]
        N2PI = CST[:, 10:11]
        SC1 = CST[:, 11:12]
        SHALF1 = CST[:, 12:13]
        MAGICC = CST[:, 13:14]

        with ExitStack() as es:
            def sb(name, shape, dt=F32):
                return es.enter_context(nc.sbuf_tensor(_u(name), shape, dt))
            ct = sb("ct", [128, 8]); cs = sb("cs", [128, 8])
            CB = sb("CB", [128, 8, 128], BF16)
            AWb = sb("AWb", [128, 8, 512], BF16)
            ADAB = sb("ADAB", [128, 6144]); ADA = sb("ADA", [128, 6144])
            GB = sb("GB", [128, 4096])
            pa = es.enter_context(nc.psum_tensor(_u("pa"), [128, 512], F32))
            dma("sp", ct[:], c_b, writes=["ct"])
            dma("pool", ADAB[:], ada_b.to_broadcast([128, 6144]), writes=["ADAB"])
            dma("pool", GB[:], gains.to_broadcast([128, 4096]), writes=["GB"])
            act(cs[:], ct[:], AF.Silu, ["ct"], ["cs"])
            for kc in range(8):
                cp("dve", CB[:, kc, :], cs[:, kc:kc + 1].to_broadcast([128, 128]), ["cs"], ["CB"])
            for n in range(12):
                dma("pool", AWb[:], ada_w[:, n * 512:(n + 1) * 512].rearrange("(k p) n -> p k n", p=128),
                    writes=["AWb"])
                for kc in range(8):
                    mm(pa[:], CB[:, kc, :], AWb[:, kc, :], kc == 0, kc == 7, ["CB", "AWb"], ["pa"])
                tt("dve", ADA[:, n * 512:(n + 1) * 512], pa[:], ADAB[:, n * 512:(n + 1) * 512], ALU.add,
                   ["pa", "ADAB"], ["ADA"])
            stt("dve", PRM[:, 0, :], ADA[:, 1024:2048], 1.0, GB[:, 0:1024], ALU.add, ALU.mult, ["ADA", "GB"], ["PRM"])
            cp("dve", PRM[:, 1, :], ADA[:, 0:1024], ["ADA"], ["PRM"])
            tt("dve", PRM[:, 2, :], ADA[:, 2048:3072], GB[:, 1024:2048], ALU.mult, ["ADA", "GB"], ["PRM"])
            stt("dve", PRM[:, 3, :], ADA[:, 4096:5120], 1.0, GB[:, 2048:3072], ALU.add, ALU.mult, ["ADA", "GB"], ["PRM"])
            cp("dve", PRM[:, 4, :], ADA[:, 3072:4096], ["ADA"], ["PRM"])
            tt("dve", G2[:], ADA[:, 5120:6144], GB[:, 3072:4096], ALU.mult, ["ADA", "GB"], ["G2"])
            T.barrier()

        def norm_mod(es_sb, xt, key_x, a_idx, sh_idx, maskcol, hb, key_hb, tmp, tmp2, ss, rs, sq):
            act(sq[:], xt, AF.Square, [key_x], ["sq", "ss"], accum_out=ss[:])
            ts("dve", rs[:], ss[:], 1.0 / 1024, 1e-6, ALU.mult, ALU.add, ["ss"], ["rs"])
            act(rs[:], rs[:], AF.Sqrt, ["rs"], ["rs"])
            op("dve", lambda e: e.reciprocal(out=rs[:], in_=rs[:]), reads=["rs"], writes=["rs"])
            stt("dve", tmp[:], xt, rs[:, 0:1], PRM[:, a_idx, :], ALU.mult, ALU.mult, [key_x, "rs", "PRM"], ["tmp"])
            tt("dve", tmp2[:], tmp[:], PRM[:, sh_idx, :], ALU.add, ["tmp", "PRM"], ["tmp2"])
            if maskcol is None:
                act(hb, tmp2[:], AF.Copy, ["tmp2"], [key_hb])
            else:
                act(hb, tmp2[:], AF.Copy, ["tmp2", "tmask"], [key_hb], scale=maskcol)

        with ExitStack() as es:
            def sb(name, shape, dt=F32):
                return es.enter_context(nc.sbuf_tensor(_u(name), shape, dt))
            def ps(name, shape, dt=F32):
                return es.enter_context(nc.psum_tensor(_u(name), shape, dt))
            WB = sb("WB", [128, 8, 5120], BF16)
            xt = sb("xt", [128, 1024]); tmp = sb("tmp", [128, 1024]); tmp2 = sb("tmp2", [128, 1024])
            sq = sb("sq", [128, 1024], BF16)
            ss = sb("ss", [128, 1]); rs = sb("rs", [128, 1])
            hb = sb("hb", [128, 1024], BF16)
            hT = sb("hT", [128, 8, 512], BF16)
            posi = sb("posi", [128, 512], I32); posf = sb("posf", [128, 512])
            a1 = sb("a1", [128, 512]); a2 = sb("a2", [128, 512])
            cosT = sb("cosT", [128, 512]); sinS = sb("sinS", [128, 512])
            m1 = sb("m1", [128, 512]); m2 = sb("m2", [128, 512])
            ob = [sb(f"ob{i}", [128, 512], BF16) for i in range(3)]
            pT = ps("pT", [128, 8, 128], BF16)
            pk = ps("pk", [128, 512]); pks = ps("pks", [128, 512])
            pm = [ps(f"pm{i}", [128, 512]) for i in range(2)]

            blocks = [(0, 0, False), (512, 1024, False), (1024, 1024, True), (1536, 1536, False),
                      (2048, 512, False), (2560, 512, True), (3072, 2048, False), (3584, 2560, False),
                      (4096, 3072, False), (4608, 3584, False)]
            for dst, src, swp in blocks:
                if not swp:
                    dma("pool", WB[:, :, dst:dst + 512], w_in[:, src:src + 512].rearrange("(k p) n -> p k n", p=128),
                        writes=["WB"])
                else:
                    srcv = w_in[:, src:src + 512].rearrange("(k p) (h t j) -> p k h t j", p=128, h=8, t=2, j=32)
                    dstv = WB[:, :, dst:dst + 512].rearrange("p k (h t j) -> p k h t j", h=8, t=2, j=32)
                    for kc in range(8):
                        dma("pool", dstv[:, kc, :, 0, :], srcv[:, kc, :, 1, :], writes=["WB"])
                        dma("pool", dstv[:, kc, :, 1, :], srcv[:, kc, :, 0, :], writes=["WB"])

            T.barrier()
            obi = [0]

            def emit(src_ps, key_ps, dst_ap, func=AF.Copy, **kw):
                o = ob[obi[0] % 3]
                k = f"ob{obi[0] % 3}"
                obi[0] += 1
                act(o[:], src_ps, func, [key_ps], [k], **kw)
                dma("sp", dst_ap, o[:], reads=[k])

            for c in range(16):
                own = c >= 12
                for j in range(4):
                    t = 4 * c + j
                    dma("sp", xt[:], x_loc[t * 128:(t + 1) * 128, :], writes=["xt"])
                    norm_mod(sb, xt[:], "xt", 0, 1, tmask[:, t:t + 1], hb[:], "hb", tmp, tmp2, ss, rs, sq)
                    for kc in range(8):
                        op("pe", lambda e: e.transpose(out=pT[:, kc, :], in_=hb[:, kc * 128:(kc + 1) * 128],
                                                       identity=identb[:]),
                           reads=["hb", "identb"], writes=["pT"])
                    cp("dve", hT[:, :, j * 128:(j + 1) * 128], pT[:], ["pT"], ["hT"])
                dma("pool", posi[:], pos_d[0:1, c * 512:(c + 1) * 512].to_broadcast([128, 512]), writes=["posi"])
                cp("dve", posf[:], posi[:], ["posi"], ["posf"])
                ts("dve", a1[:], posf[:], INVF, None, ALU.mult, None, ["posf", "CST"], ["a1"])
                reduce_angle(a1[:], "a1", a2[:], "a2", m1[:], "m1", m2[:], "m2")
                act(sinS[:], m1[:], AF.Sin, ["m1", "CST"], ["sinS"], scale=SGNR)
                act(cosT[:], m2[:], AF.Sin, ["m2", "CST"], ["cosT"], scale=NEG1, bias=HALFPI)

                def rope_proj(c0, c0s, dst, scale):
                    for cb in range(4):
                        for kc in range(8):
                            mm(pk[:], WB[:, kc, c0 + cb * 128:c0 + (cb + 1) * 128], hT[:, kc, :], kc == 0, kc == 7,
                               ["WB", "hT"], ["pk"])
                        for kc in range(8):
                            mm(pks[:], WB[:, kc, c0s + cb * 128:c0s + (cb + 1) * 128], hT[:, kc, :], kc == 0, kc == 7,
                               ["WB", "hT"], ["pks"])
                        tt("dve", m1[:], pk[:], cosT[:], ALU.mult, ["pk", "cosT"], ["m1"])
                        tt("dve", m2[:], pks[:], sinS[:], ALU.mult, ["pks", "sinS"], ["m2"])
                        tt("dve", m1[:], m1[:], m2[:], ALU.add, ["m1", "m2"], ["m1"])
                        emit(m1[:], "m1", dst(cb), scale=scale)

                rope_proj(512, 1024, lambda cb: KT[cb * 128:(cb + 1) * 128, c * 512:(c + 1) * 512], 1.0)
                for cb in range(4):
                    p = pm[cb % 2]; kp = f"pm{cb % 2}"
                    for kc in range(8):
                        mm(p[:], WB[:, kc, cb * 128:(cb + 1) * 128], hT[:, kc, :], kc == 0, kc == 7, ["WB", "hT"], [kp])
                    emit(p[:], kp, UT[cb * 128:(cb + 1) * 128, c * 512:(c + 1) * 512])
                for j in range(4):
                    p = pm[j % 2]; kp = f"pm{j % 2}"
                    for kc in range(8):
                        mm(p[:], hT[:, kc, j * 128:(j + 1) * 128], WB[:, kc, 1536:2048], kc == 0, kc == 7, ["WB", "hT"], [kp])
                    emit(p[:], kp, VV[(4 * c + j) * 128:(4 * c + j + 1) * 128, :])
                if own:
                    co = c - 12
                    rope_proj(2048, 2560, lambda cb: QT[cb * 128:(cb + 1) * 128, co * 512:(co + 1) * 512], 0.125)
                    for gi, (c0, dstT) in enumerate(((3072, GST), (4096, GAT))):
                        for db in range(8):
                            p = pm[db % 2]; kp = f"pm{db % 2}"
                            for kc in range(8):
                                mm(p[:], WB[:, kc, c0 + db * 128:c0 + (db + 1) * 128], hT[:, kc, :], kc == 0, kc == 7,
                                   ["WB", "hT"], [kp])
                            emit(p[:], kp, dstT[db * 128:(db + 1) * 128, co * 512:(co + 1) * 512], func=AF.Sigmoid)
            T.barrier()

        SEG = 1024
        NSEG = 8192 // SEG
        with ExitStack() as es:
            def sb(name, shape, dt=F32):
                return es.enter_context(nc.sbuf_tensor(_u(name), shape, dt))
            Y = sb("Y", [128, 4, 2048])
            LA = sb("LA", [128, 32, 128], BF16); LB = sb("LB", [128, 32, 128], BF16)
            BL1b = sb("BL1b", [128, 32, 128], BF16); BL2b = sb("BL2b", [128, 32, 128], BF16)
            TH = sb("TH", [128, 32]); RHO = sb("RHO", [128, 32]); CAR = sb("CAR", [128, 32])
            PHf = sb("PHf", [128, 32]); OFFT = sb("OFFT", [128, 8, 32]); NOFF = sb("NOFF", [128, 8, 32])
            SB1 = sb("SB1", [128, 8, 32]); SB2 = sb("SB2", [128, 8, 32])
            DSK = sb("DSK", [128, 4])
            dma("sp", DSK[:], dsk_d, writes=["DSK"])
            with ExitStack() as e2:
                def sb2(name, shape, dt=F32):
                    return e2.enter_context(nc.sbuf_tensor(_u(name), shape, dt))
                LRE = sb2("LRE", [128, 32]); LIM = sb2("LIM", [128, 32]); LDT = sb2("LDT", [128, 32])
                CR = sb2("CR", [128, 32, 16]); CI = sb2("CI", [128, 32, 16])
                w = [sb2(f"w{i}", [128, 32]) for i in range(10)]
                c1 = sb2("c1", [128, 32, 16]); c2 = sb2("c2", [128, 32, 16])
                cpr = sb2("cpr", [128, 32, 16]); cpi = sb2("cpi", [128, 32, 16])
                for src, dstb, kb in ((bl1_d, BL1b, "BL1b"), (bl2_d, BL2b, "BL2b")):
                    dma("pool", dstb[:].rearrange("p g m -> p (g m)"), src, writes=[kb])
                dma("sp", LRE[:], lre_d, writes=["LRE"])
                dma("sp", LIM[:], lim_d, writes=["LIM"])
                dma("pool", LDT[:], ldt_d.to_broadcast([128, 32]), writes=["LDT"])
                dma("sp", CR[:].rearrange("p g c -> p (g c)"), cr_d, writes=["CR"])
                dma("sp", CI[:].rearrange("p g c -> p (g c)"), ci_d, writes=["CI"])
                K = "tb"
                dt_ = w[0]
                act(dt_[:], LDT[:], AF.Exp, ["LDT"], [K])
                tt("dve", TH[:], LIM[:], dt_[:], ALU.mult, ["LIM", K], ["TH"])
                tt("dve", w[1][:], LRE[:], dt_[:], ALU.mult, ["LRE", K], [K])
                act(RHO[:], w[1][:], AF.Exp, [K], ["RHO"])
                reduce_angle(TH[:], "TH", w[2][:], K, w[3][:], K, w[9][:], K)
                act(w[4][:], w[3][:], AF.Sin, [K, "CST"], [K], scale=ONE9)
                act(w[5][:], w[9][:], AF.Sin, [K, "CST"], [K], scale=NEG1, bias=HALFPI)
                tt("dve", w[6][:], RHO[:], w[5][:], ALU.mult, ["RHO", K], [K])
                ts("dve", w[6][:], w[6][:], -1.0, None, ALU.add, None, [K], [K])
                tt("dve", w[7][:], RHO[:], w[4][:], ALU.mult, ["RHO", K], [K])
                tt("dve", w[8][:], LRE[:], LRE[:], ALU.mult, ["LRE"], [K])
                tt("dve", w[9][:], LIM[:], LIM[:], ALU.mult, ["LIM"], [K])
                tt("dve", w[8][:], w[8][:], w[9][:], ALU.add, [K], [K])
                op("dve", lambda e: e.reciprocal(out=w[8][:], in_=w[8][:]), reads=[K], writes=[K])
                tt("dve", w[0][:], w[6][:], LRE[:], ALU.mult, [K, "LRE"], [K])
                tt("dve", w[1][:], w[7][:], LIM[:], ALU.mult, [K, "LIM"], [K])
                tt("dve", w[0][:], w[0][:], w[1][:], ALU.add, [K], [K])
                tt("dve", w[2][:], w[0][:], w[8][:], ALU.mult, [K], [K])
                tt("dve", w[0][:], w[7][:], LRE[:], ALU.mult, [K, "LRE"], [K])
                tt("dve", w[1][:], w[6][:], LIM[:], ALU.mult, [K, "LIM"], [K])
                tt("dve", w[0][:], w[0][:], w[1][:], ALU.subtract, [K], [K])
                tt("dve", w[3][:], w[0][:], w[8][:], ALU.mult, [K], [K])
                qrb = w[2][:, :].unsqueeze(2).to_broadcast([128, 32, 16])
                qib = w[3][:, :].unsqueeze(2).to_broadcast([128, 32, 16])
                tt("dve", c1[:], CR[:], qrb, ALU.mult, ["CR", K], [K])
                tt("dve", c2[:], CI[:], qib, ALU.mult, ["CI", K], [K])
                tt("dve", cpr[:], c1[:], c2[:], ALU.subtract, [K], [K])
                tt("dve", c1[:], CR[:], qib, ALU.mult, ["CR", K], [K])
                tt("dve", c2[:], CI[:], qrb, ALU.mult, ["CI", K], [K])
                tt("dve", cpi[:], c1[:], c2[:], ALU.add, [K], [K])
                ts("dve", c1[:], cpr[:], MLO, None, ALU.mult, None, [K, "CST"], [K])
                stt("dve", c1[:], cpi[:], NMHI, c1[:], ALU.mult, ALU.add, [K, "CST"], [K])
                ts("dve", c2[:], cpi[:], NMLO, None, ALU.mult, None, [K, "CST"], [K])
                stt("dve", c2[:], cpr[:], NMHI, c2[:], ALU.mult, ALU.add, [K, "CST"], [K])
                op("pool", lambda e: e.memset(LA[:], 0.0), writes=["LA"])
                op("pool", lambda e: e.memset(LB[:], 0.0), writes=["LB"])
                for src, dst, kd in ((c1, LA, "LA"), (c2, LB, "LB")):
                    dv = dst[:].rearrange("p (a r) (s c) -> p a r s c", r=8, s=8, c=16)
                    sv = src[:].rearrange("p (a r) c -> p a r c", r=8)
                    for r in range(8):
                        cp("dve", dv[:, :, r, r, :], sv[:, :, r, :], [K], [kd])
                op("dve", lambda e: e.memset(CAR[:], 0.0), writes=["CAR"])
                ts("dve", w[0][:], TH[:], INV2PI, MAGIC, ALU.mult, ALU.add, ["TH"], [K])
                ts("dve", w[0][:], w[0][:], -MAGIC, None, ALU.add, None, [K], [K])
                stt("dve", PHf[:], TH[:], INV2PI, w[0][:], ALU.mult, ALU.subtract, ["TH", K], ["PHf"])
                for sg in range(8):
                    ts("dve", w[1][:], PHf[:], float(sg * 1024), None, ALU.mult, None, ["PHf"], [K])
                    ts("dve", w[2][:], w[1][:], MAGIC, None, ALU.add, None, [K], [K])
                    stt("dve", OFFT[:, sg, :], w[2][:], -MAGIC, w[1][:], ALU.add, ALU.subtract, [K], ["OFFT"])
                    ts("dve", OFFT[:, sg, :], OFFT[:, sg, :], -1.0, None, ALU.mult, None, ["OFFT"], ["OFFT"])
                ts("dve", NOFF[:].rearrange("p a g -> p (a g)"), OFFT[:].rearrange("p a g -> p (a g)"), -1.0, None,
                   ALU.mult, None, ["OFFT"], ["NOFF"])
                ts("dve", SB2[:].rearrange("p a g -> p (a g)"), OFFT[:].rearrange("p a g -> p (a g)"),
                   TWO_PI, None, ALU.mult, None, ["OFFT"], ["SB2"])
                ts("dve", SB1[:].rearrange("p a g -> p (a g)"), SB2[:].rearrange("p a g -> p (a g)"), SHALF1, None,
                   ALU.mult, None, ["SB2", "CST"], ["SB1"])
                T.barrier()

            with ExitStack() as e2:
                def sb2(name, shape, dt=F32):
                    return e2.enter_context(nc.sbuf_tensor(_u(name), shape, dt))
                def ps2(name, shape, dt=F32):
                    return e2.enter_context(nc.psum_tensor(_u(name), shape, dt))
                UTb = sb2("UTb", [128, 8192], BF16)
                IOT = sb2("IOT", [128, SEG])
                UB = sb2("UB", [128, SEG]); U_ = sb2("U_", [128, SEG]); KK = sb2("KK", [128, SEG]); NF = sb2("NF", [128, SEG]); AB = sb2("AB", [128, SEG])
                COS2 = sb2("COS2", [128, SEG]); SINS = sb2("SINS", [128, SEG]); SIN2 = sb2("SIN2", [128, SEG])
                m2 = sb2("sm2", [128, 512])
                W = sb2("W", [128, SEG]); RB = sb2("RB", [128, SEG]); Z = sb2("Z", [128, SEG])
                A1b = sb2("A1b", [128, SEG], BF16); A2b = sb2("A2b", [128, SEG], BF16)
                py = [ps2(f"py{i}", [128, 512]) for i in range(4)]
                p1 = [ps2(f"p1{i}", [128, 512]) for i in range(2)]
                p2 = [ps2(f"p2{i}", [128, 512]) for i in range(2)]
                dma("pool", IOT[:], iota_d.to_broadcast([128, SEG]), writes=["IOT"])
                ZT = sb2("ZT", [128, 4096], BF16)
                op("pool", lambda e: e.memset(ZT[:], 0.0), writes=["ZT"])
                xdv = XD.rearrange("(p i) d -> p (i d)", p=128)
                pi_ = 0
                for cbk in range(4):
                    dma("sp", UTb[:], UT[cbk * 128:(cbk + 1) * 128, :], writes=["UTb"])
                    if cbk == 0:
                        for zi in range(64):
                            dma("sp", xdv[:, zi * 4096:(zi + 1) * 4096], ZT[:], reads=["ZT"], writes=[f"XDz{zi}"])
                    for gg in range(8):
                        g = cbk * 8 + gg
                        base = min((gg // 2) * 32, 64)
                        kr = 32 if gg // 2 < 3 else 64
                        cp("dve", RB[:], RHO[:, g:g + 1].to_broadcast([128, SEG]), ["RHO"], ["RB"])
                        ts("dve", UB[:], IOT[:], PHf[:, g:g + 1], None, ALU.mult, None, ["IOT", "PHf"], ["UB"])
                        for seg in range(NSEG):
                            ownseg = seg >= NSEG - 2048 // SEG
                            act(U_[:], UB[:], AF.Identity, ["UB", "OFFT"], ["U_"], bias=OFFT[:, seg, g:g + 1])
                            ts("dve", KK[:], U_[:], MAGIC, None, ALU.add, None, ["U_"], ["KK"])
                            stt("dve", NF[:], KK[:], -MAGIC, U_[:], ALU.add, ALU.subtract, ["KK", "U_"], ["NF"])
                            act(AB[:], NF[:], AF.Abs, ["NF"], ["AB"])
                            act(COS2[:], AB[:], AF.Sin, ["AB", "CST"], ["COS2"], scale=N2PI, bias=HALFPI)
                            act(SINS[:], NF[:], AF.Sin, ["NF", "CST"], ["SINS"], scale=SC1)
                            if ownseg:
                                act(SIN2[:], NF[:], AF.Sin, ["NF", "CST"], ["SIN2"], scale=N2PI)
                            for ch in range(SEG // 512):
                                tok0 = seg * SEG + ch * 512
                                q1 = p1[pi_ % 2]; q2 = p2[pi_ % 2]; k1 = f"p1{pi_ % 2}"; k2 = f"p2{pi_ % 2}"
                                pi_ += 1
                                mm(q1[:], BL1b[base:base + kr, g, :], UTb[base:base + kr, tok0:tok0 + 512], True, True,
                                   ["BL1b", "UTb"], [k1])
                                mm(q2[:], BL2b[base:base + kr, g, :], UTb[base:base + kr, tok0:tok0 + 512], True, True,
                                   ["BL2b", "UTb"], [k2])
                                wv = W[:, ch * 512:(ch + 1) * 512]
                                tt("dve", wv, q1[:], COS2[:, ch * 512:(ch + 1) * 512], ALU.mult, [k1, "COS2"], ["W"])
                                tt("dve", m2[:], q2[:], SINS[:, ch * 512:(ch + 1) * 512], ALU.mult, [k2, "SINS"], ["m2"])
                                tt("dve", wv, wv, m2[:], ALU.add, ["W", "m2"], ["W"])
                            op("dve", lambda e: e.tensor_tensor_scan(out=Z[:], data0=RB[:], data1=W[:],
                                                                      initial=CAR[:, g:g + 1], op0=ALU.mult, op1=ALU.add),
                               reads=["RB", "W", "CAR"], writes=["Z"])
                            cp("dve", CAR[:, g:g + 1], Z[:, SEG - 1:SEG], ["Z"], ["CAR"])
                            if ownseg:
                                tt("dve", A1b[:], Z[:], COS2[:], ALU.mult, ["Z", "COS2"], ["A1b"])
                                tt("dve", A2b[:], Z[:], SIN2[:], ALU.mult, ["Z", "SIN2"], ["A2b"])
                                so = seg - (NSEG - 2048 // SEG)
                                for ch in range(SEG // 512):
                                    yi = so * (SEG // 512) + ch
                                    mm(py[yi][:], LA[:, g, :], A1b[:, ch * 512:(ch + 1) * 512], gg == 0, False,
                                       ["LA", "A1b"], [f"py{yi}"])
                                    mm(py[yi][:], LB[:, g, :], A2b[:, ch * 512:(ch + 1) * 512], False, gg == 7,
                                       ["LB", "A2b"], [f"py{yi}"])
                    for yi in range(4):
                        stt("dve", Y[:, cbk, yi * 512:(yi + 1) * 512], UTb[:, 6144 + yi * 512:6144 + (yi + 1) * 512],
                            DSK[:, cbk:cbk + 1], py[yi][:], ALU.mult, ALU.add, ["UTb", "DSK", f"py{yi}"], ["Y"])
                T.barrier()

            with ExitStack() as e2:
                def sb2(name, shape, dt=F32):
                    return e2.enter_context(nc.sbuf_tensor(_u(name), shape, dt))
                def ps2(name, shape, dt=F32):
                    return e2.enter_context(nc.psum_tensor(_u(name), shape, dt))
                WGb = sb2("WGb", [128, 4, 512], BF16)
                t1 = sb2("gt1", [128, 2048]); t2 = sb2("gt2", [128, 2048])
                YGb = sb2("YGb", [128, 4, 2048], BF16)
                sgl = sb2("sgl", [128, 512]); ysb = [sb2(f"ysb{i}", [128, 512], BF16) for i in range(2)]
                pg = [ps2(f"pgl{i}", [128, 512]) for i in range(2)]
                dma("pool", WGb[:], wglu_d.rearrange("(k p) n -> p k n", p=128), writes=["WGb"])
                for cbk in range(4):
                    yv = Y[:, cbk, :]
                    act(t1[:], yv, AF.Square, ["Y"], ["t1"])
                    ts("dve", t1[:], t1[:], 0.044715, 1.0, ALU.mult, ALU.add, ["t1"], ["t1"])
                    tt("dve", t1[:], t1[:], yv, ALU.mult, ["t1", "Y"], ["t1"])
                    act(t2[:], t1[:], AF.Sigmoid, ["t1"], ["t2"], scale=1.5957691216057308)
                    tt("dve", yv, yv, t2[:], ALU.mult, ["Y", "t2"], ["Y"])
                    act(YGb[:, cbk, :], yv, AF.Copy, ["Y"], ["YGb"])
                i = 0
                for cbo in range(4):
                    for ch in range(4):
                        p = pg[i % 2]; kp = f"pgl{i % 2}"; o = ysb[i % 2]; ko = f"ysb{i % 2}"
                        i += 1
                        for kc in range(4):
                            mm(p[:], WGb[:, kc, cbo * 128:(cbo + 1) * 128], YGb[:, kc, ch * 512:(ch + 1) * 512],
                               kc == 0, kc == 3, ["WGb", "YGb"], [kp])
                        act(sgl[:], p[:], AF.Sigmoid, [kp], ["sgl"])
                        tt("dve", o[:], Y[:, cbo, ch * 512:(ch + 1) * 512], sgl[:], ALU.mult, ["Y", "sgl"], [ko])
                        dma("sp", YST[cbo * 128:(cbo + 1) * 128, ch * 512:(ch + 1) * 512], o[:], reads=[ko])
                T.barrier()

        with ExitStack() as es:
            def sb(name, shape, dt=F32):
                return es.enter_context(nc.sbuf_tensor(_u(name), shape, dt))
            def ps(name, shape, dt=F32):
                return es.enter_context(nc.psum_tensor(_u(name), shape, dt))
            KA = sb("KA", [128, 8192], BF16)
            VA = sb("VA", [128, 64, 65], BF16)
            QA = sb("QA", [128, 2048], BF16)
            DM = sb("DM", [128, 4, 512], BF16)
            VB = sb("VB", [128, 8, 96]); VVd = sb("VVd", [128, 8, 96]); VO = sb("VO", [128, 8, 96])
            KM = sb("KM", [64, 32]); KMb = sb("KMb", [64, 32], BF16)
            gt_ = sb("gt", [128, 96]); top8 = sb("top8", [128, 8]); sel = sb("sel", [128, 96])
            pt = [sb(f"pt{i}", [128, 512], BF16) for i in range(3)]
            rrow = sb("rrow", [128, 512]); rhi = sb("rhi", [128, 512], BF16); rlo = sb("rlo", [128, 512], BF16)
            rtmp = sb("rtmp", [128, 512])
            onesb = sb("onesb", [128, 64], BF16)
            bc = sb("bc", [64, 512]); oab = sb("oab", [64, 512], BF16)
            pS = [ps(f"pS{i}", [128, 512]) for i in range(3)]
            pO = [ps(f"pO{i}", [128, 512]) for i in range(2)]
            pG = ps("pG", [128, 96]); pB = ps("pB", [128, 128]); pO2 = ps("pO2", [64, 512])
            dma("sp", KA[64:96, :], bonehot_d, writes=["KAoh"])
            dma("sp", DM[:].rearrange("p a n -> p (a n)"), dm_d, writes=["DM"])
            dma("pool", VB[:].rearrange("p a n -> p (a n)"), vb_d.to_broadcast([128, 768]), writes=["VB"])
            dma("pool", VVd[:].rearrange("p a n -> p (a n)"), vv_d.to_broadcast([128, 768]), writes=["VVd"])
            dma("pool", VO[:].rearrange("p a n -> p (a n)"), vo_d.to_broadcast([128, 768]), writes=["VO"])
            op("pool", lambda e: e.memset(VA[:, :, 64:65], 1.0), writes=["VA1"])
            op("pool", lambda e: e.memset(onesb[:], 1.0), writes=["onesb"])
            op("pool", lambda e: e.memset(gt_[:], 0.0), writes=["gt"])
            itb = [0]
            for h in range(8):
                dma("sp", KA[0:64, :], KT[h * 64:(h + 1) * 64, :], writes=["KA"])
                dma("sp", QA[0:64, :], QT[h * 64:(h + 1) * 64, :], writes=["QA"])
                dma("pool", VA[:, :, 0:64], VV[:, h * 64:(h + 1) * 64].rearrange("(t p) d -> p t d", p=128),
                    writes=["VA"])
                op("dve", lambda e: e.tensor_reduce(out=KM[:], in_=KA[0:64, :].rearrange("p (n l) -> p n l", l=256),
                                                    axis=AX.X, op=ALU.add), reads=["KA"], writes=["KM"])
                cp("dve", KMb[:], KM[:], ["KM"], ["KMb"])
                for qt in range(16):
                    qb = qt // 2
                    mm(pG[:, 64:96], QA[0:64, qt * 128:(qt + 1) * 128], KMb[:], True, True, ["QA", "KMb"], ["pG"])
                    tt("dve", gt_[:, 64:96], pG[:, 64:96], VB[:, qb, 64:96], ALU.add, ["pG", "VB"], ["gt"])
                    op("dve", lambda e: e.max(out=top8[:], in_=gt_[:, 64:96]), reads=["gt"], writes=["top8"])
                    ts("dve", sel[:, 64:96], gt_[:, 64:96], top8[:, 2:3], None, ALU.is_ge, None, ["gt", "top8"], ["sel"])
                    tt("dve", sel[:, 64:96], sel[:, 64:96], VVd[:, qb, 64:96], ALU.mult, ["sel", "VVd"], ["sel"])
                    tt("dve", sel[:, 64:96], sel[:, 64:96], VO[:, qb, 64:96], ALU.add, ["sel", "VO"], ["sel"])
                    ts("dve", gt_[:, 64:96], sel[:, 64:96], -1.0, -NEG, ALU.add, ALU.mult, ["sel"], ["gt"])
                    op("pe", lambda e: e.transpose(out=pB[0:96, :], in_=gt_[:, 0:96], identity=ident[:]),
                       reads=["gt", "ident"], writes=["pB"])
                    cp("dve", QA[64:96, qt * 128:(qt + 1) * 128], pB[64:96, :], ["pB"], ["QAb"])
                for G in range(4):
                    cnt = 52 + 4 * G
                    po = pO[G % 2]; kpo = f"pO{G % 2}"

                    def qk(kt):
                        i3 = (itb[0] + kt) % 3
                        mm(pS[i3][:], KA[0:96, kt * 128:(kt + 1) * 128], QA[0:96, G * 512:(G + 1) * 512], True, True,
                           ["KA", "KAoh", "QA", "QAb"], [f"pS{i3}"])
                    qk(0)
                    qk(1)
                    for kt in range(cnt):
                        i3 = (itb[0] + kt) % 3
                        s_ = pS[i3]; ks = f"pS{i3}"; p_ = pt[i3]; kp = f"pt{i3}"
                        if kt + 2 < cnt:
                            qk(kt + 2)
                        act(p_[:], s_[:], AF.Exp, [ks], [kp])
                        di = kt - (cnt - 4)
                        if di >= 0:
                            tt("dve", p_[:], p_[:], DM[:, di, :], ALU.mult, [kp, "DM"], [kp])
                        mm(po[0:65, :], VA[:, kt, 0:65], p_[:], kt == 0, kt == cnt - 1, ["VA", "VA1", kp], [kpo])
                    itb[0] += cnt
                    cp("dve", rrow[64:65, :], po[64:65, :], [kpo], ["rrow"])
                    op("dve", lambda e: e.reciprocal(out=rrow[64:65, :], in_=rrow[64:65, :]), reads=["rrow"], writes=["rrow"])
                    cp("dve", rhi[64:65, :], rrow[64:65, :], ["rrow"], ["rhi"])
                    cp("dve", rtmp[64:65, :], rhi[64:65, :], ["rhi"], ["rtmp"])
                    tt("dve", rtmp[64:65, :], rrow[64:65, :], rtmp[64:65, :], ALU.subtract, ["rrow", "rtmp"], ["rtmp"])
                    cp("dve", rlo[64:65, :], rtmp[64:65, :], ["rtmp"], ["rlo"])
                    mm(pO2[:], onesb[64:65, 0:64], rhi[64:65, :], True, False, ["onesb", "rhi"], ["pO2"])
                    mm(pO2[:], onesb[64:65, 0:64], rlo[64:65, :], False, True, ["onesb", "rlo"], ["pO2"])
                    cp("dve", bc[:], pO2[:], ["pO2"], ["bc"])
                    tt("dve", oab[:], po[0:64, :], bc[:], ALU.mult, [kpo, "bc"], ["oab"])
                    dma("sp", OAT[h * 64:(h + 1) * 64, G * 512:(G + 1) * 512], oab[:], reads=["oab"])
            T.barrier()

        with ExitStack() as es:
            def sb(name, shape, dt=F32):
                return es.enter_context(nc.sbuf_tensor(_u(name), shape, dt))
            def ps(name, shape, dt=F32):
                return es.enter_context(nc.psum_tensor(_u(name), shape, dt))
            Wsb = sb("Wsb", [128, 4, 1024], BF16); Wab = sb("Wab", [128, 4, 1024], BF16)
            Wo = sb("Wo", [128, 8, 1024], BF16)
            ys = sb("ys", [128, 4, 512], BF16); oa = sb("oa", [128, 4, 512], BF16)
            gsT = sb("gsT", [128, 8, 512], BF16); gaT = sb("gaT", [128, 8, 512], BF16)
            MT = sb("MT", [128, 8, 512], BF16)
            b1 = sb("b1", [128, 512]); b2 = sb("b2", [128, 512])
            xt = sb("xt2", [128, 1024]); x1 = sb("x1", [128, 1024]); tmp = sb("tmpE", [128, 1024]); tmp2 = sb("tmp2E", [128, 1024])
            sq = sb("sqE", [128, 1024], BF16); ss = sb("ssE", [128, 1]); ss2 = sb("ss2E", [128, 1]); rs = sb("rsE", [128, 1])
            hbs = [sb(f"hbE{i}", [128, 1024], BF16) for i in range(2)]; h2T = sb("h2T", [128, 8, 512], BF16)
            RWb = sb("RWb", [128, 8, 32], BF16); RBb = sb("RBb", [128, 32]); LT = sb("LT", [128, 128], BF16)
            ONESb = sb("ONESb", [128, 128], BF16); CNT = sb("CNT", [128, 32]); EOFF = sb("EOFF", [128, 32])
            mb = sb("mb", [128, 32], BF16); posf = sb("posfE", [128, 32]); idxf = sb("idxf", [128, 32])
            lg = sb("lg", [128, 32]); top8 = sb("top8F", [128, 8]); msk = sb("msk", [128, 32]); ex = sb("ex", [128, 32])
            nmx = sb("nmx", [128, 1]); sm = sb("sm", [128, 1])
            pl = ps("pl", [128, 32]); pp = ps("pp", [128, 32]); pc = ps("pc", [128, 32])
            dma("pool", RWb[:], rw_d.rearrange("(k p) n -> p k n", p=128), writes=["RWb"])
            dma("pool", RBb[:], rb_d.to_broadcast([128, 32]), writes=["RBb"])
            dma("pool", EOFF[:], eoff_d.to_broadcast([128, 32]), writes=["EOFF"])
            dma("sp", LT[:], ltri_d, writes=["LT"])
            op("pool", lambda e: e.memset(ONESb[:], 1.0), writes=["ONESb"])
            op("pool", lambda e: e.memset(CNT[:], 0.0), writes=["CNT"])
            pb1 = ps("pb1", [128, 512]); pb2 = ps("pb2", [128, 512])
            pmx = ps("pmx", [128, 1024]); pT = ps("pTE", [128, 8, 128], BF16)
            for (src, dst, kd, nk) in ((wsb_d, Wsb, "Wsb", 4), (wab_d, Wab, "Wab", 4), (wout_d, Wo, "Wo", 8)):
                dma("pool", dst[:], src.rearrange("(k p) n -> p k n", p=128), writes=[kd])
            for c in range(4):
                cs_ = slice(c * 512, (c + 1) * 512)
                dma("sp", ys[:], YST[:, cs_].rearrange("(k p) n -> p k n", p=128), writes=["ys"])
                dma("sp", oa[:], OAT[:, cs_].rearrange("(k p) n -> p k n", p=128), writes=["oa"])
                dma("pool", gsT[:], GST[:, cs_].rearrange("(k p) n -> p k n", p=128), writes=["gsT"])
                dma("pool", gaT[:], GAT[:, cs_].rearrange("(k p) n -> p k n", p=128), writes=["gaT"])
                for db in range(8):
                    for kc in range(4):
                        mm(pb1[:], Wsb[:, kc, db * 128:(db + 1) * 128], ys[:, kc, :], kc == 0, kc == 3, ["Wsb", "ys"], ["pb1"])
                    for kc in range(4):
                        mm(pb2[:], Wab[:, kc, db * 128:(db + 1) * 128], oa[:, kc, :], kc == 0, kc == 3, ["Wab", "oa"], ["pb2"])
                    tt("dve", b1[:], pb1[:], gsT[:, db, :], ALU.mult, ["pb1", "gsT"], ["b1"])
                    tt("dve", b2[:], pb2[:], gaT[:, db, :], ALU.mult, ["pb2", "gaT"], ["b2"])
                    tt("dve", MT[:, db, :], b1[:], b2[:], ALU.add, ["b1", "b2"], ["MT"])
                for j in range(4):
                    t = 4 * c + j
                    for half in range(2):
                        for kc in range(8):
                            mm(pmx[:, half * 512:(half + 1) * 512], MT[:, kc, j * 128:(j + 1) * 128],
                               Wo[:, kc, half * 512:(half + 1) * 512], kc == 0, kc == 7, ["MT", "Wo"], ["pmx"])
                    dma("sp", xt[:], x_loc[6144 + t * 128:6144 + (t + 1) * 128, :], writes=["xt"])
                    act(sq[:, 0:512], pmx[:, 0:512], AF.Square, ["pmx"], ["sq", "ss"], accum_out=ss[:])
                    act(sq[:, 512:1024], pmx[:, 512:1024], AF.Square, ["pmx"], ["sq", "ss2"], accum_out=ss2[:])
                    tt("dve", ss[:], ss[:], ss2[:], ALU.add, ["ss", "ss2"], ["ss"])
                    ts("dve", rs[:], ss[:], 1.0 / 1024, 1e-6, ALU.mult, ALU.add, ["ss"], ["rs"])
                    act(rs[:], rs[:], AF.Sqrt, ["rs"], ["rs"])
                    op("dve", lambda e: e.reciprocal(out=rs[:], in_=rs[:]), reads=["rs"], writes=["rs"])
                    cp("dve", tmp[:], pmx[:], ["pmx"], ["tmp"])
                    stt("dve", tmp[:], tmp[:], rs[:, 0:1], PRM[:, 2, :], ALU.mult, ALU.mult, ["tmp", "rs", "PRM"], ["tmp"])
                    tt("dve", x1[:], tmp[:], xt[:], ALU.add, ["tmp", "xt"], ["x1"])
                    dma("sp", X1[t * 128:(t + 1) * 128, :], x1[:], reads=["x1"])
                    hb = hbs[t % 2]; khb = f"hb{t % 2}"
                    norm_mod(sb, x1[:], "x1", 3, 4, None, hb[:], khb, tmp, tmp2, ss, rs, sq)
                    for kc in range(8):
                        op("pe", lambda e: e.transpose(out=pT[:, kc, :], in_=hb[:, kc * 128:(kc + 1) * 128],
                                                       identity=identb[:]),
                           reads=[khb, "identb"], writes=["pT"])
                    cp("dve", h2T[:, :, j * 128:(j + 1) * 128], pT[:], ["pT"], ["h2T"])
                    for kc in range(8):
                        mm(pl[:], h2T[:, kc, j * 128:(j + 1) * 128], RWb[:, kc, :], kc == 0, kc == 7, ["h2T", "RWb"], ["pl"])
                    tt("dve", lg[:], pl[:], RBb[:], ALU.add, ["pl", "RBb"], ["lg"])
                    op("dve", lambda e: e.max(out=top8[:], in_=lg[:]), reads=["lg"], writes=["top8"])
                    ts("dve", msk[:], lg[:], top8[:, 3:4], None, ALU.is_ge, None, ["lg", "top8"], ["msk"])
                    ts("dve", nmx[:], top8[:, 0:1], -1.0, None, ALU.mult, None, ["top8"], ["nmx"])
                    act(ex[:], lg[:], AF.Exp, ["lg", "nmx"], ["ex"], bias=nmx[:, 0:1])
                    tt("dve", ex[:], ex[:], msk[:], ALU.mult, ["ex", "msk"], ["ex"])
                    op("dve", lambda e: e.reduce_sum(out=sm[:], in_=ex[:], axis=AX.X), reads=["ex"], writes=["sm"])
                    op("dve", lambda e: e.reciprocal(out=sm[:], in_=sm[:]), reads=["sm"], writes=["sm"])
                    ts("dve", RWT[:, t, :], ex[:], sm[:, 0:1], None, ALU.mult, None, ["ex", "sm"], ["RWT"])
                    cp("dve", mb[:], msk[:], ["msk"], ["mb"])
                    mm(pp[:], LT[:], mb[:], True, True, ["LT", "mb"], ["pp"])
                    mm(pc[:], ONESb[:], mb[:], True, True, ["ONESb", "mb"], ["pc"])
                    tt("dve", posf[:], pp[:], CNT[:], ALU.add, ["pp", "CNT"], ["posf"])
                    tt("dve", CNT[:], CNT[:], pc[:], ALU.add, ["CNT", "pc"], ["CNT"])
                    ts("dve", idxf[:], posf[:], float(CAP), None, ALU.is_lt, None, ["posf"], ["idxf"])
                    tt("dve", msk[:], msk[:], idxf[:], ALU.mult, ["msk", "idxf"], ["msk"])
                    tt("dve", idxf[:], posf[:], EOFF[:], ALU.add, ["posf", "EOFF"], ["idxf"])
                    tt("dve", idxf[:], idxf[:], msk[:], ALU.mult, ["idxf", "msk"], ["idxf"])
                    ts("dve", idxf[:], idxf[:], 40000.0, None, ALU.add, None, ["idxf"], ["idxf"])
                    cp("dve", IDX[:, t * 32:(t + 1) * 32], idxf[:], ["idxf"], [f"IDX{t}"])
                    for e_ in range(32):
                        T.dma_fn("pool", lambda g, e_=e_, hb=hb: g.indirect_dma_start(
                            out=XD[:, :], out_offset=bass.IndirectOffsetOnAxis(ap=IDX[:, t * 32 + e_:t * 32 + e_ + 1], axis=0),
                            in_=hb[:, :], in_offset=None, bounds_check=BCREG, oob_is_err=False),
                            reads=[khb, f"IDX{t}"], writes=[f"XD{e_}"])
                dma("sp", H2T[:, cs_].rearrange("(k p) n -> p k n", p=128), h2T[:], reads=["h2T"])
            T.barrier()

        aes.close()
        with ExitStack() as es:
            def sb(name, shape, dt=F32):
                return es.enter_context(nc.sbuf_tensor(_u(name), shape, dt))
            def ps(name, shape, dt=F32):
                return es.enter_context(nc.psum_tensor(_u(name), shape, dt))
            acc = sb("acc", [128, 16, 1024])
            Wg = [sb(f"Wg{i}", [128, 8, 1024], BF16) for i in range(2)]
            Wu = [sb(f"Wu{i}", [128, 8, 1024], BF16) for i in range(2)]
            Wds = [sb(f"Wd{i}", [128, 8, 1024], BF16) for i in range(2)]
            XT1 = sb("XT0", [128, 8, 512], BF16)
            XT = [XT1, XT1]
            xs = [sb(f"xs{i}", [128, 1024], BF16) for i in range(2)]
            BG = sb("BG", [128, 32, 8]); BU = sb("BU", [128, 32, 8])
            BD = sb("BD", [128, 1024], BF16)
            aT = sb("aT", [128, 8, 512], BF16)
            gb = [sb("g_0", [128, 512])] * 2; ub = [sb("u_0", [128, 512])] * 2
            sb_ = [sb("s_0", [128, 512])] * 2; u0b = [sb("u0_0", [128, 512])] * 2
            yo = [sb(f"yo{i}", [128, 512], BF16) for i in range(2)]
            tg = [sb(f"tg{i}", [128, 1024], BF16) for i in range(2)]
            ssF = sb("ssF", [128, 1]); ss2F = sb("ss2F", [128, 1])
            pgt = [ps(f"pgt{i}", [128, 512]) for i in range(2)]
            put = [ps(f"put{i}", [128, 512]) for i in range(2)]
            pdn = [ps(f"pdn{i}", [128, 512]) for i in range(2)]
            pT = ps("pTF", [128, 8, 128], BF16)
            dma("sp", BG[:].rearrange("p e k -> p (e k)"), bg_d, writes=["BG"])
            dma("sp", BU[:].rearrange("p e k -> p (e k)"), bu_d, writes=["BU"])
            op("pool", lambda e: e.memset(acc[:], 0.0), writes=["acc"])
            for i in range(2):
                op("pool", lambda e, i=i: e.memset(tg[i][:], 0.0), writes=[f"tg{i}"])

            def load_gu(e):
                sl = e % 2
                dma("pool", Wg[sl][:], wg_d[e].rearrange("(k p) n -> p k n", p=128), writes=[f"Wg{sl}"])
                dma("pool", Wu[sl][:], wu_d[e].rearrange("(k p) n -> p k n", p=128), writes=[f"Wu{sl}"])

            def load_d(e):
                dma("pool", Wds[e % 2][:], wd_d[e].rearrange("(k p) n -> p k n", p=128), writes=[f"Wd{e % 2}"])

            def load_bd(e):
                dma("pool", BD[:], bd_d[e:e + 1, :].to_broadcast([128, 1024]), writes=["BD"])

            xi = [0]

            def build_xt(e, c):
                for st in range(4):
                    x_ = xs[xi[0] % 2]; kx = f"xs{xi[0] % 2}"
                    xi[0] += 1
                    r0 = e * CAP + c * 512 + st * 128
                    dma("sp", x_[:], XD[r0:r0 + 128, :], writes=[kx])
                    for kc in range(8):
                        op("pe", lambda en: en.transpose(out=pT[:, kc, :], in_=x_[:, kc * 128:(kc + 1) * 128],
                                                        identity=identb[:]),
                           reads=[kx, "identb"], writes=["pTF"])
                    act(XT[c][:, :, st * 128:(st + 1) * 128], pT[:], AF.Copy, ["pTF"], ["XT0"])

            def GU(e, c):
                sl = e % 2
                h_ = XT[c]; kh = "XT0"

                def tail(fc):
                    i2 = 0
                    ts("dve", ub[i2][:], u0b[i2][:], -7.0, 7.0, ALU.max, ALU.min, [f"u0_{i2}"], [f"u_{i2}"])
                    stt("dve", aT[:, fc, :], ub[i2][:], 1.0, sb_[i2][:], ALU.add, ALU.mult,
                        [f"u_{i2}", f"s_{i2}"], ["aT"])

                for fc in range(8):
                    i2 = fc % 2
                    pg_ = pgt[i2]; kg = f"pgt{i2}"; pu_ = put[i2]; ku = f"put{i2}"
                    for kc in range(8):
                        mm(pg_[:], Wg[sl][:, kc, fc * 128:(fc + 1) * 128], h_[:, kc, :], kc == 0, kc == 7,
                           [f"Wg{sl}", kh], [kg])
                    for kc in range(8):
                        mm(pu_[:], Wu[sl][:, kc, fc * 128:(fc + 1) * 128], h_[:, kc, :], kc == 0, kc == 7,
                           [f"Wu{sl}", kh], [ku])
                    ts("dve", gb[0][:], pg_[:], BG[:, e, fc:fc + 1], 7.0, ALU.add, ALU.min, [kg, "BG"], ["g_0"])
                    act(u0b[0][:], pu_[:], AF.Identity, [ku, "BU"], ["u0_0"], bias=BU[:, e, fc:fc + 1])
                    act(sb_[0][:], gb[0][:], AF.Silu, ["g_0"], ["s_0"], scale=1.702)
                    tail(fc)
                    combine_step()

            yi = [0]

            def DN(e, c):
                for j in range(4):
                    r0 = e * CAP + c * 512 + j * 128
                    for half in range(2):
                        pd_ = pdn[half]; kd = f"pdn{half}"
                        for fc in range(8):
                            mm(pd_[:], aT[:, fc, j * 128:(j + 1) * 128], Wds[e % 2][:, fc, half * 512:(half + 1) * 512],
                               fc == 0, fc == 7, ["aT", f"Wd{e % 2}"], [kd])
                        y_ = yo[yi[0] % 2]; ky = f"yo{yi[0] % 2}"
                        yi[0] += 1
                        stt("dve", y_[:], pd_[:], 1.0 / 1.702, BD[:, half * 512:(half + 1) * 512], ALU.mult, ALU.add,
                            [kd, "BD"], [ky])
                        dma("sp", YD[r0:r0 + 128, half * 512:(half + 1) * 512], y_[:], reads=[ky],
                            writes=[f"YD{e}_{c * 8 + j * 2 + half}"])

            gi = [0]
            pend_g = []
            pend_a = []

            def combine(e):
                for i in range(16):
                    pend_g.append((e, i))

            def combine_step():
                if pend_a:
                    e, i, k = pend_a.pop(0)
                    stt("dve", acc[:, i, :], tg[k][:], RWT[:, i, e:e + 1], acc[:, i, :], ALU.mult, ALU.add,
                        [f"tg{k}", "acc"], ["acc"])
                if pend_g:
                    e, i = pend_g.pop(0)
                    k = gi[0] % 2
                    gi[0] += 1
                    t_ = tg[k]
                    T.dma_fn("pool", lambda g, t_=t_, i=i, e=e: g.indirect_dma_start(
                        out=t_[:, :], out_offset=None, in_=YD[:, :],
                        in_offset=bass.IndirectOffsetOnAxis(ap=IDX[:, i * 32 + e:i * 32 + e + 1], axis=0),
                        bounds_check=BCREG, oob_is_err=False),
                        reads=[f"YD{e}_{m}" for m in range(16)], writes=[f"tg{k}"])
                    pend_a.append((e, i, k))

            load_gu(0)
            load_d(0)
            load_bd(0)
            load_gu(1)
            load_d(1)
            build_xt(0, 0)
            for e in range(32):
                GU(e, 0)
                build_xt(e, 1)
                DN(e, 0)
                GU(e, 1)
                if e + 2 < 32:
                    load_gu(e + 2)
                if e + 1 < 32:
                    build_xt(e + 1, 0)
                DN(e, 1)
                if e + 2 < 32:
                    load_d(e + 2)
                if e + 1 < 32:
                    load_bd(e + 1)
                combine(e)
            while pend_g or pend_a:
                combine_step()
            T.barrier()
            xv = Wg[0][:].rearrange("p k n -> p (k n)").bitcast(F32)
            sqv = Wu[0][:].rearrange("p k n -> p (k n)")
            for t in range(16):
                x1v = xv[:, (t % 2) * 2048:(t % 2) * 2048 + 1024]; ov = xv[:, (t % 2) * 2048 + 1024:(t % 2) * 2048 + 2048]
                kx = f"x1v{t % 2}"; ko = f"ov{t % 2}"
                dma("sp", x1v, X1[t * 128:(t + 1) * 128, :], writes=[kx])
                act(sqv[:, 0:1024], acc[:, t, :], AF.Square, ["acc"], ["sqv", "ssF"], accum_out=ssF[:])
                ts("dve", ss2F[:], ssF[:], 1.0 / 1024, 1e-6, ALU.mult, ALU.add, ["ssF"], ["ss2F"])
                act(ss2F[:], ss2F[:], AF.Sqrt, ["ss2F"], ["ss2F"])
                op("dve", lambda e: e.reciprocal(out=ss2F[:], in_=ss2F[:]), reads=["ss2F"], writes=["ss2F"])
                stt("dve", ov, acc[:, t, :], ss2F[:, 0:1], G2[:], ALU.mult, ALU.mult, ["acc", "ss2F", "G2"], [ko])
                tt("dve", ov, ov, x1v, ALU.add, [ko, kx], [ko])
                dma("sp", out_d[t * 128:(t + 1) * 128, :], ov, reads=[ko])
            T.finish("sp")
    return nc


_NC = None


def _bf16(a):
    return a.astype(ml_dtypes.bfloat16)


def kernel(**inp):
    global _NC
    f32 = np.float32
    x = np.asarray(inp["x"], f32)
    c = np.asarray(inp["c"], f32)
    positions = np.asarray(inp["positions"]).astype(np.int32)
    sq = lambda k: np.asarray(inp[k])[0]
    if _NC is None:
        _NC = build_program()
    nc = _NC
    gains = np.concatenate([sq("mix_pre_g"), sq("mix_post_g"), sq("ffn_pre_g"), sq("ffn_post_g")])[None, :].astype(f32)
    p = np.arange(128)
    inv_freq = (10000.0 ** (-np.arange(32, dtype=np.float32) / np.float32(32))).astype(f32)
    cst = np.zeros((128, 16), f32)
    sgn_r = np.where((p % 64) < 32, -1.0, 1.0).astype(f32)
    shalf = np.where(p < 64, 1.0, -1.0).astype(f32)
    cst[:, 1] = sgn_r
    cst[:, 2] = np.pi / 2
    cst[:, 3] = inv_freq[p % 32]
    cst[:, 4] = (p < 64)
    cst[:, 5] = -(p >= 64).astype(f32)
    cst[:, 6] = shalf * 0.999999
    cst[:, 7] = -1.0
    cst[:, 8] = -(p < 64).astype(f32)
    cst[:, 9] = 1.0
    cst[:, 10] = -TWO_PI_LO
    cst[:, 11] = -TWO_PI_LO * shalf
    cst[:, 13] = 12582912.0
    cst[:, 12] = shalf
    ident = np.eye(128, dtype=f32)
    iota = np.arange(1024, dtype=f32)[None, :]
    lre = np.concatenate([sq("ssm_lam_re").T, sq("ssm_lam_re").T], 0).astype(f32)
    lim = np.concatenate([sq("ssm_lam_im").T, sq("ssm_lam_im").T], 0).astype(f32)
    ldt = sq("ssm_log_dt")[None, :].astype(f32)
    bre = sq("ssm_b_re"); bim = sq("ssm_b_im")
    bl1 = np.zeros((128, 32, 128), f32); bl2 = np.zeros((128, 32, 128), f32)
    for g in range(32):
        r0 = ((g % 8) // 2) * 32 + (g % 2) * 16
        bl1[r0:r0 + 16, g, 0:64] = bre[g].T; bl1[r0:r0 + 16, g, 64:128] = bim[g].T
        bl2[r0:r0 + 16, g, 0:64] = bim[g].T; bl2[r0:r0 + 16, g, 64:128] = bre[g].T
    cre = sq("ssm_c_re"); cim = sq("ssm_c_im")
    crp = np.transpose(cre, (2, 0, 1)); cip = np.transpose(cim, (2, 0, 1))
    cr = np.concatenate([crp, crp], 0).reshape(128, 512).astype(f32)
    ci = np.concatenate([cip, cip], 0).reshape(128, 512).astype(f32)
    dsk = sq("ssm_d").reshape(4, 128).T.copy().astype(f32)
    bonehot = np.zeros((32, 8192), f32)
    for n in range(32):
        bonehot[n, n * 256:(n + 1) * 256] = 1.0
    bonehot = _bf16(bonehot)
    dmask = np.ones((128, 4, 512), f32)
    kk = np.arange(128)[:, None]; qq = np.arange(128)[None, :]
    for i in range(4):
        for j in range(4):
            if i // 2 == j // 2:
                if i == j:
                    dmask[:, i, j * 128:(j + 1) * 128] = (kk <= qq)
                elif i > j:
                    dmask[:, i, j * 128:(j + 1) * 128] = 0.0
    dmask = _bf16(dmask.reshape(128, 2048))
    bg = np.transpose(sq("b_gate").reshape(32, 8, 128), (2, 0, 1)).reshape(128, 256).astype(f32)
    bu = np.transpose(sq("b_up").reshape(32, 8, 128), (2, 0, 1)).reshape(128, 256).astype(f32)
    ltri = _bf16((np.arange(128)[:, None] < np.arange(128)[None, :]).astype(f32))
    eoff = (np.arange(32, dtype=f32) * 1024.0 - 40000.0)[None, :]
    shared = {
        "ltri": ltri, "eoff": eoff,
        "ada_w": sq("ada_w"), "ada_b": sq("ada_b")[None, :], "gains": gains, "w_in": sq("w_in"),
        "cst": cst, "ident": ident, "iota": iota, "lre": lre, "lim": lim, "ldt": ldt,
        "bl1": bl1.reshape(128, 4096), "bl2": bl2.reshape(128, 4096), "cr": cr, "ci": ci, "dsk": dsk,
        "w_glu": sq("ssm_w_glu"), "w_sb": sq("w_ssm_branch"), "w_ab": sq("w_attn_branch"), "w_out": sq("w_out"),
        "bonehot": bonehot, "dmask": dmask, "router_w": sq("router_w"), "router_b": sq("router_b")[None, :],
        "w_gate": sq("w_gate"), "w_up": sq("w_up"), "w_down": sq("w_down"),
        "b_gate": bg, "b_up": bu, "b_down": sq("b_down"),
    }
    shared = {k: np.ascontiguousarray(v) for k, v in shared.items()}
    in_maps = []
    for core in range(8):
        b, j = core // 4, core % 4
        nprev = j * 2048
        x_loc = np.zeros((8192, 1024), f32)
        x_loc[6144 - nprev:] = x[b, :nprev + 2048]
        tm = np.zeros(8192, f32); tm[6144 - nprev:] = 1.0
        pos = np.zeros(8192, np.int32); pos[6144 - nprev:] = positions[b, :nprev + 2048]
        ninv = (3 - j) * 8
        vb = np.zeros((8, 96), f32); vv = np.zeros((8, 96), f32); vo = np.zeros((8, 96), f32)
        for qb in range(8):
            n = np.arange(32)
            valid = (n >= ninv) & (n < 24 + qb)
            vb[qb, 64:96] = np.where(valid, 0.0, -1e30)
            vv[qb, 64:96] = valid
            vo[qb, 64 + 24 + qb] = 1.0
        m = dict(shared)
        m.update({
            "x_loc": x_loc, "tmask": np.ascontiguousarray(tm.reshape(64, 128).T),
            "pos_loc": pos[None, :], "c_b": np.ascontiguousarray(c[b].reshape(8, 128).T),
            "vbias": vb.reshape(1, 768), "vvalid": vv.reshape(1, 768), "vown": vo.reshape(1, 768),
        })
        in_maps.append(m)
    res = run_bass_kernel_spmd(nc, in_maps, core_ids=list(range(8)))
    out = np.zeros((2, 8192, 1024), f32)
    for core in range(8):
        b, j = core // 4, core % 4
        out[b, j * 2048:(j + 1) * 2048] = res.results[core]["out"]
    return out
```

```python
import numpy as np
from contextlib import ExitStack
import ml_dtypes
import concourse.bass as bass
import concourse.mybir as mybir
from concourse.bass_utils import run_bass_kernel_spmd

F32 = mybir.dt.float32
BF16 = mybir.dt.bfloat16
I32 = mybir.dt.int32
ALU = mybir.AluOpType
AF = mybir.ActivationFunctionType
AX = mybir.AxisListType
PI = float(np.pi)
TWO_PI = float(2 * np.pi)
PI_LO = 3.1415925
TWO_PI_LO = 6.283185
SEM_CAP = 30000
NEG = -30000.0


class Tracker:
    def __init__(self, nc):
        self.nc = nc
        self.engs = {"pe": nc.tensor, "act": nc.scalar, "dve": nc.vector,
                     "pool": nc.gpsimd, "sp": nc.sync}
        self.cur_sem = {}
        self.cnt = {}
        self.nsem = 0
        for e in ("pe", "act", "dve", "pool"):
            self._new_epoch(e)
        self.seen = {e: {} for e in self.engs}
        self.lastw = {}
        self.reads = {}
        self.NS = 8
        self.dq = {}
        for q in ("sp", "pool"):
            sems = [self._alloc(f"dq_{q}_{i}") for i in range(self.NS)]
            self.dq[q] = {"sems": sems, "i": 0, "last": {}}

    def _alloc(self, name):
        self.nsem += 1
        return self.nc.alloc_semaphore(name)

    def _new_epoch(self, e):
        self.cur_sem[e] = self._alloc(f"c_{e}_{self.nsem}")
        self.cnt[e] = 0

    def _wait(self, e, ev):
        if ev is None:
            return
        sem, val, src = ev
        if src == "pe" and e == "pe":
            return
        k = id(sem)
        old = self.seen[e].get(k)
        if old is not None and old >= val:
            return
        self.seen[e][k] = val
        self.engs[e].wait_ge(sem, val)

    def _deps(self, e, reads, writes):
        for r in reads:
            self._wait(e, self.lastw.get(r))
        for w in writes:
            self._wait(e, self.lastw.get(w))
            for ev in self.reads.get(w, ()):
                self._wait(e, ev)

    def _commit(self, ev, reads, writes):
        for r in reads:
            lst = self.reads.setdefault(r, [])
            lst.append(ev)
            if len(lst) > 24:
                d = {}
                for s, v, src in lst:
                    if id(s) not in d or d[id(s)][1] < v:
                        d[id(s)] = (s, v, src)
                self.reads[r] = list(d.values())
        for w in writes:
            self.lastw[w] = ev
            self.reads[w] = []

    def op(self, e, fn, reads=(), writes=()):
        self._deps(e, reads, writes)
        if self.cnt[e] >= SEM_CAP:
            self._new_epoch(e)
        ins = fn(self.engs[e])
        self.cnt[e] += 1
        ins.then_inc(self.cur_sem[e], 1)
        ev = (self.cur_sem[e], self.cnt[e], e)
        self._commit(ev, reads, writes)
        return ev

    def dma(self, q, out, in_, reads=(), writes=(), **kw):
        d = self.dq[q]
        i = d["i"]
        d["i"] += 1
        slot = i % self.NS
        sem = d["sems"][slot]
        prev = 16 * (i // self.NS)
        if prev + 16 > 2 * SEM_CAP:
            d["sems"] = [self._alloc(f"dq_{q}_{i}_{k}") for k in range(self.NS)]
            d["i"] = 1
            i = 0
            slot = 0
            sem = d["sems"][0]
            prev = 0
        if prev > 0:
            self._wait(q, (sem, prev, "dma"))
        self._deps(q, reads, writes)
        ins = self.engs[q].dma_start(out=out, in_=in_, **kw)
        ins.then_inc(sem, 16)
        ev = (sem, prev + 16, "dma")
        self._commit(ev, reads, writes)
        d["last"][id(sem)] = ev
        return ev

    def dma_fn(self, q, fn, reads=(), writes=()):
        d = self.dq[q]
        i = d["i"]
        d["i"] += 1
        slot = i % self.NS
        sem = d["sems"][slot]
        prev = 16 * (i // self.NS)
        if prev + 16 > 2 * SEM_CAP:
            d["sems"] = [self._alloc(f"dq_{q}_{i}_{k}") for k in range(self.NS)]
            d["i"] = 1
            slot = 0
            sem = d["sems"][0]
            prev = 0
        if prev > 0:
            self._wait(q, (sem, prev, "dma"))
        self._deps(q, reads, writes)
        ins = fn(self.engs[q])
        ins.then_inc(sem, 16)
        ev = (sem, prev + 16, "dma")
        self._commit(ev, reads, writes)
        d["last"][id(sem)] = ev
        return ev

    def _all_events(self):
        evs = []
        for e in ("pe", "act", "dve", "pool"):
            if self.cnt[e] > 0:
                evs.append((self.cur_sem[e], self.cnt[e], "bar"))
        for q, d in self.dq.items():
            for ev in d["last"].values():
                evs.append((ev[0], ev[1], "bar"))
        return evs

    def barrier(self):
        evs = self._all_events()
        for e in self.engs:
            for ev in evs:
                self._wait(e, ev)
        self.lastw.clear()
        self.reads.clear()

    def finish(self, eng="sp"):
        for ev in self._all_events():
            self._wait(eng, ev)


def build_program():
    nc = bass.Bass("TRN2", target_bir_lowering=False)

    def din(name, shape, dt=F32):
        return nc.dram_tensor(name, list(shape), dt, kind="ExternalInput").ap()

    def dscr(name, shape, dt):
        return nc.dram_tensor(name, list(shape), dt, kind="Internal").ap()

    x_loc = din("x_loc", [8192, 1024])
    tmask_d = din("tmask", [128, 64])
    pos_d = din("pos_loc", [1, 8192], I32)
    c_b = din("c_b", [128, 8])
    ada_w = din("ada_w", [1024, 6144])
    ada_b = din("ada_b", [1, 6144])
    gains = din("gains", [1, 4096])
    w_in = din("w_in", [1024, 4096])
    cst_d = din("cst", [128, 16])
    ident_d = din("ident", [128, 128])
    iota_d = din("iota", [1, 1024])
    lre_d = din("lre", [128, 32])
    lim_d = din("lim", [128, 32])
    ldt_d = din("ldt", [1, 32])
    bl1_d = din("bl1", [128, 32 * 128])
    bl2_d = din("bl2", [128, 32 * 128])
    cr_d = din("cr", [128, 512])
    ci_d = din("ci", [128, 512])
    dsk_d = din("dsk", [128, 4])
    wglu_d = din("w_glu", [512, 512])
    wsb_d = din("w_sb", [512, 1024])
    wab_d = din("w_ab", [512, 1024])
    wout_d = din("w_out", [1024, 1024])
    bonehot_d = din("bonehot", [32, 8192], BF16)
    dm_d = din("dmask", [128, 2048], BF16)
    vb_d = din("vbias", [1, 8 * 96])
    vv_d = din("vvalid", [1, 8 * 96])
    vo_d = din("vown", [1, 8 * 96])
    rw_d = din("router_w", [1024, 32])
    rb_d = din("router_b", [1, 32])
    wg_d = din("w_gate", [32, 1024, 1024])
    wu_d = din("w_up", [32, 1024, 1024])
    wd_d = din("w_down", [32, 1024, 1024])
    bg_d = din("b_gate", [128, 32 * 8])
    bu_d = din("b_up", [128, 32 * 8])
    bd_d = din("b_down", [32, 1024])
    ltri_d = din("ltri", [128, 128], BF16)
    eoff_d = din("eoff", [1, 32])
    out_d = nc.dram_tensor("out", [2048, 1024], F32, kind="ExternalOutput").ap()
    CAP = 1024
    XD = dscr("XD", [32 * CAP, 1024], BF16)
    YD = dscr("YD", [32 * CAP, 1024], BF16)

    UT = dscr("UT", [512, 8192], BF16)
    KT = dscr("KT", [512, 8192], BF16)
    VV = dscr("VV", [8192, 512], BF16)
    QT = dscr("QT", [512, 2048], BF16)
    GST = dscr("GST", [1024, 2048], BF16)
    GAT = dscr("GAT", [1024, 2048], BF16)
    YST = dscr("YST", [512, 2048], BF16)
    OAT = dscr("OAT", [512, 2048], BF16)
    X1 = dscr("X1", [2048, 1024], F32)
    H2T = dscr("H2T", [1024, 2048], BF16)

    T = Tracker(nc)
    BCREG = nc.gpsimd.to_reg(32 * 1024 - 1)
    _cnt = [0]

    def _u(n):
        _cnt[0] += 1
        return f'sb{_cnt[0]}_{n}'
    op = T.op
    dma = T.dma
    uid = [0]

    def mm(out, lhsT, rhs, start, stop, reads, writes):
        return op("pe", lambda e: e.matmul(out, lhsT=lhsT, rhs=rhs, start=start, stop=stop),
                  reads=reads, writes=writes)

    def act(out, in_, func, reads, writes, eng="act", **kw):
        return op(eng, lambda e: e.activation(out=out, in_=in_, func=func, **kw), reads=reads, writes=writes)

    def tt(eng, out, a, b, o, reads, writes):
        return op(eng, lambda e: e.tensor_tensor(out=out, in0=a, in1=b, op=o), reads=reads, writes=writes)

    def ts(eng, out, a, s1, s2, o0, o1, reads, writes):
        if o1 is None:
            return op(eng, lambda e: e.tensor_scalar(out=out, in0=a, scalar1=s1, scalar2=None, op0=o0),
                      reads=reads, writes=writes)
        return op(eng, lambda e: e.tensor_scalar(out=out, in0=a, scalar1=s1, scalar2=s2, op0=o0, op1=o1),
                  reads=reads, writes=writes)

    def stt(eng, out, a, s, b, o0, o1, reads, writes):
        return op(eng, lambda e: e.scalar_tensor_tensor(out=out, in0=a, scalar=s, in1=b, op0=o0, op1=o1),
                  reads=reads, writes=writes)

    def cp(eng, out, a, reads, writes):
        return op(eng, lambda e: e.tensor_copy(out=out, in_=a), reads=reads, writes=writes)


    MAGIC = 12582912.0
    INV2PI = float(1.0 / (2 * np.pi))

    def reduce_angle(x, kx, k, kk, r, kr, ab, kab):
        ts("dve", k, x, INV2PI, MAGIC, ALU.mult, ALU.add, [kx], [kk])
        ts("dve", k, k, -MAGIC, None, ALU.add, None, [kk], [kk])
        stt("dve", r, k, -TWO_PI, x, ALU.mult, ALU.add, [kk, kx], [kr])
        ts("dve", r, r, PI_LO, -PI_LO, ALU.min, ALU.max, [kr], [kr])
        act(ab, r, AF.Abs, [kr], [kab])

    with ExitStack() as gs:
        def gsb(name, shape, dt=F32):
            return gs.enter_context(nc.sbuf_tensor(_u(name), shape, dt))
        G2 = gsb("G2", [128, 1024])
        ident = gsb("ident", [128, 128])
        identb = gsb("identb", [128, 128], BF16)
        CST = gsb("CST", [128, 16])
        tmask = gsb("tmaskt", [128, 64])
        RWT = gsb("RWT", [128, 16, 32])
        IDX = gsb("IDX", [128, 512], I32)
        aes = ExitStack()
        PRM = aes.enter_context(nc.sbuf_tensor(_u("PRM"), [128, 5, 1024], F32))
        dma("sp", ident[:], ident_d, writes=["ident"])
        dma("sp", CST[:], cst_d, writes=["CST"])
        dma("sp", tmask[:], tmask_d, writes=["tmask"])
        cp("dve", identb[:], ident[:], ["ident"], ["identb"])
        SGNR = CST[:, 1:2]
        HALFPI = CST[:, 2:3]
        INVF = CST[:, 3:4]
        MLO = CST[:, 4:5]
        NMHI = CST[:, 5:6]
        SHALF = CST[:, 6:7]
        NEG1 = CST[:, 7:8]
        NMLO = CST[:, 8:9]
        ONE9 = CST[:, 9:10]
        N2PI = CST[:, 10:11]
        SC1 = CST[:, 11:12]
        SHALF1 = CST[:, 12:13]
        MAGICC = CST[:, 13:14]

        with ExitStack() as es:
            def sb(name, shape, dt=F32):
                return es.enter_context(nc.sbuf_tensor(_u(name), shape, dt))
            ct = sb("ct", [128, 8]); cs = sb("cs", [128, 8])
            CB = sb("CB", [128, 8, 128], BF16)
            AWb = sb("AWb", [128, 8, 512], BF16)
            ADAB = sb("ADAB", [128, 6144]); ADA = sb("ADA", [128, 6144])
            GB = sb("GB", [128, 4096])
            pa = es.enter_context(nc.psum_tensor(_u("pa"), [128, 512], F32))
            dma("sp", ct[:], c_b, writes=["ct"])
            dma("pool", ADAB[:], ada_b.to_broadcast([128, 6144]), writes=["ADAB"])
            dma("pool", GB[:], gains.to_broadcast([128, 4096]), writes=["GB"])
            act(cs[:], ct[:], AF.Silu, ["ct"], ["cs"])
            for kc in range(8):
                cp("dve", CB[:, kc, :], cs[:, kc:kc + 1].to_broadcast([128, 128]), ["cs"], ["CB"])
            for n in range(12):
                dma("pool", AWb[:], ada_w[:, n * 512:(n + 1) * 512].rearrange("(k p) n -> p k n", p=128),
                    writes=["AWb"])
                for kc in range(8):
                    mm(pa[:], CB[:, kc, :], AWb[:, kc, :], kc == 0, kc == 7, ["CB", "AWb"], ["pa"])
                tt("dve", ADA[:, n * 512:(n + 1) * 512], pa[:], ADAB[:, n * 512:(n + 1) * 512], ALU.add,
                   ["pa", "ADAB"], ["ADA"])
            stt("dve", PRM[:, 0, :], ADA[:, 1024:2048], 1.0, GB[:, 0:1024], ALU.add, ALU.mult, ["ADA", "GB"], ["PRM"])
            cp("dve", PRM[:, 1, :], ADA[:, 0:1024], ["ADA"], ["PRM"])
            tt("dve", PRM[:, 2, :], ADA[:, 2048:3072], GB[:, 1024:2048], ALU.mult, ["ADA", "GB"], ["PRM"])
            stt("dve", PRM[:, 3, :], ADA[:, 4096:5120], 1.0, GB[:, 2048:3072], ALU.add, ALU.mult, ["ADA", "GB"], ["PRM"])
            cp("dve", PRM[:, 4, :], ADA[:, 3072:4096], ["ADA"], ["PRM"])
            tt("dve", G2[:], ADA[:, 5120:6144], GB[:, 3072:4096], ALU.mult, ["ADA", "GB"], ["G2"])
            T.barrier()

        def norm_mod(es_sb, xt, key_x, a_idx, sh_idx, maskcol, hb, key_hb, tmp, tmp2, ss, rs, sq):
            act(sq[:], xt, AF.Square, [key_x], ["sq", "ss"], accum_out=ss[:])
            ts("dve", rs[:], ss[:], 1.0 / 1024, 1e-6, ALU.mult, ALU.add, ["ss"], ["rs"])
            act(rs[:], rs[:], AF.Sqrt, ["rs"], ["rs"])
            op("dve", lambda e: e.reciprocal(out=rs[:], in_=rs[:]), reads=["rs"], writes=["rs"])
            stt("dve", tmp[:], xt, rs[:, 0:1], PRM[:, a_idx, :], ALU.mult, ALU.mult, [key_x, "rs", "PRM"], ["tmp"])
            tt("dve", tmp2[:], tmp[:], PRM[:, sh_idx, :], ALU.add, ["tmp", "PRM"], ["tmp2"])
            if maskcol is None:
                act(hb, tmp2[:], AF.Copy, ["tmp2"], [key_hb])
            else:
                act(hb, tmp2[:], AF.Copy, ["tmp2", "tmask"], [key_hb], scale=maskcol)

        with ExitStack() as es:
            def sb(name, shape, dt=F32):
                return es.enter_context(nc.sbuf_tensor(_u(name), shape, dt))
            def ps(name, shape, dt=F32):
                return es.enter_context(nc.psum_tensor(_u(name), shape, dt))
            WB = sb("WB", [128, 8, 5120], BF16)
            xt = sb("xt", [128, 1024]); tmp = sb("tmp", [128, 1024]); tmp2 = sb("tmp2", [128, 1024])
            sq = sb("sq", [128, 1024], BF16)
            ss = sb("ss", [128, 1]); rs = sb("rs", [128, 1])
            hb = sb("hb", [128, 1024], BF16)
            hT = sb("hT", [128, 8, 512], BF16)
            posi = sb("posi", [128, 512], I32); posf = sb("posf", [128, 512])
            a1 = sb("a1", [128, 512]); a2 = sb("a2", [128, 512])
            cosT = sb("cosT", [128, 512]); sinS = sb("sinS", [128, 512])
            m1 = sb("m1", [128, 512]); m2 = sb("m2", [128, 512])
            ob = [sb(f"ob{i}", [128, 512], BF16) for i in range(3)]
            pT = ps("pT", [128, 8, 128], BF16)
            pk = ps("pk", [128, 512]); pks = ps("pks", [128, 512])
            pm = [ps(f"pm{i}", [128, 512]) for i in range(2)]

            blocks = [(0, 0, False), (512, 1024, False), (1024, 1024, True), (1536, 1536, False),
                      (2048, 512, False), (2560, 512, True), (3072, 2048, False), (3584, 2560, False),
                      (4096, 3072, False), (4608, 3584, False)]
            for dst, src, swp in blocks:
                if not swp:
                    dma("pool", WB[:, :, dst:dst + 512], w_in[:, src:src + 512].rearrange("(k p) n -> p k n", p=128),
                        writes=["WB"])
                else:
                    srcv = w_in[:, src:src + 512].rearrange("(k p) (h t j) -> p k h t j", p=128, h=8, t=2, j=32)
                    dstv = WB[:, :, dst:dst + 512].rearrange("p k (h t j) -> p k h t j", h=8, t=2, j=32)
                    for kc in range(8):
                        dma("pool", dstv[:, kc, :, 0, :], srcv[:, kc, :, 1, :], writes=["WB"])
                        dma("pool", dstv[:, kc, :, 1, :], srcv[:, kc, :, 0, :], writes=["WB"])

            T.barrier()
            obi = [0]

            def emit(src_ps, key_ps, dst_ap, func=AF.Copy, **kw):
                o = ob[obi[0] % 3]
                k = f"ob{obi[0] % 3}"
                obi[0] += 1
                act(o[:], src_ps, func, [key_ps], [k], **kw)
                dma("sp", dst_ap, o[:], reads=[k])

            for c in range(16):
                own = c >= 12
                for j in range(4):
                    t = 4 * c + j
                    dma("sp", xt[:], x_loc[t * 128:(t + 1) * 128, :], writes=["xt"])
                    norm_mod(sb, xt[:], "xt", 0, 1, tmask[:, t:t + 1], hb[:], "hb", tmp, tmp2, ss, rs, sq)
                    for kc in range(8):
                        op("pe", lambda e: e.transpose(out=pT[:, kc, :], in_=hb[:, kc * 128:(kc + 1) * 128],
                                                       identity=identb[:]),
                           reads=["hb", "identb"], writes=["pT"])
                    cp("dve", hT[:, :, j * 128:(j + 1) * 128], pT[:], ["pT"], ["hT"])
                dma("pool", posi[:], pos_d[0:1, c * 512:(c + 1) * 512].to_broadcast([128, 512]), writes=["posi"])
                cp("dve", posf[:], posi[:], ["posi"], ["posf"])
                ts("dve", a1[:], posf[:], INVF, None, ALU.mult, None, ["posf", "CST"], ["a1"])
                reduce_angle(a1[:], "a1", a2[:], "a2", m1[:], "m1", m2[:], "m2")
                act(sinS[:], m1[:], AF.Sin, ["m1", "CST"], ["sinS"], scale=SGNR)
                act(cosT[:], m2[:], AF.Sin, ["m2", "CST"], ["cosT"], scale=NEG1, bias=HALFPI)

                def rope_proj(c0, c0s, dst, scale):
                    for cb in range(4):
                        for kc in range(8):
                            mm(pk[:], WB[:, kc, c0 + cb * 128:c0 + (cb + 1) * 128], hT[:, kc, :], kc == 0, kc == 7,
                               ["WB", "hT"], ["pk"])
                        for kc in range(8):
                            mm(pks[:], WB[:, kc, c0s + cb * 128:c0s + (cb + 1) * 128], hT[:, kc, :], kc == 0, kc == 7,
                               ["WB", "hT"], ["pks"])
                        tt("dve", m1[:], pk[:], cosT[:], ALU.mult, ["pk", "cosT"], ["m1"])
                        tt("dve", m2[:], pks[:], sinS[:], ALU.mult, ["pks", "sinS"], ["m2"])
                        tt("dve", m1[:], m1[:], m2[:], ALU.add, ["m1", "m2"], ["m1"])
                        emit(m1[:], "m1", dst(cb), scale=scale)

                rope_proj(512, 1024, lambda cb: KT[cb * 128:(cb + 1) * 128, c * 512:(c + 1) * 512], 1.0)
                for cb in range(4):
                    p = pm[cb % 2]; kp = f"pm{cb % 2}"
                    for kc in range(8):
                        mm(p[:], WB[:, kc, cb * 128:(cb + 1) * 128], hT[:, kc, :], kc == 0, kc == 7, ["WB", "hT"], [kp])
                    emit(p[:], kp, UT[cb * 128:(cb + 1) * 128, c * 512:(c + 1) * 512])
                for j in range(4):
                    p = pm[j % 2]; kp = f"pm{j % 2}"
                    for kc in range(8):
                        mm(p[:], hT[:, kc, j * 128:(j + 1) * 128], WB[:, kc, 1536:2048], kc == 0, kc == 7, ["WB", "hT"], [kp])
                    emit(p[:], kp, VV[(4 * c + j) * 128:(4 * c + j + 1) * 128, :])
                if own:
                    co = c - 12
                    rope_proj(2048, 2560, lambda cb: QT[cb * 128:(cb + 1) * 128, co * 512:(co + 1) * 512], 0.125)
                    for gi, (c0, dstT) in enumerate(((3072, GST), (4096, GAT))):
                        for db in range(8):
                            p = pm[db % 2]; kp = f"pm{db % 2}"
                            for kc in range(8):
                                mm(p[:], WB[:, kc, c0 + db * 128:c0 + (db + 1) * 128], hT[:, kc, :], kc == 0, kc == 7,
                                   ["WB", "hT"], [kp])
                            emit(p[:], kp, dstT[db * 128:(db + 1) * 128, co * 512:(co + 1) * 512], func=AF.Sigmoid)
            T.barrier()

        SEG = 1024
        NSEG = 8192 // SEG
        with ExitStack() as es:
            def sb(name, shape, dt=F32):
                return es.enter_context(nc.sbuf_tensor(_u(name), shape, dt))
            Y = sb("Y", [128, 4, 2048])
            LA = sb("LA", [128, 32, 128], BF16); LB = sb("LB", [128, 32, 128], BF16)
            BL1b = sb("BL1b", [128, 32, 128], BF16); BL2b = sb("BL2b", [128, 32, 128], BF16)
            TH = sb("TH", [128, 32]); RHO = sb("RHO", [128, 32]); CAR = sb("CAR", [128, 32])
            PHf = sb("PHf", [128, 32]); OFFT = sb("OFFT", [128, 8, 32]); NOFF = sb("NOFF", [128, 8, 32])
            SB1 = sb("SB1", [128, 8, 32]); SB2 = sb("SB2", [128, 8, 32])
            DSK = sb("DSK", [128, 4])
            dma("sp", DSK[:], dsk_d, writes=["DSK"])
            with ExitStack() as e2:
                def sb2(name, shape, dt=F32):
                    return e2.enter_context(nc.sbuf_tensor(_u(name), shape, dt))
                LRE = sb2("LRE", [128, 32]); LIM = sb2("LIM", [128, 32]); LDT = sb2("LDT", [128, 32])
                CR = sb2("CR", [128, 32, 16]); CI = sb2("CI", [128, 32, 16])
                w = [sb2(f"w{i}", [128, 32]) for i in range(10)]
                c1 = sb2("c1", [128, 32, 16]); c2 = sb2("c2", [128, 32, 16])
                cpr = sb2("cpr", [128, 32, 16]); cpi = sb2("cpi", [128, 32, 16])
                for src, dstb, kb in ((bl1_d, BL1b, "BL1b"), (bl2_d, BL2b, "BL2b")):
                    dma("pool", dstb[:].rearrange("p g m -> p (g m)"), src, writes=[kb])
                dma("sp", LRE[:], lre_d, writes=["LRE"])
                dma("sp", LIM[:], lim_d, writes=["LIM"])
                dma("pool", LDT[:], ldt_d.to_broadcast([128, 32]), writes=["LDT"])
                dma("sp", CR[:].rearrange("p g c -> p (g c)"), cr_d, writes=["CR"])
                dma("sp", CI[:].rearrange("p g c -> p (g c)"), ci_d, writes=["CI"])
                K = "tb"
                dt_ = w[0]
                act(dt_[:], LDT[:], AF.Exp, ["LDT"], [K])
                tt("dve", TH[:], LIM[:], dt_[:], ALU.mult, ["LIM", K], ["TH"])
                tt("dve", w[1][:], LRE[:], dt_[:], ALU.mult, ["LRE", K], [K])
                act(RHO[:], w[1][:], AF.Exp, [K], ["RHO"])
                reduce_angle(TH[:], "TH", w[2][:], K, w[3][:], K, w[9][:], K)
                act(w[4][:], w[3][:], AF.Sin, [K, "CST"], [K], scale=ONE9)
                act(w[5][:], w[9][:], AF.Sin, [K, "CST"], [K], scale=NEG1, bias=HALFPI)
                tt("dve", w[6][:], RHO[:], w[5][:], ALU.mult, ["RHO", K], [K])
                ts("dve", w[6][:], w[6][:], -1.0, None, ALU.add, None, [K], [K])
                tt("dve", w[7][:], RHO[:], w[4][:], ALU.mult, ["RHO", K], [K])
                tt("dve", w[8][:], LRE[:], LRE[:], ALU.mult, ["LRE"], [K])
                tt("dve", w[9][:], LIM[:], LIM[:], ALU.mult, ["LIM"], [K])
                tt("dve", w[8][:], w[8][:], w[9][:], ALU.add, [K], [K])
                op("dve", lambda e: e.reciprocal(out=w[8][:], in_=w[8][:]), reads=[K], writes=[K])
                tt("dve", w[0][:], w[6][:], LRE[:], ALU.mult, [K, "LRE"], [K])
                tt("dve", w[1][:], w[7][:], LIM[:], ALU.mult, [K, "LIM"], [K])
                tt("dve", w[0][:], w[0][:], w[1][:], ALU.add, [K], [K])
                tt("dve", w[2][:], w[0][:], w[8][:], ALU.mult, [K], [K])
                tt("dve", w[0][:], w[7][:], LRE[:], ALU.mult, [K, "LRE"], [K])
                tt("dve", w[1][:], w[6][:], LIM[:], ALU.mult, [K, "LIM"], [K])
                tt("dve", w[0][:], w[0][:], w[1][:], ALU.subtract, [K], [K])
                tt("dve", w[3][:], w[0][:], w[8][:], ALU.mult, [K], [K])
                qrb = w[2][:, :].unsqueeze(2).to_broadcast([128, 32, 16])
                qib = w[3][:, :].unsqueeze(2).to_broadcast([128, 32, 16])
                tt("dve", c1[:], CR[:], qrb, ALU.mult, ["CR", K], [K])
                tt("dve", c2[:], CI[:], qib, ALU.mult, ["CI", K], [K])
                tt("dve", cpr[:], c1[:], c2[:], ALU.subtract, [K], [K])
                tt("dve", c1[:], CR[:], qib, ALU.mult, ["CR", K], [K])
                tt("dve", c2[:], CI[:], qrb, ALU.mult, ["CI", K], [K])
                tt("dve", cpi[:], c1[:], c2[:], ALU.add, [K], [K])
                ts("dve", c1[:], cpr[:], MLO, None, ALU.mult, None, [K, "CST"], [K])
                stt("dve", c1[:], cpi[:], NMHI, c1[:], ALU.mult, ALU.add, [K, "CST"], [K])
                ts("dve", c2[:], cpi[:], NMLO, None, ALU.mult, None, [K, "CST"], [K])
                stt("dve", c2[:], cpr[:], NMHI, c2[:], ALU.mult, ALU.add, [K, "CST"], [K])
                op("pool", lambda e: e.memset(LA[:], 0.0), writes=["LA"])
                op("pool", lambda e: e.memset(LB[:], 0.0), writes=["LB"])
                for src, dst, kd in ((c1, LA, "LA"), (c2, LB, "LB")):
                    dv = dst[:].rearrange("p (a r) (s c) -> p a r s c", r=8, s=8, c=16)
                    sv = src[:].rearrange("p (a r) c -> p a r c", r=8)
                    for r in range(8):
                        cp("dve", dv[:, :, r, r, :], sv[:, :, r, :], [K], [kd])
                op("dve", lambda e: e.memset(CAR[:], 0.0), writes=["CAR"])
                ts("dve", w[0][:], TH[:], INV2PI, MAGIC, ALU.mult, ALU.add, ["TH"], [K])
                ts("dve", w[0][:], w[0][:], -MAGIC, None, ALU.add, None, [K], [K])
                stt("dve", PHf[:], TH[:], INV2PI, w[0][:], ALU.mult, ALU.subtract, ["TH", K], ["PHf"])
                for sg in range(8):
                    ts("dve", w[1][:], PHf[:], float(sg * 1024), None, ALU.mult, None, ["PHf"], [K])
                    ts("dve", w[2][:], w[1][:], MAGIC, None, ALU.add, None, [K], [K])
                    stt("dve", OFFT[:, sg, :], w[2][:], -MAGIC, w[1][:], ALU.add, ALU.subtract, [K], ["OFFT"])
                    ts("dve", OFFT[:, sg, :], OFFT[:, sg, :], -1.0, None, ALU.mult, None, ["OFFT"], ["OFFT"])
                ts("dve", NOFF[:].rearrange("p a g -> p (a g)"), OFFT[:].rearrange("p a g -> p (a g)"), -1.0, None,
                   ALU.mult, None, ["OFFT"], ["NOFF"])
                ts("dve", SB2[:].rearrange("p a g -> p (a g)"), OFFT[:].rearrange("p a g -> p (a g)"),
                   TWO_PI, None, ALU.mult, None, ["OFFT"], ["SB2"])
                ts("dve", SB1[:].rearrange("p a g -> p (a g)"), SB2[:].rearrange("p a g -> p (a g)"), SHALF1, None,
                   ALU.mult, None, ["SB2", "CST"], ["SB1"])
                T.barrier()

            with ExitStack() as e2:
                def sb2(name, shape, dt=F32):
                    return e2.enter_context(nc.sbuf_tensor(_u(name), shape, dt))
                def ps2(name, shape, dt=F32):
                    return e2.enter_context(nc.psum_tensor(_u(name), shape, dt))
                UTb = sb2("UTb", [128, 8192], BF16)
                IOT = sb2("IOT", [128, SEG])
                UB = sb2("UB", [128, SEG]); U_ = sb2("U_", [128, SEG]); KK = sb2("KK", [128, SEG]); NF = sb2("NF", [128, SEG]); AB = sb2("AB", [128, SEG])
                COS2 = sb2("COS2", [128, SEG]); SINS = sb2("SINS", [128, SEG]); SIN2 = sb2("SIN2", [128, SEG])
                m2 = sb2("sm2", [128, 512])
                W = sb2("W", [128, SEG]); RB = sb2("RB", [128, SEG]); Z = sb2("Z", [128, SEG])
                A1b = sb2("A1b", [128, SEG], BF16); A2b = sb2("A2b", [128, SEG], BF16)
                py = [ps2(f"py{i}", [128, 512]) for i in range(4)]
                p1 = [ps2(f"p1{i}", [128, 512]) for i in range(2)]
                p2 = [ps2(f"p2{i}", [128, 512]) for i in range(2)]
                dma("pool", IOT[:], iota_d.to_broadcast([128, SEG]), writes=["IOT"])
                ZT = sb2("ZT", [128, 4096], BF16)
                op("pool", lambda e: e.memset(ZT[:], 0.0), writes=["ZT"])
                xdv = XD.rearrange("(p i) d -> p (i d)", p=128)
                pi_ = 0
                for cbk in range(4):
                    dma("sp", UTb[:], UT[cbk * 128:(cbk + 1) * 128, :], writes=["UTb"])
                    if cbk == 0:
                        for zi in range(64):
                            dma("sp", xdv[:, zi * 4096:(zi + 1) * 4096], ZT[:], reads=["ZT"], writes=[f"XDz{zi}"])
                    for gg in range(8):
                        g = cbk * 8 + gg
                        base = min((gg // 2) * 32, 64)
                        kr = 32 if gg // 2 < 3 else 64
                        cp("dve", RB[:], RHO[:, g:g + 1].to_broadcast([128, SEG]), ["RHO"], ["RB"])
                        ts("dve", UB[:], IOT[:], PHf[:, g:g + 1], None, ALU.mult, None, ["IOT", "PHf"], ["UB"])
                        for seg in range(NSEG):
                            ownseg = seg >= NSEG - 2048 // SEG
                            act(U_[:], UB[:], AF.Identity, ["UB", "OFFT"], ["U_"], bias=OFFT[:, seg, g:g + 1])
                            act(KK[:], U_[:], AF.Identity, ["U_", "CST"], ["KK"], bias=MAGICC)
                            stt("dve", NF[:], KK[:], -MAGIC, U_[:], ALU.add, ALU.subtract, ["KK", "U_"], ["NF"])
                            act(AB[:], NF[:], AF.Abs, ["NF"], ["AB"])
                            act(COS2[:], AB[:], AF.Sin, ["AB", "CST"], ["COS2"], scale=N2PI, bias=HALFPI)
                            act(SINS[:], NF[:], AF.Sin, ["NF", "CST"], ["SINS"], scale=SC1)
                            if ownseg:
                                act(SIN2[:], NF[:], AF.Sin, ["NF", "CST"], ["SIN2"], scale=N2PI)
                            for ch in range(SEG // 512):
                                tok0 = seg * SEG + ch * 512
                                q1 = p1[pi_ % 2]; q2 = p2[pi_ % 2]; k1 = f"p1{pi_ % 2}"; k2 = f"p2{pi_ % 2}"
                                pi_ += 1
                                mm(q1[:], BL1b[base:base + kr, g, :], UTb[base:base + kr, tok0:tok0 + 512], True, True,
                                   ["BL1b", "UTb"], [k1])
                                mm(q2[:], BL2b[base:base + kr, g, :], UTb[base:base + kr, tok0:tok0 + 512], True, True,
                                   ["BL2b", "UTb"], [k2])
                                wv = W[:, ch * 512:(ch + 1) * 512]
                                tt("dve", wv, q1[:], COS2[:, ch * 512:(ch + 1) * 512], ALU.mult, [k1, "COS2"], ["W"])
                                tt("dve", m2[:], q2[:], SINS[:, ch * 512:(ch + 1) * 512], ALU.mult, [k2, "SINS"], ["m2"])
                                tt("dve", wv, wv, m2[:], ALU.add, ["W", "m2"], ["W"])
                            op("dve", lambda e: e.tensor_tensor_scan(out=Z[:], data0=RB[:], data1=W[:],
                                                                      initial=CAR[:, g:g + 1], op0=ALU.mult, op1=ALU.add),
                               reads=["RB", "W", "CAR"], writes=["Z"])
                            cp("dve", CAR[:, g:g + 1], Z[:, SEG - 1:SEG], ["Z"], ["CAR"])
                            if ownseg:
                                tt("dve", A1b[:], Z[:], COS2[:], ALU.mult, ["Z", "COS2"], ["A1b"])
                                tt("dve", A2b[:], Z[:], SIN2[:], ALU.mult, ["Z", "SIN2"], ["A2b"])
                                so = seg - (NSEG - 2048 // SEG)
                                for ch in range(SEG // 512):
                                    yi = so * (SEG // 512) + ch
                                    mm(py[yi][:], LA[:, g, :], A1b[:, ch * 512:(ch + 1) * 512], gg == 0, False,
                                       ["LA", "A1b"], [f"py{yi}"])
                                    mm(py[yi][:], LB[:, g, :], A2b[:, ch * 512:(ch + 1) * 512], False, gg == 7,
                                       ["LB", "A2b"], [f"py{yi}"])
                    for yi in range(4):
                        stt("dve", Y[:, cbk, yi * 512:(yi + 1) * 512], UTb[:, 6144 + yi * 512:6144 + (yi + 1) * 512],
                            DSK[:, cbk:cbk + 1], py[yi][:], ALU.mult, ALU.add, ["UTb", "DSK", f"py{yi}"], ["Y"])
                T.barrier()

            with ExitStack() as e2:
                def sb2(name, shape, dt=F32):
                    return e2.enter_context(nc.sbuf_tensor(_u(name), shape, dt))
                def ps2(name, shape, dt=F32):
                    return e2.enter_context(nc.psum_tensor(_u(name), shape, dt))
                WGb = sb2("WGb", [128, 4, 512], BF16)
                t1 = sb2("gt1", [128, 2048]); t2 = sb2("gt2", [128, 2048])
                YGb = sb2("YGb", [128, 4, 2048], BF16)
                sgl = sb2("sgl", [128, 512]); ysb = [sb2(f"ysb{i}", [128, 512], BF16) for i in range(2)]
                pg = [ps2(f"pgl{i}", [128, 512]) for i in range(2)]
                dma("pool", WGb[:], wglu_d.rearrange("(k p) n -> p k n", p=128), writes=["WGb"])
                for cbk in range(4):
                    yv = Y[:, cbk, :]
                    act(t1[:], yv, AF.Square, ["Y"], ["t1"])
                    ts("dve", t1[:], t1[:], 0.044715, 1.0, ALU.mult, ALU.add, ["t1"], ["t1"])
                    tt("dve", t1[:], t1[:], yv, ALU.mult, ["t1", "Y"], ["t1"])
                    act(t2[:], t1[:], AF.Sigmoid, ["t1"], ["t2"], scale=1.5957691216057308)
                    tt("dve", yv, yv, t2[:], ALU.mult, ["Y", "t2"], ["Y"])
                    act(YGb[:, cbk, :], yv, AF.Copy, ["Y"], ["YGb"])
                i = 0
                for cbo in range(4):
                    for ch in range(4):
                        p = pg[i % 2]; kp = f"pgl{i % 2}"; o = ysb[i % 2]; ko = f"ysb{i % 2}"
                        i += 1
                        for kc in range(4):
                            mm(p[:], WGb[:, kc, cbo * 128:(cbo + 1) * 128], YGb[:, kc, ch * 512:(ch + 1) * 512],
                               kc == 0, kc == 3, ["WGb", "YGb"], [kp])
                        act(sgl[:], p[:], AF.Sigmoid, [kp], ["sgl"])
                        tt("dve", o[:], Y[:, cbo, ch * 512:(ch + 1) * 512], sgl[:], ALU.mult, ["Y", "sgl"], [ko])
                        dma("sp", YST[cbo * 128:(cbo + 1) * 128, ch * 512:(ch + 1) * 512], o[:], reads=[ko])
                T.barrier()

        with ExitStack() as es:
            def sb(name, shape, dt=F32):
                return es.enter_context(nc.sbuf_tensor(_u(name), shape, dt))
            def ps(name, shape, dt=F32):
                return es.enter_context(nc.psum_tensor(_u(name), shape, dt))
            KA = sb("KA", [128, 8192], BF16)
            VA = sb("VA", [128, 64, 65], BF16)
            QA = sb("QA", [128, 2048], BF16)
            DM = sb("DM", [128, 4, 512], BF16)
            VB = sb("VB", [128, 8, 96]); VVd = sb("VVd", [128, 8, 96]); VO = sb("VO", [128, 8, 96])
            KM = sb("KM", [64, 32]); KMb = sb("KMb", [64, 32], BF16)
            gt_ = sb("gt", [128, 96]); top8 = sb("top8", [128, 8]); sel = sb("sel", [128, 96])
            pt = [sb(f"pt{i}", [128, 512], BF16) for i in range(3)]
            rrow = sb("rrow", [128, 512]); rhi = sb("rhi", [128, 512], BF16); rlo = sb("rlo", [128, 512], BF16)
            rtmp = sb("rtmp", [128, 512])
            onesb = sb("onesb", [128, 64], BF16)
            bc = sb("bc", [64, 512]); oab = sb("oab", [64, 512], BF16)
            pS = [ps(f"pS{i}", [128, 512]) for i in range(3)]
            pO = [ps(f"pO{i}", [128, 512]) for i in range(2)]
            pG = ps("pG", [128, 96]); pB = ps("pB", [128, 128]); pO2 = ps("pO2", [64, 512])
            dma("sp", KA[64:96, :], bonehot_d, writes=["KAoh"])
            dma("sp", DM[:].rearrange("p a n -> p (a n)"), dm_d, writes=["DM"])
            dma("pool", VB[:].rearrange("p a n -> p (a n)"), vb_d.to_broadcast([128, 768]), writes=["VB"])
            dma("pool", VVd[:].rearrange("p a n -> p (a n)"), vv_d.to_broadcast([128, 768]), writes=["VVd"])
            dma("pool", VO[:].rearrange("p a n -> p (a n)"), vo_d.to_broadcast([128, 768]), writes=["VO"])
            op("pool", lambda e: e.memset(VA[:, :, 64:65], 1.0), writes=["VA1"])
            op("pool", lambda e: e.memset(onesb[:], 1.0), writes=["onesb"])
            op("pool", lambda e: e.memset(gt_[:], 0.0), writes=["gt"])
            itb = [0]
            for h in range(8):
                dma("sp", KA[0:64, :], KT[h * 64:(h + 1) * 64, :], writes=["KA"])
                dma("sp", QA[0:64, :], QT[h * 64:(h + 1) * 64, :], writes=["QA"])
                dma("pool", VA[:, :, 0:64], VV[:, h * 64:(h + 1) * 64].rearrange("(t p) d -> p t d", p=128),
                    writes=["VA"])
                op("dve", lambda e: e.tensor_reduce(out=KM[:], in_=KA[0:64, :].rearrange("p (n l) -> p n l", l=256),
                                                    axis=AX.X, op=ALU.add), reads=["KA"], writes=["KM"])
                cp("dve", KMb[:], KM[:], ["KM"], ["KMb"])
                for qt in range(16):
                    qb = qt // 2
                    mm(pG[:, 64:96], QA[0:64, qt * 128:(qt + 1) * 128], KMb[:], True, True, ["QA", "KMb"], ["pG"])
                    tt("dve", gt_[:, 64:96], pG[:, 64:96], VB[:, qb, 64:96], ALU.add, ["pG", "VB"], ["gt"])
                    op("dve", lambda e: e.max(out=top8[:], in_=gt_[:, 64:96]), reads=["gt"], writes=["top8"])
                    ts("dve", sel[:, 64:96], gt_[:, 64:96], top8[:, 2:3], None, ALU.is_ge, None, ["gt", "top8"], ["sel"])
                    tt("dve", sel[:, 64:96], sel[:, 64:96], VVd[:, qb, 64:96], ALU.mult, ["sel", "VVd"], ["sel"])
                    tt("dve", sel[:, 64:96], sel[:, 64:96], VO[:, qb, 64:96], ALU.add, ["sel", "VO"], ["sel"])
                    ts("dve", gt_[:, 64:96], sel[:, 64:96], -1.0, -NEG, ALU.add, ALU.mult, ["sel"], ["gt"])
                    op("pe", lambda e: e.transpose(out=pB[0:96, :], in_=gt_[:, 0:96], identity=ident[:]),
                       reads=["gt", "ident"], writes=["pB"])
                    cp("dve", QA[64:96, qt * 128:(qt + 1) * 128], pB[64:96, :], ["pB"], ["QAb"])
                for G in range(4):
                    cnt = 52 + 4 * G
                    po = pO[G % 2]; kpo = f"pO{G % 2}"

                    def qk(kt):
                        i3 = (itb[0] + kt) % 3
                        mm(pS[i3][:], KA[0:96, kt * 128:(kt + 1) * 128], QA[0:96, G * 512:(G + 1) * 512], True, True,
                           ["KA", "KAoh", "QA", "QAb"], [f"pS{i3}"])
                    qk(0)
                    qk(1)
                    for kt in range(cnt):
                        i3 = (itb[0] + kt) % 3
                        s_ = pS[i3]; ks = f"pS{i3}"; p_ = pt[i3]; kp = f"pt{i3}"
                        if kt + 2 < cnt:
                            qk(kt + 2)
                        act(p_[:], s_[:], AF.Exp, [ks], [kp])
                        di = kt - (cnt - 4)
                        if di >= 0:
                            tt("dve", p_[:], p_[:], DM[:, di, :], ALU.mult, [kp, "DM"], [kp])
                        mm(po[0:65, :], VA[:, kt, 0:65], p_[:], kt == 0, kt == cnt - 1, ["VA", "VA1", kp], [kpo])
                    itb[0] += cnt
                    cp("dve", rrow[64:65, :], po[64:65, :], [kpo], ["rrow"])
                    op("dve", lambda e: e.reciprocal(out=rrow[64:65, :], in_=rrow[64:65, :]), reads=["rrow"], writes=["rrow"])
                    cp("dve", rhi[64:65, :], rrow[64:65, :], ["rrow"], ["rhi"])
                    cp("dve", rtmp[64:65, :], rhi[64:65, :], ["rhi"], ["rtmp"])
                    tt("dve", rtmp[64:65, :], rrow[64:65, :], rtmp[64:65, :], ALU.subtract, ["rrow", "rtmp"], ["rtmp"])
                    cp("dve", rlo[64:65, :], rtmp[64:65, :], ["rtmp"], ["rlo"])
                    mm(pO2[:], onesb[64:65, 0:64], rhi[64:65, :], True, False, ["onesb", "rhi"], ["pO2"])
                    mm(pO2[:], onesb[64:65, 0:64], rlo[64:65, :], False, True, ["onesb", "rlo"], ["pO2"])
                    cp("dve", bc[:], pO2[:], ["pO2"], ["bc"])
                    tt("dve", oab[:], po[0:64, :], bc[:], ALU.mult, [kpo, "bc"], ["oab"])
                    dma("sp", OAT[h * 64:(h + 1) * 64, G * 512:(G + 1) * 512], oab[:], reads=["oab"])
            T.barrier()

        with ExitStack() as es:
            def sb(name, shape, dt=F32):
                return es.enter_context(nc.sbuf_tensor(_u(name), shape, dt))
            def ps(name, shape, dt=F32):
                return es.enter_context(nc.psum_tensor(_u(name), shape, dt))
            Wsb = sb("Wsb", [128, 4, 1024], BF16); Wab = sb("Wab", [128, 4, 1024], BF16)
            Wo = sb("Wo", [128, 8, 1024], BF16)
            ys = sb("ys", [128, 4, 512], BF16); oa = sb("oa", [128, 4, 512], BF16)
            gsT = sb("gsT", [128, 8, 512], BF16); gaT = sb("gaT", [128, 8, 512], BF16)
            MT = sb("MT", [128, 8, 512], BF16)
            b1 = sb("b1", [128, 512]); b2 = sb("b2", [128, 512])
            xt = sb("xt2", [128, 1024]); x1 = sb("x1", [128, 1024]); tmp = sb("tmpE", [128, 1024]); tmp2 = sb("tmp2E", [128, 1024])
            sq = sb("sqE", [128, 1024], BF16); ss = sb("ssE", [128, 1]); ss2 = sb("ss2E", [128, 1]); rs = sb("rsE", [128, 1])
            hbs = [sb(f"hbE{i}", [128, 1024], BF16) for i in range(2)]; h2T = sb("h2T", [128, 8, 512], BF16)
            RWb = sb("RWb", [128, 8, 32], BF16); RBb = sb("RBb", [128, 32]); LT = sb("LT", [128, 128], BF16)
            ONESb = sb("ONESb", [128, 128], BF16); CNT = sb("CNT", [128, 32]); EOFF = sb("EOFF", [128, 32])
            mb = sb("mb", [128, 32], BF16); posf = sb("posfE", [128, 32]); idxf = sb("idxf", [128, 32])
            lg = sb("lg", [128, 32]); top8 = sb("top8F", [128, 8]); msk = sb("msk", [128, 32]); ex = sb("ex", [128, 32])
            nmx = sb("nmx", [128, 1]); sm = sb("sm", [128, 1])
            pl = ps("pl", [128, 32]); pp = ps("pp", [128, 32]); pc = ps("pc", [128, 32])
            dma("pool", RWb[:], rw_d.rearrange("(k p) n -> p k n", p=128), writes=["RWb"])
            dma("pool", RBb[:], rb_d.to_broadcast([128, 32]), writes=["RBb"])
            dma("pool", EOFF[:], eoff_d.to_broadcast([128, 32]), writes=["EOFF"])
            dma("sp", LT[:], ltri_d, writes=["LT"])
            op("pool", lambda e: e.memset(ONESb[:], 1.0), writes=["ONESb"])
            op("pool", lambda e: e.memset(CNT[:], 0.0), writes=["CNT"])
            pb1 = ps("pb1", [128, 512]); pb2 = ps("pb2", [128, 512])
            pmx = ps("pmx", [128, 1024]); pT = ps("pTE", [128, 8, 128], BF16)
            for (src, dst, kd, nk) in ((wsb_d, Wsb, "Wsb", 4), (wab_d, Wab, "Wab", 4), (wout_d, Wo, "Wo", 8)):
                dma("pool", dst[:], src.rearrange("(k p) n -> p k n", p=128), writes=[kd])
            for c in range(4):
                cs_ = slice(c * 512, (c + 1) * 512)
                dma("sp", ys[:], YST[:, cs_].rearrange("(k p) n -> p k n", p=128), writes=["ys"])
                dma("sp", oa[:], OAT[:, cs_].rearrange("(k p) n -> p k n", p=128), writes=["oa"])
                dma("pool", gsT[:], GST[:, cs_].rearrange("(k p) n -> p k n", p=128), writes=["gsT"])
                dma("pool", gaT[:], GAT[:, cs_].rearrange("(k p) n -> p k n", p=128), writes=["gaT"])
                for db in range(8):
                    for kc in range(4):
                        mm(pb1[:], Wsb[:, kc, db * 128:(db + 1) * 128], ys[:, kc, :], kc == 0, kc == 3, ["Wsb", "ys"], ["pb1"])
                    for kc in range(4):
                        mm(pb2[:], Wab[:, kc, db * 128:(db + 1) * 128], oa[:, kc, :], kc == 0, kc == 3, ["Wab", "oa"], ["pb2"])
                    tt("dve", b1[:], pb1[:], gsT[:, db, :], ALU.mult, ["pb1", "gsT"], ["b1"])
                    tt("dve", b2[:], pb2[:], gaT[:, db, :], ALU.mult, ["pb2", "gaT"], ["b2"])
                    tt("dve", MT[:, db, :], b1[:], b2[:], ALU.add, ["b1", "b2"], ["MT"])
                for j in range(4):
                    t = 4 * c + j
                    for half in range(2):
                        for kc in range(8):
                            mm(pmx[:, half * 512:(half + 1) * 512], MT[:, kc, j * 128:(j + 1) * 128],
                               Wo[:, kc, half * 512:(half + 1) * 512], kc == 0, kc == 7, ["MT", "Wo"], ["pmx"])
                    dma("sp", xt[:], x_loc[6144 + t * 128:6144 + (t + 1) * 128, :], writes=["xt"])
                    act(sq[:, 0:512], pmx[:, 0:512], AF.Square, ["pmx"], ["sq", "ss"], accum_out=ss[:])
                    act(sq[:, 512:1024], pmx[:, 512:1024], AF.Square, ["pmx"], ["sq", "ss2"], accum_out=ss2[:])
                    tt("dve", ss[:], ss[:], ss2[:], ALU.add, ["ss", "ss2"], ["ss"])
                    ts("dve", rs[:], ss[:], 1.0 / 1024, 1e-6, ALU.mult, ALU.add, ["ss"], ["rs"])
                    act(rs[:], rs[:], AF.Sqrt, ["rs"], ["rs"])
                    op("dve", lambda e: e.reciprocal(out=rs[:], in_=rs[:]), reads=["rs"], writes=["rs"])
                    cp("dve", tmp[:], pmx[:], ["pmx"], ["tmp"])
                    stt("dve", tmp[:], tmp[:], rs[:, 0:1], PRM[:, 2, :], ALU.mult, ALU.mult, ["tmp", "rs", "PRM"], ["tmp"])
                    tt("dve", x1[:], tmp[:], xt[:], ALU.add, ["tmp", "xt"], ["x1"])
                    dma("sp", X1[t * 128:(t + 1) * 128, :], x1[:], reads=["x1"])
                    hb = hbs[t % 2]; khb = f"hb{t % 2}"
                    norm_mod(sb, x1[:], "x1", 3, 4, None, hb[:], khb, tmp, tmp2, ss, rs, sq)
                    for kc in range(8):
                        op("pe", lambda e: e.transpose(out=pT[:, kc, :], in_=hb[:, kc * 128:(kc + 1) * 128],
                                                       identity=identb[:]),
                           reads=[khb, "identb"], writes=["pT"])
                    cp("dve", h2T[:, :, j * 128:(j + 1) * 128], pT[:], ["pT"], ["h2T"])
                    for kc in range(8):
                        mm(pl[:], h2T[:, kc, j * 128:(j + 1) * 128], RWb[:, kc, :], kc == 0, kc == 7, ["h2T", "RWb"], ["pl"])
                    tt("dve", lg[:], pl[:], RBb[:], ALU.add, ["pl", "RBb"], ["lg"])
                    op("dve", lambda e: e.max(out=top8[:], in_=lg[:]), reads=["lg"], writes=["top8"])
                    ts("dve", msk[:], lg[:], top8[:, 3:4], None, ALU.is_ge, None, ["lg", "top8"], ["msk"])
                    ts("dve", nmx[:], top8[:, 0:1], -1.0, None, ALU.mult, None, ["top8"], ["nmx"])
                    act(ex[:], lg[:], AF.Exp, ["lg", "nmx"], ["ex"], bias=nmx[:, 0:1])
                    tt("dve", ex[:], ex[:], msk[:], ALU.mult, ["ex", "msk"], ["ex"])
                    op("dve", lambda e: e.reduce_sum(out=sm[:], in_=ex[:], axis=AX.X), reads=["ex"], writes=["sm"])
                    op("dve", lambda e: e.reciprocal(out=sm[:], in_=sm[:]), reads=["sm"], writes=["sm"])
                    ts("dve", RWT[:, t, :], ex[:], sm[:, 0:1], None, ALU.mult, None, ["ex", "sm"], ["RWT"])
                    cp("dve", mb[:], msk[:], ["msk"], ["mb"])
                    mm(pp[:], LT[:], mb[:], True, True, ["LT", "mb"], ["pp"])
                    mm(pc[:], ONESb[:], mb[:], True, True, ["ONESb", "mb"], ["pc"])
                    tt("dve", posf[:], pp[:], CNT[:], ALU.add, ["pp", "CNT"], ["posf"])
                    tt("dve", CNT[:], CNT[:], pc[:], ALU.add, ["CNT", "pc"], ["CNT"])
                    ts("dve", idxf[:], posf[:], float(CAP), None, ALU.is_lt, None, ["posf"], ["idxf"])
                    tt("dve", msk[:], msk[:], idxf[:], ALU.mult, ["msk", "idxf"], ["msk"])
                    tt("dve", idxf[:], posf[:], EOFF[:], ALU.add, ["posf", "EOFF"], ["idxf"])
                    tt("dve", idxf[:], idxf[:], msk[:], ALU.mult, ["idxf", "msk"], ["idxf"])
                    ts("dve", idxf[:], idxf[:], 40000.0, None, ALU.add, None, ["idxf"], ["idxf"])
                    cp("dve", IDX[:, t * 32:(t + 1) * 32], idxf[:], ["idxf"], [f"IDX{t}"])
                    for e_ in range(32):
                        T.dma_fn("pool", lambda g, e_=e_, hb=hb: g.indirect_dma_start(
                            out=XD[:, :], out_offset=bass.IndirectOffsetOnAxis(ap=IDX[:, t * 32 + e_:t * 32 + e_ + 1], axis=0),
                            in_=hb[:, :], in_offset=None, bounds_check=BCREG, oob_is_err=False),
                            reads=[khb, f"IDX{t}"], writes=[f"XD{e_}"])
                dma("sp", H2T[:, cs_].rearrange("(k p) n -> p k n", p=128), h2T[:], reads=["h2T"])
            T.barrier()

        aes.close()
        with ExitStack() as es:
            def sb(name, shape, dt=F32):
                return es.enter_context(nc.sbuf_tensor(_u(name), shape, dt))
            def ps(name, shape, dt=F32):
                return es.enter_context(nc.psum_tensor(_u(name), shape, dt))
            acc = sb("acc", [128, 16, 1024])
            Wg = [sb(f"Wg{i}", [128, 8, 1024], BF16) for i in range(2)]
            Wu = [sb(f"Wu{i}", [128, 8, 1024], BF16) for i in range(2)]
            Wds = [sb(f"Wd{i}", [128, 8, 1024], BF16) for i in range(2)]
            XT1 = sb("XT0", [128, 8, 512], BF16)
            XT = [XT1, XT1]
            xs = [sb(f"xs{i}", [128, 1024], BF16) for i in range(2)]
            BG = sb("BG", [128, 32, 8]); BU = sb("BU", [128, 32, 8])
            BD = sb("BD", [128, 1024], BF16)
            aT = sb("aT", [128, 8, 512], BF16)
            gb = [sb("g_0", [128, 512])] * 2; ub = [sb("u_0", [128, 512])] * 2
            sb_ = [sb("s_0", [128, 512])] * 2; u0b = [sb("u0_0", [128, 512])] * 2
            yo = [sb(f"yo{i}", [128, 512], BF16) for i in range(2)]
            tg = [sb(f"tg{i}", [128, 1024], BF16) for i in range(2)]
            ssF = sb("ssF", [128, 1]); ss2F = sb("ss2F", [128, 1])
            pgt = [ps(f"pgt{i}", [128, 512]) for i in range(2)]
            put = [ps(f"put{i}", [128, 512]) for i in range(2)]
            pdn = [ps(f"pdn{i}", [128, 512]) for i in range(2)]
            pT = ps("pTF", [128, 8, 128], BF16)
            dma("sp", BG[:].rearrange("p e k -> p (e k)"), bg_d, writes=["BG"])
            dma("sp", BU[:].rearrange("p e k -> p (e k)"), bu_d, writes=["BU"])
            op("pool", lambda e: e.memset(acc[:], 0.0), writes=["acc"])
            for i in range(2):
                op("pool", lambda e, i=i: e.memset(tg[i][:], 0.0), writes=[f"tg{i}"])

            def load_gu(e):
                sl = e % 2
                dma("pool", Wg[sl][:], wg_d[e].rearrange("(k p) n -> p k n", p=128), writes=[f"Wg{sl}"])
                dma("pool", Wu[sl][:], wu_d[e].rearrange("(k p) n -> p k n", p=128), writes=[f"Wu{sl}"])

            def load_d(e):
                dma("pool", Wds[e % 2][:], wd_d[e].rearrange("(k p) n -> p k n", p=128), writes=[f"Wd{e % 2}"])

            def load_bd(e):
                dma("pool", BD[:], bd_d[e:e + 1, :].to_broadcast([128, 1024]), writes=["BD"])

            xi = [0]

            def build_xt(e, c):
                for st in range(4):
                    x_ = xs[xi[0] % 2]; kx = f"xs{xi[0] % 2}"
                    xi[0] += 1
                    r0 = e * CAP + c * 512 + st * 128
                    dma("sp", x_[:], XD[r0:r0 + 128, :], writes=[kx])
                    for kc in range(8):
                        op("pe", lambda en: en.transpose(out=pT[:, kc, :], in_=x_[:, kc * 128:(kc + 1) * 128],
                                                        identity=identb[:]),
                           reads=[kx, "identb"], writes=["pTF"])
                    act(XT[c][:, :, st * 128:(st + 1) * 128], pT[:], AF.Copy, ["pTF"], ["XT0"])

            def GU(e, c):
                sl = e % 2
                h_ = XT[c]; kh = "XT0"

                def tail(fc):
                    i2 = 0
                    ts("dve", ub[i2][:], u0b[i2][:], -7.0, 7.0, ALU.max, ALU.min, [f"u0_{i2}"], [f"u_{i2}"])
                    stt("dve", aT[:, fc, :], ub[i2][:], 1.0, sb_[i2][:], ALU.add, ALU.mult,
                        [f"u_{i2}", f"s_{i2}"], ["aT"])

                for fc in range(8):
                    i2 = fc % 2
                    pg_ = pgt[i2]; kg = f"pgt{i2}"; pu_ = put[i2]; ku = f"put{i2}"
                    for kc in range(8):
                        mm(pg_[:], Wg[sl][:, kc, fc * 128:(fc + 1) * 128], h_[:, kc, :], kc == 0, kc == 7,
                           [f"Wg{sl}", kh], [kg])
                    for kc in range(8):
                        mm(pu_[:], Wu[sl][:, kc, fc * 128:(fc + 1) * 128], h_[:, kc, :], kc == 0, kc == 7,
                           [f"Wu{sl}", kh], [ku])
                    ts("dve", gb[0][:], pg_[:], BG[:, e, fc:fc + 1], 7.0, ALU.add, ALU.min, [kg, "BG"], ["g_0"])
                    act(u0b[0][:], pu_[:], AF.Identity, [ku, "BU"], ["u0_0"], bias=BU[:, e, fc:fc + 1])
                    act(sb_[0][:], gb[0][:], AF.Silu, ["g_0"], ["s_0"], scale=1.702)
                    tail(fc)
                    combine_step()

            yi = [0]

            def DN(e, c):
                for j in range(4):
                    r0 = e * CAP + c * 512 + j * 128
                    for half in range(2):
                        pd_ = pdn[half]; kd = f"pdn{half}"
                        for fc in range(8):
                            mm(pd_[:], aT[:, fc, j * 128:(j + 1) * 128], Wds[e % 2][:, fc, half * 512:(half + 1) * 512],
                               fc == 0, fc == 7, ["aT", f"Wd{e % 2}"], [kd])
                        y_ = yo[yi[0] % 2]; ky = f"yo{yi[0] % 2}"
                        yi[0] += 1
                        stt("dve", y_[:], pd_[:], 1.0 / 1.702, BD[:, half * 512:(half + 1) * 512], ALU.mult, ALU.add,
                            [kd, "BD"], [ky])
                        dma("sp", YD[r0:r0 + 128, half * 512:(half + 1) * 512], y_[:], reads=[ky],
                            writes=[f"YD{e}_{c * 8 + j * 2 + half}"])

            gi = [0]
            pend_g = []
            pend_a = []

            def combine(e):
                for i in range(16):
                    pend_g.append((e, i))

            def combine_step():
                if pend_a:
                    e, i, k = pend_a.pop(0)
                    stt("dve", acc[:, i, :], tg[k][:], RWT[:, i, e:e + 1], acc[:, i, :], ALU.mult, ALU.add,
                        [f"tg{k}", "acc"], ["acc"])
                if pend_g:
                    e, i = pend_g.pop(0)
                    k = gi[0] % 2
                    gi[0] += 1
                    t_ = tg[k]
                    T.dma_fn("pool", lambda g, t_=t_, i=i, e=e: g.indirect_dma_start(
                        out=t_[:, :], out_offset=None, in_=YD[:, :],
                        in_offset=bass.IndirectOffsetOnAxis(ap=IDX[:, i * 32 + e:i * 32 + e + 1], axis=0),
                        bounds_check=BCREG, oob_is_err=False),
                        reads=[f"YD{e}_{m}" for m in range(16)], writes=[f"tg{k}"])
                    pend_a.append((e, i, k))

            load_gu(0)
            load_d(0)
            load_bd(0)
            load_gu(1)
            load_d(1)
            build_xt(0, 0)
            for e in range(32):
                GU(e, 0)
                build_xt(e, 1)
                DN(e, 0)
                GU(e, 1)
                if e + 2 < 32:
                    load_gu(e + 2)
                if e + 1 < 32:
                    build_xt(e + 1, 0)
                DN(e, 1)
                if e + 2 < 32:
                    load_d(e + 2)
                if e + 1 < 32:
                    load_bd(e + 1)
                combine(e)
            while pend_g or pend_a:
                combine_step()
            T.barrier()
            xv = Wg[0][:].rearrange("p k n -> p (k n)").bitcast(F32)
            sqv = Wu[0][:].rearrange("p k n -> p (k n)")
            for t in range(16):
                x1v = xv[:, (t % 2) * 2048:(t % 2) * 2048 + 1024]; ov = xv[:, (t % 2) * 2048 + 1024:(t % 2) * 2048 + 2048]
                kx = f"x1v{t % 2}"; ko = f"ov{t % 2}"
                dma("sp", x1v, X1[t * 128:(t + 1) * 128, :], writes=[kx])
                act(sqv[:, 0:1024], acc[:, t, :], AF.Square, ["acc"], ["sqv", "ssF"], accum_out=ssF[:])
                ts("dve", ss2F[:], ssF[:], 1.0 / 1024, 1e-6, ALU.mult, ALU.add, ["ssF"], ["ss2F"])
                act(ss2F[:], ss2F[:], AF.Sqrt, ["ss2F"], ["ss2F"])
                op("dve", lambda e: e.reciprocal(out=ss2F[:], in_=ss2F[:]), reads=["ss2F"], writes=["ss2F"])
                stt("dve", ov, acc[:, t, :], ss2F[:, 0:1], G2[:], ALU.mult, ALU.mult, ["acc", "ss2F", "G2"], [ko])
                tt("dve", ov, ov, x1v, ALU.add, [ko, kx], [ko])
                dma("sp", out_d[t * 128:(t + 1) * 128, :], ov, reads=[ko])
            T.finish("sp")
    return nc


_NC = None


def _bf16(a):
    return a.astype(ml_dtypes.bfloat16)


def kernel(**inp):
    global _NC
    f32 = np.float32
    x = np.asarray(inp["x"], f32)
    c = np.asarray(inp["c"], f32)
    positions = np.asarray(inp["positions"]).astype(np.int32)
    sq = lambda k: np.asarray(inp[k])[0]
    if _NC is None:
        _NC = build_program()
    nc = _NC
    gains = np.concatenate([sq("mix_pre_g"), sq("mix_post_g"), sq("ffn_pre_g"), sq("ffn_post_g")])[None, :].astype(f32)
    p = np.arange(128)
    inv_freq = (10000.0 ** (-np.arange(32, dtype=np.float32) / np.float32(32))).astype(f32)
    cst = np.zeros((128, 16), f32)
    sgn_r = np.where((p % 64) < 32, -1.0, 1.0).astype(f32)
    shalf = np.where(p < 64, 1.0, -1.0).astype(f32)
    cst[:, 1] = sgn_r
    cst[:, 2] = np.pi / 2
    cst[:, 3] = inv_freq[p % 32]
    cst[:, 4] = (p < 64)
    cst[:, 5] = -(p >= 64).astype(f32)
    cst[:, 6] = shalf * 0.999999
    cst[:, 7] = -1.0
    cst[:, 8] = -(p < 64).astype(f32)
    cst[:, 9] = 1.0
    cst[:, 10] = -TWO_PI_LO
    cst[:, 11] = -TWO_PI_LO * shalf
    cst[:, 13] = 12582912.0
    cst[:, 12] = shalf
    ident = np.eye(128, dtype=f32)
    iota = np.arange(1024, dtype=f32)[None, :]
    lre = np.concatenate([sq("ssm_lam_re").T, sq("ssm_lam_re").T], 0).astype(f32)
    lim = np.concatenate([sq("ssm_lam_im").T, sq("ssm_lam_im").T], 0).astype(f32)
    ldt = sq("ssm_log_dt")[None, :].astype(f32)
    bre = sq("ssm_b_re"); bim = sq("ssm_b_im")
    bl1 = np.zeros((128, 32, 128), f32); bl2 = np.zeros((128, 32, 128), f32)
    for g in range(32):
        r0 = ((g % 8) // 2) * 32 + (g % 2) * 16
        bl1[r0:r0 + 16, g, 0:64] = bre[g].T; bl1[r0:r0 + 16, g, 64:128] = bim[g].T
        bl2[r0:r0 + 16, g, 0:64] = bim[g].T; bl2[r0:r0 + 16, g, 64:128] = bre[g].T
    cre = sq("ssm_c_re"); cim = sq("ssm_c_im")
    crp = np.transpose(cre, (2, 0, 1)); cip = np.transpose(cim, (2, 0, 1))
    cr = np.concatenate([crp, crp], 0).reshape(128, 512).astype(f32)
    ci = np.concatenate([cip, cip], 0).reshape(128, 512).astype(f32)
    dsk = sq("ssm_d").reshape(4, 128).T.copy().astype(f32)
    bonehot = np.zeros((32, 8192), f32)
    for n in range(32):
        bonehot[n, n * 256:(n + 1) * 256] = 1.0
    bonehot = _bf16(bonehot)
    dmask = np.ones((128, 4, 512), f32)
    kk = np.arange(128)[:, None]; qq = np.arange(128)[None, :]
    for i in range(4):
        for j in range(4):
            if i // 2 == j // 2:
                if i == j:
                    dmask[:, i, j * 128:(j + 1) * 128] = (kk <= qq)
                elif i > j:
                    dmask[:, i, j * 128:(j + 1) * 128] = 0.0
    dmask = _bf16(dmask.reshape(128, 2048))
    bg = np.transpose(sq("b_gate").reshape(32, 8, 128), (2, 0, 1)).reshape(128, 256).astype(f32)
    bu = np.transpose(sq("b_up").reshape(32, 8, 128), (2, 0, 1)).reshape(128, 256).astype(f32)
    ltri = _bf16((np.arange(128)[:, None] < np.arange(128)[None, :]).astype(f32))
    eoff = (np.arange(32, dtype=f32) * 1024.0 - 40000.0)[None, :]
    shared = {
        "ltri": ltri, "eoff": eoff,
        "ada_w": sq("ada_w"), "ada_b": sq("ada_b")[None, :], "gains": gains, "w_in": sq("w_in"),
        "cst": cst, "ident": ident, "iota": iota, "lre": lre, "lim": lim, "ldt": ldt,
        "bl1": bl1.reshape(128, 4096), "bl2": bl2.reshape(128, 4096), "cr": cr, "ci": ci, "dsk": dsk,
        "w_glu": sq("ssm_w_glu"), "w_sb": sq("w_ssm_branch"), "w_ab": sq("w_attn_branch"), "w_out": sq("w_out"),
        "bonehot": bonehot, "dmask": dmask, "router_w": sq("router_w"), "router_b": sq("router_b")[None, :],
        "w_gate": sq("w_gate"), "w_up": sq("w_up"), "w_down": sq("w_down"),
        "b_gate": bg, "b_up": bu, "b_down": sq("b_down"),
    }
    shared = {k: np.ascontiguousarray(v) for k, v in shared.items()}
    in_maps = []
    for core in range(8):
        b, j = core // 4, core % 4
        nprev = j * 2048
        x_loc = np.zeros((8192, 1024), f32)
        x_loc[6144 - nprev:] = x[b, :nprev + 2048]
        tm = np.zeros(8192, f32); tm[6144 - nprev:] = 1.0
        pos = np.zeros(8192, np.int32); pos[6144 - nprev:] = positions[b, :nprev + 2048]
        ninv = (3 - j) * 8
        vb = np.zeros((8, 96), f32); vv = np.zeros((8, 96), f32); vo = np.zeros((8, 96), f32)
        for qb in range(8):
            n = np.arange(32)
            valid = (n >= ninv) & (n < 24 + qb)
            vb[qb, 64:96] = np.where(valid, 0.0, -1e30)
            vv[qb, 64:96] = valid
            vo[qb, 64 + 24 + qb] = 1.0
        m = dict(shared)
        m.update({
            "x_loc": x_loc, "tmask": np.ascontiguousarray(tm.reshape(64, 128).T),
            "pos_loc": pos[None, :], "c_b": np.ascontiguousarray(c[b].reshape(8, 128).T),
            "vbias": vb.reshape(1, 768), "vvalid": vv.reshape(1, 768), "vown": vo.reshape(1, 768),
        })
        in_maps.append(m)
    res = run_bass_kernel_spmd(nc, in_maps, core_ids=list(range(8)))
    out = np.zeros((2, 8192, 1024), f32)
    for core in range(8):
        b, j = core // 4, core % 4
        out[b, j * 2048:(j + 1) * 2048] = res.results[core]["out"]
    return out
```

```python
import numpy as np
from contextlib import ExitStack
import ml_dtypes
import concourse.bass as bass
import concourse.mybir as mybir
from concourse.bass_utils import run_bass_kernel_spmd

F32 = mybir.dt.float32
BF16 = mybir.dt.bfloat16
I32 = mybir.dt.int32
ALU = mybir.AluOpType
AF = mybir.ActivationFunctionType
AX = mybir.AxisListType
PI = float(np.pi)
TWO_PI = float(2 * np.pi)
PI_LO = 3.1415925
TWO_PI_LO = 6.283185
SEM_CAP = 30000
NEG = -30000.0


class Tracker:
    def __init__(self, nc):
        self.nc = nc
        self.engs = {"pe": nc.tensor, "act": nc.scalar, "dve": nc.vector,
                     "pool": nc.gpsimd, "sp": nc.sync}
        self.cur_sem = {}
        self.cnt = {}
        self.nsem = 0
        for e in ("pe", "act", "dve", "pool"):
            self._new_epoch(e)
        self.seen = {e: {} for e in self.engs}
        self.lastw = {}
        self.reads = {}
        self.NS = 8
        self.dq = {}
        for q in ("sp", "pool"):
            sems = [self._alloc(f"dq_{q}_{i}") for i in range(self.NS)]
            self.dq[q] = {"sems": sems, "i": 0, "last": {}}

    def _alloc(self, name):
        self.nsem += 1
        return self.nc.alloc_semaphore(name)

    def _new_epoch(self, e):
        self.cur_sem[e] = self._alloc(f"c_{e}_{self.nsem}")
        self.cnt[e] = 0

    def _wait(self, e, ev):
        if ev is None:
            return
        sem, val, src = ev
        if src == "pe" and e == "pe":
            return
        k = id(sem)
        old = self.seen[e].get(k)
        if old is not None and old >= val:
            return
        self.seen[e][k] = val
        self.engs[e].wait_ge(sem, val)

    def _deps(self, e, reads, writes):
        for r in reads:
            self._wait(e, self.lastw.get(r))
        for w in writes:
            self._wait(e, self.lastw.get(w))
            for ev in self.reads.get(w, ()):
                self._wait(e, ev)

    def _commit(self, ev, reads, writes):
        for r in reads:
            lst = self.reads.setdefault(r, [])
            lst.append(ev)
            if len(lst) > 24:
                d = {}
                for s, v, src in lst:
                    if id(s) not in d or d[id(s)][1] < v:
                        d[id(s)] = (s, v, src)
                self.reads[r] = list(d.values())
        for w in writes:
            self.lastw[w] = ev
            self.reads[w] = []

    def op(self, e, fn, reads=(), writes=()):
        self._deps(e, reads, writes)
        if self.cnt[e] >= SEM_CAP:
            self._new_epoch(e)
        ins = fn(self.engs[e])
        self.cnt[e] += 1
        ins.then_inc(self.cur_sem[e], 1)
        ev = (self.cur_sem[e], self.cnt[e], e)
        self._commit(ev, reads, writes)
        return ev

    def dma(self, q, out, in_, reads=(), writes=(), **kw):
        d = self.dq[q]
        i = d["i"]
        d["i"] += 1
        slot = i % self.NS
        sem = d["sems"][slot]
        prev = 16 * (i // self.NS)
        if prev + 16 > 2 * SEM_CAP:
            d["sems"] = [self._alloc(f"dq_{q}_{i}_{k}") for k in range(self.NS)]
            d["i"] = 1
            i = 0
            slot = 0
            sem = d["sems"][0]
            prev = 0
        if prev > 0:
            self._wait(q, (sem, prev, "dma"))
        self._deps(q, reads, writes)
        ins = self.engs[q].dma_start(out=out, in_=in_, **kw)
        ins.then_inc(sem, 16)
        ev = (sem, prev + 16, "dma")
        self._commit(ev, reads, writes)
        d["last"][id(sem)] = ev
        return ev

    def dma_fn(self, q, fn, reads=(), writes=()):
        d = self.dq[q]
        i = d["i"]
        d["i"] += 1
        slot = i % self.NS
        sem = d["sems"][slot]
        prev = 16 * (i // self.NS)
        if prev + 16 > 2 * SEM_CAP:
            d["sems"] = [self._alloc(f"dq_{q}_{i}_{k}") for k in range(self.NS)]
            d["i"] = 1
            slot = 0
            sem = d["sems"][0]
            prev = 0
        if prev > 0:
            self._wait(q, (sem, prev, "dma"))
        self._deps(q, reads, writes)
        ins = fn(self.engs[q])
        ins.then_inc(sem, 16)
        ev = (sem, prev + 16, "dma")
        self._commit(ev, reads, writes)
        d["last"][id(sem)] = ev
        return ev

    def _all_events(self):
        evs = []
        for e in ("pe", "act", "dve", "pool"):
            if self.cnt[e] > 0:
                evs.append((self.cur_sem[e], self.cnt[e], "bar"))
        for q, d in self.dq.items():
            for ev in d["last"].values():
                evs.append((ev[0], ev[1], "bar"))
        return evs

    def barrier(self):
        evs = self._all_events()
        for e in self.engs:
            for ev in evs:
                self._wait(e, ev)
        self.lastw.clear()
        self.reads.clear()

    def finish(self, eng="sp"):
        for ev in self._all_events():
            self._wait(eng, ev)


def build_program():
    nc = bass.Bass("TRN2", target_bir_lowering=False)

    def din(name, shape, dt=F32):
        return nc.dram_tensor(name, list(shape), dt, kind="ExternalInput").ap()

    def dscr(name, shape, dt):
        return nc.dram_tensor(name, list(shape), dt, kind="Internal").ap()

    x_loc = din("x_loc", [8192, 1024])
    tmask_d = din("tmask", [128, 64])
    pos_d = din("pos_loc", [1, 8192], I32)
    c_b = din("c_b", [128, 8])
    ada_w = din("ada_w", [1024, 6144])
    ada_b = din("ada_b", [1, 6144])
    gains = din("gains", [1, 4096])
    w_in = din("w_in", [1024, 4096])
    cst_d = din("cst", [128, 16])
    ident_d = din("ident", [128, 128])
    iota_d = din("iota", [1, 1024])
    lre_d = din("lre", [128, 32])
    lim_d = din("lim", [128, 32])
    ldt_d = din("ldt", [1, 32])
    bl1_d = din("bl1", [128, 32 * 128])
    bl2_d = din("bl2", [128, 32 * 128])
    cr_d = din("cr", [128, 512])
    ci_d = din("ci", [128, 512])
    dsk_d = din("dsk", [128, 4])
    wglu_d = din("w_glu", [512, 512])
    wsb_d = din("w_sb", [512, 1024])
    wab_d = din("w_ab", [512, 1024])
    wout_d = din("w_out", [1024, 1024])
    bonehot_d = din("bonehot", [32, 8192], BF16)
    dm_d = din("dmask", [128, 2048], BF16)
    vb_d = din("vbias", [1, 8 * 96])
    vv_d = din("vvalid", [1, 8 * 96])
    vo_d = din("vown", [1, 8 * 96])
    rw_d = din("router_w", [1024, 32])
    rb_d = din("router_b", [1, 32])
    wg_d = din("w_gate", [32, 1024, 1024])
    wu_d = din("w_up", [32, 1024, 1024])
    wd_d = din("w_down", [32, 1024, 1024])
    bg_d = din("b_gate", [128, 32 * 8])
    bu_d = din("b_up", [128, 32 * 8])
    bd_d = din("b_down", [32, 1024])
    ltri_d = din("ltri", [128, 128], BF16)
    eoff_d = din("eoff", [1, 32])
    out_d = nc.dram_tensor("out", [2048, 1024], F32, kind="ExternalOutput").ap()
    CAP = 1024
    XD = dscr("XD", [32 * CAP, 1024], BF16)
    YD = dscr("YD", [32 * CAP, 1024], BF16)

    UT = dscr("UT", [512, 8192], BF16)
    KT = dscr("KT", [512, 8192], BF16)
    VV = dscr("VV", [8192, 512], BF16)
    QT = dscr("QT", [512, 2048], BF16)
    GST = dscr("GST", [1024, 2048], BF16)
    GAT = dscr("GAT", [1024, 2048], BF16)
    YST = dscr("YST", [512, 2048], BF16)
    OAT = dscr("OAT", [512, 2048], BF16)
    X1 = dscr("X1", [2048, 1024], F32)
    H2T = dscr("H2T", [1024, 2048], BF16)

    T = Tracker(nc)
    BCREG = nc.gpsimd.to_reg(32 * 1024 - 1)
    _cnt = [0]

    def _u(n):
        _cnt[0] += 1
        return f'sb{_cnt[0]}_{n}'
    op = T.op
    dma = T.dma
    uid = [0]

    def mm(out, lhsT, rhs, start, stop, reads, writes):
        return op("pe", lambda e: e.matmul(out, lhsT=lhsT, rhs=rhs, start=start, stop=stop),
                  reads=reads, writes=writes)

    def act(out, in_, func, reads, writes, eng="act", **kw):
        return op(eng, lambda e: e.activation(out=out, in_=in_, func=func, **kw), reads=reads, writes=writes)

    def tt(eng, out, a, b, o, reads, writes):
        return op(eng, lambda e: e.tensor_tensor(out=out, in0=a, in1=b, op=o), reads=reads, writes=writes)

    def ts(eng, out, a, s1, s2, o0, o1, reads, writes):
        if o1 is None:
            return op(eng, lambda e: e.tensor_scalar(out=out, in0=a, scalar1=s1, scalar2=None, op0=o0),
                      reads=reads, writes=writes)
        return op(eng, lambda e: e.tensor_scalar(out=out, in0=a, scalar1=s1, scalar2=s2, op0=o0, op1=o1),
                  reads=reads, writes=writes)

    def stt(eng, out, a, s, b, o0, o1, reads, writes):
        return op(eng, lambda e: e.scalar_tensor_tensor(out=out, in0=a, scalar=s, in1=b, op0=o0, op1=o1),
                  reads=reads, writes=writes)

    def cp(eng, out, a, reads, writes):
        return op(eng, lambda e: e.tensor_copy(out=out, in_=a), reads=reads, writes=writes)


    MAGIC = 12582912.0
    INV2PI = float(1.0 / (2 * np.pi))

    def reduce_angle(x, kx, k, kk, r, kr, ab, kab):
        ts("dve", k, x, INV2PI, MAGIC, ALU.mult, ALU.add, [kx], [kk])
        ts("dve", k, k, -MAGIC, None, ALU.add, None, [kk], [kk])
        stt("dve", r, k, -TWO_PI, x, ALU.mult, ALU.add, [kk, kx], [kr])
        ts("dve", r, r, PI_LO, -PI_LO, ALU.min, ALU.max, [kr], [kr])
        act(ab, r, AF.Abs, [kr], [kab])

    with ExitStack() as gs:
        def gsb(name, shape, dt=F32):
            return gs.enter_context(nc.sbuf_tensor(_u(name), shape, dt))
        G2 = gsb("G2", [128, 1024])
        ident = gsb("ident", [128, 128])
        identb = gsb("identb", [128, 128], BF16)
        CST = gsb("CST", [128, 16])
        tmask = gsb("tmaskt", [128, 64])
        RWT = gsb("RWT", [128, 16, 32])
        IDX = gsb("IDX", [128, 512], I32)
        aes = ExitStack()
        PRM = aes.enter_context(nc.sbuf_tensor(_u("PRM"), [128, 5, 1024], F32))
        dma("sp", ident[:], ident_d, writes=["ident"])
        dma("sp", CST[:], cst_d, writes=["CST"])
        dma("sp", tmask[:], tmask_d, writes=["tmask"])
        cp("dve", identb[:], ident[:], ["ident"], ["identb"])
        SGNR = CST[:, 1:2]
        HALFPI = CST[:, 2:3]
        INVF = CST[:, 3:4]
        MLO = CST[:, 4:5]
        NMHI = CST[:, 5:6]
        SHALF = CST[:, 6:7]
        NEG1 = CST[:, 7:8]
        NMLO = CST[:, 8:9]
        ONE9 = CST[:, 9:10]
        N2PI = CST[:, 10:11]
        SC1 = CST[:, 11:12]
        SHALF1 = CST[:, 12:13]
        MAGICC = CST[:, 13:14]

        with ExitStack() as es:
            def sb(name, shape, dt=F32):
                return es.enter_context(nc.sbuf_tensor(_u(name), shape, dt))
            ct = sb("ct", [128, 8]); cs = sb("cs", [128, 8])
            CB = sb("CB", [128, 8, 128], BF16)
            AWb = sb("AWb", [128, 8, 512], BF16)
            ADAB = sb("ADAB", [128, 6144]); ADA = sb("ADA", [128, 6144])
            GB = sb("GB", [128, 4096])
            pa = es.enter_context(nc.psum_tensor(_u("pa"), [128, 512], F32))
            dma("sp", ct[:], c_b, writes=["ct"])
            dma("pool", ADAB[:], ada_b.to_broadcast([128, 6144]), writes=["ADAB"])
            dma("pool", GB[:], gains.to_broadcast([128, 4096]), writes=["GB"])
            act(cs[:], ct[:], AF.Silu, ["ct"], ["cs"])
            for kc in range(8):
                cp("dve", CB[:, kc, :], cs[:, kc:kc + 1].to_broadcast([128, 128]), ["cs"], ["CB"])
            for n in range(12):
                dma("pool", AWb[:], ada_w[:, n * 512:(n + 1) * 512].rearrange("(k p) n -> p k n", p=128),
                    writes=["AWb"])
                for kc in range(8):
                    mm(pa[:], CB[:, kc, :], AWb[:, kc, :], kc == 0, kc == 7, ["CB", "AWb"], ["pa"])
                tt("dve", ADA[:, n * 512:(n + 1) * 512], pa[:], ADAB[:, n * 512:(n + 1) * 512], ALU.add,
                   ["pa", "ADAB"], ["ADA"])
            stt("dve", PRM[:, 0, :], ADA[:, 1024:2048], 1.0, GB[:, 0:1024], ALU.add, ALU.mult, ["ADA", "GB"], ["PRM"])
            cp("dve", PRM[:, 1, :], ADA[:, 0:1024], ["ADA"], ["PRM"])
            tt("dve", PRM[:, 2, :], ADA[:, 2048:3072], GB[:, 1024:2048], ALU.mult, ["ADA", "GB"], ["PRM"])
            stt("dve", PRM[:, 3, :], ADA[:, 4096:5120], 1.0, GB[:, 2048:3072], ALU.add, ALU.mult, ["ADA", "GB"], ["PRM"])
            cp("dve", PRM[:, 4, :], ADA[:, 3072:4096], ["ADA"], ["PRM"])
            tt("dve", G2[:], ADA[:, 5120:6144], GB[:, 3072:4096], ALU.mult, ["ADA", "GB"], ["G2"])
            T.barrier()

        def norm_mod(es_sb, xt, key_x, a_idx, sh_idx, maskcol, hb, key_hb, tmp, tmp2, ss, rs, sq):
            act(sq[:], xt, AF.Square, [key_x], ["sq", "ss"], accum_out=ss[:])
            ts("dve", rs[:], ss[:], 1.0 / 1024, 1e-6, ALU.mult, ALU.add, ["ss"], ["rs"])
            act(rs[:], rs[:], AF.Sqrt, ["rs"], ["rs"])
            op("dve", lambda e: e.reciprocal(out=rs[:], in_=rs[:]), reads=["rs"], writes=["rs"])
            stt("dve", tmp[:], xt, rs[:, 0:1], PRM[:, a_idx, :], ALU.mult, ALU.mult, [key_x, "rs", "PRM"], ["tmp"])
            tt("dve", tmp2[:], tmp[:], PRM[:, sh_idx, :], ALU.add, ["tmp", "PRM"], ["tmp2"])
            if maskcol is None:
                act(hb, tmp2[:], AF.Copy, ["tmp2"], [key_hb])
            else:
                act(hb, tmp2[:], AF.Copy, ["tmp2", "tmask"], [key_hb], scale=maskcol)

        with ExitStack() as es:
            def sb(name, shape, dt=F32):
                return es.enter_context(nc.sbuf_tensor(_u(name), shape, dt))
            def ps(name, shape, dt=F32):
                return es.enter_context(nc.psum_tensor(_u(name), shape, dt))
            WB = sb("WB", [128, 8, 5120], BF16)
            xt = sb("xt", [128, 1024]); tmp = sb("tmp", [128, 1024]); tmp2 = sb("tmp2", [128, 1024])
            sq = sb("sq", [128, 1024], BF16)
            ss = sb("ss", [128, 1]); rs = sb("rs", [128, 1])
            hb = sb("hb", [128, 1024], BF16)
            hT = sb("hT", [128, 8, 512], BF16)
            posi = sb("posi", [128, 512], I32); posf = sb("posf", [128, 512])
            a1 = sb("a1", [128, 512]); a2 = sb("a2", [128, 512])
            cosT = sb("cosT", [128, 512]); sinS = sb("sinS", [128, 512])
            m1 = sb("m1", [128, 512]); m2 = sb("m2", [128, 512])
            ob = [sb(f"ob{i}", [128, 512], BF16) for i in range(3)]
            pT = ps("pT", [128, 8, 128], BF16)
            pk = ps("pk", [128, 512]); pks = ps("pks", [128, 512])
            pm = [ps(f"pm{i}", [128, 512]) for i in range(2)]

            blocks = [(0, 0, False), (512, 1024, False), (1024, 1024, True), (1536, 1536, False),
                      (2048, 512, False), (2560, 512, True), (3072, 2048, False), (3584, 2560, False),
                      (4096, 3072, False), (4608, 3584, False)]
            for dst, src, swp in blocks:
                if not swp:
                    dma("pool", WB[:, :, dst:dst + 512], w_in[:, src:src + 512].rearrange("(k p) n -> p k n", p=128),
                        writes=["WB"])
                else:
                    srcv = w_in[:, src:src + 512].rearrange("(k p) (h t j) -> p k h t j", p=128, h=8, t=2, j=32)
                    dstv = WB[:, :, dst:dst + 512].rearrange("p k (h t j) -> p k h t j", h=8, t=2, j=32)
                    for kc in range(8):
                        dma("pool", dstv[:, kc, :, 0, :], srcv[:, kc, :, 1, :], writes=["WB"])
                        dma("pool", dstv[:, kc, :, 1, :], srcv[:, kc, :, 0, :], writes=["WB"])

            T.barrier()
            obi = [0]

            def emit(src_ps, key_ps, dst_ap, func=AF.Copy, **kw):
                o = ob[obi[0] % 3]
                k = f"ob{obi[0] % 3}"
                obi[0] += 1
                act(o[:], src_ps, func, [key_ps], [k], **kw)
                dma("sp", dst_ap, o[:], reads=[k])

            for c in range(16):
                own = c >= 12
                for j in range(4):
                    t = 4 * c + j
                    dma("sp", xt[:], x_loc[t * 128:(t + 1) * 128, :], writes=["xt"])
                    norm_mod(sb, xt[:], "xt", 0, 1, tmask[:, t:t + 1], hb[:], "hb", tmp, tmp2, ss, rs, sq)
                    for kc in range(8):
                        op("pe", lambda e: e.transpose(out=pT[:, kc, :], in_=hb[:, kc * 128:(kc + 1) * 128],
                                                       identity=identb[:]),
                           reads=["hb", "identb"], writes=["pT"])
                    cp("dve", hT[:, :, j * 128:(j + 1) * 128], pT[:], ["pT"], ["hT"])
                dma("pool", posi[:], pos_d[0:1, c * 512:(c + 1) * 512].to_broadcast([128, 512]), writes=["posi"])
                cp("dve", posf[:], posi[:], ["posi"], ["posf"])
                ts("dve", a1[:], posf[:], INVF, None, ALU.mult, None, ["posf", "CST"], ["a1"])
                reduce_angle(a1[:], "a1", a2[:], "a2", m1[:], "m1", m2[:], "m2")
                act(sinS[:], m1[:], AF.Sin, ["m1", "CST"], ["sinS"], scale=SGNR)
                act(cosT[:], m2[:], AF.Sin, ["m2", "CST"], ["cosT"], scale=NEG1, bias=HALFPI)

                def rope_proj(c0, c0s, dst, scale):
                    for cb in range(4):
                        for kc in range(8):
                            mm(pk[:], WB[:, kc, c0 + cb * 128:c0 + (cb + 1) * 128], hT[:, kc, :], kc == 0, kc == 7,
                               ["WB", "hT"], ["pk"])
                        for kc in range(8):
                            mm(pks[:], WB[:, kc, c0s + cb * 128:c0s + (cb + 1) * 128], hT[:, kc, :], kc == 0, kc == 7,
                               ["WB", "hT"], ["pks"])
                        tt("dve", m1[:], pk[:], cosT[:], ALU.mult, ["pk", "cosT"], ["m1"])
                        tt("dve", m2[:], pks[:], sinS[:], ALU.mult, ["pks", "sinS"], ["m2"])
                        tt("dve", m1[:], m1[:], m2[:], ALU.add, ["m1", "m2"], ["m1"])
                        emit(m1[:], "m1", dst(cb), scale=scale)

                rope_proj(512, 1024, lambda cb: KT[cb * 128:(cb + 1) * 128, c * 512:(c + 1) * 512], 1.0)
                for cb in range(4):
                    p = pm[cb % 2]; kp = f"pm{cb % 2}"
                    for kc in range(8):
                        mm(p[:], WB[:, kc, cb * 128:(cb + 1) * 128], hT[:, kc, :], kc == 0, kc == 7, ["WB", "hT"], [kp])
                    emit(p[:], kp, UT[cb * 128:(cb + 1) * 128, c * 512:(c + 1) * 512])
                for j in range(4):
                    p = pm[j % 2]; kp = f"pm{j % 2}"
                    for kc in range(8):
                        mm(p[:], hT[:, kc, j * 128:(j + 1) * 128], WB[:, kc, 1536:2048], kc == 0, kc == 7, ["WB", "hT"], [kp])
                    emit(p[:], kp, VV[(4 * c + j) * 128:(4 * c + j + 1) * 128, :])
                if own:
                    co = c - 12
                    rope_proj(2048, 2560, lambda cb: QT[cb * 128:(cb + 1) * 128, co * 512:(co + 1) * 512], 0.125)
                    for gi, (c0, dstT) in enumerate(((3072, GST), (4096, GAT))):
                        for db in range(8):
                            p = pm[db % 2]; kp = f"pm{db % 2}"
                            for kc in range(8):
                                mm(p[:], WB[:, kc, c0 + db * 128:c0 + (db + 1) * 128], hT[:, kc, :], kc == 0, kc == 7,
                                   ["WB", "hT"], [kp])
                            emit(p[:], kp, dstT[db * 128:(db + 1) * 128, co * 512:(co + 1) * 512], func=AF.Sigmoid)
            T.barrier()

        SEG = 1024
        NSEG = 8192 // SEG
        with ExitStack() as es:
            def sb(name, shape, dt=F32):
                return es.enter_context(nc.sbuf_tensor(_u(name), shape, dt))
            Y = sb("Y", [128, 4, 2048])
            LA = sb("LA", [128, 32, 128], BF16); LB = sb("LB", [128, 32, 128], BF16)
            BL1b = sb("BL1b", [128, 32, 128], BF16); BL2b = sb("BL2b", [128, 32, 128], BF16)
            TH = sb("TH", [128, 32]); RHO = sb("RHO", [128, 32]); CAR = sb("CAR", [128, 32])
            PHf = sb("PHf", [128, 32]); OFFT = sb("OFFT", [128, 8, 32]); NOFF = sb("NOFF", [128, 8, 32])
            SB1 = sb("SB1", [128, 8, 32]); SB2 = sb("SB2", [128, 8, 32])
            DSK = sb("DSK", [128, 4])
            dma("sp", DSK[:], dsk_d, writes=["DSK"])
            with ExitStack() as e2:
                def sb2(name, shape, dt=F32):
                    return e2.enter_context(nc.sbuf_tensor(_u(name), shape, dt))
                LRE = sb2("LRE", [128, 32]); LIM = sb2("LIM", [128, 32]); LDT = sb2("LDT", [128, 32])
                CR = sb2("CR", [128, 32, 16]); CI = sb2("CI", [128, 32, 16])
                w = [sb2(f"w{i}", [128, 32]) for i in range(10)]
                c1 = sb2("c1", [128, 32, 16]); c2 = sb2("c2", [128, 32, 16])
                cpr = sb2("cpr", [128, 32, 16]); cpi = sb2("cpi", [128, 32, 16])
                for src, dstb, kb in ((bl1_d, BL1b, "BL1b"), (bl2_d, BL2b, "BL2b")):
                    dma("pool", dstb[:].rearrange("p g m -> p (g m)"), src, writes=[kb])
                dma("sp", LRE[:], lre_d, writes=["LRE"])
                dma("sp", LIM[:], lim_d, writes=["LIM"])
                dma("pool", LDT[:], ldt_d.to_broadcast([128, 32]), writes=["LDT"])
                dma("sp", CR[:].rearrange("p g c -> p (g c)"), cr_d, writes=["CR"])
                dma("sp", CI[:].rearrange("p g c -> p (g c)"), ci_d, writes=["CI"])
                K = "tb"
                dt_ = w[0]
                act(dt_[:], LDT[:], AF.Exp, ["LDT"], [K])
                tt("dve", TH[:], LIM[:], dt_[:], ALU.mult, ["LIM", K], ["TH"])
                tt("dve", w[1][:], LRE[:], dt_[:], ALU.mult, ["LRE", K], [K])
                act(RHO[:], w[1][:], AF.Exp, [K], ["RHO"])
                reduce_angle(TH[:], "TH", w[2][:], K, w[3][:], K, w[9][:], K)
                act(w[4][:], w[3][:], AF.Sin, [K, "CST"], [K], scale=ONE9)
                act(w[5][:], w[9][:], AF.Sin, [K, "CST"], [K], scale=NEG1, bias=HALFPI)
                tt("dve", w[6][:], RHO[:], w[5][:], ALU.mult, ["RHO", K], [K])
                ts("dve", w[6][:], w[6][:], -1.0, None, ALU.add, None, [K], [K])
                tt("dve", w[7][:], RHO[:], w[4][:], ALU.mult, ["RHO", K], [K])
                tt("dve", w[8][:], LRE[:], LRE[:], ALU.mult, ["LRE"], [K])
                tt("dve", w[9][:], LIM[:], LIM[:], ALU.mult, ["LIM"], [K])
                tt("dve", w[8][:], w[8][:], w[9][:], ALU.add, [K], [K])
                op("dve", lambda e: e.reciprocal(out=w[8][:], in_=w[8][:]), reads=[K], writes=[K])
                tt("dve", w[0][:], w[6][:], LRE[:], ALU.mult, [K, "LRE"], [K])
                tt("dve", w[1][:], w[7][:], LIM[:], ALU.mult, [K, "LIM"], [K])
                tt("dve", w[0][:], w[0][:], w[1][:], ALU.add, [K], [K])
                tt("dve", w[2][:], w[0][:], w[8][:], ALU.mult, [K], [K])
                tt("dve", w[0][:], w[7][:], LRE[:], ALU.mult, [K, "LRE"], [K])
                tt("dve", w[1][:], w[6][:], LIM[:], ALU.mult, [K, "LIM"], [K])
                tt("dve", w[0][:], w[0][:], w[1][:], ALU.subtract, [K], [K])
                tt("dve", w[3][:], w[0][:], w[8][:], ALU.mult, [K], [K])
                qrb = w[2][:, :].unsqueeze(2).to_broadcast([128, 32, 16])
                qib = w[3][:, :].unsqueeze(2).to_broadcast([128, 32, 16])
                tt("dve", c1[:], CR[:], qrb, ALU.mult, ["CR", K], [K])
                tt("dve", c2[:], CI[:], qib, ALU.mult, ["CI", K], [K])
                tt("dve", cpr[:], c1[:], c2[:], ALU.subtract, [K], [K])
                tt("dve", c1[:], CR[:], qib, ALU.mult, ["CR", K], [K])
                tt("dve", c2[:], CI[:], qrb, ALU.mult, ["CI", K], [K])
                tt("dve", cpi[:], c1[:], c2[:], ALU.add, [K], [K])
                ts("dve", c1[:], cpr[:], MLO, None, ALU.mult, None, [K, "CST"], [K])
                stt("dve", c1[:], cpi[:], NMHI, c1[:], ALU.mult, ALU.add, [K, "CST"], [K])
                ts("dve", c2[:], cpi[:], NMLO, None, ALU.mult, None, [K, "CST"], [K])
                stt("dve", c2[:], cpr[:], NMHI, c2[:], ALU.mult, ALU.add, [K, "CST"], [K])
                op("pool", lambda e: e.memset(LA[:], 0.0), writes=["LA"])
                op("pool", lambda e: e.memset(LB[:], 0.0), writes=["LB"])
                for src, dst, kd in ((c1, LA, "LA"), (c2, LB, "LB")):
                    dv = dst[:].rearrange("p (a r) (s c) -> p a r s c", r=8, s=8, c=16)
                    sv = src[:].rearrange("p (a r) c -> p a r c", r=8)
                    for r in range(8):
                        cp("dve", dv[:, :, r, r, :], sv[:, :, r, :], [K], [kd])
                op("dve", lambda e: e.memset(CAR[:], 0.0), writes=["CAR"])
                ts("dve", w[0][:], TH[:], INV2PI, MAGIC, ALU.mult, ALU.add, ["TH"], [K])
                ts("dve", w[0][:], w[0][:], -MAGIC, None, ALU.add, None, [K], [K])
                stt("dve", PHf[:], TH[:], INV2PI, w[0][:], ALU.mult, ALU.subtract, ["TH", K], ["PHf"])
                for sg in range(8):
                    ts("dve", w[1][:], PHf[:], float(sg * 1024), None, ALU.mult, None, ["PHf"], [K])
                    ts("dve", w[2][:], w[1][:], MAGIC, None, ALU.add, None, [K], [K])
                    stt("dve", OFFT[:, sg, :], w[2][:], -MAGIC, w[1][:], ALU.add, ALU.subtract, [K], ["OFFT"])
                    ts("dve", OFFT[:, sg, :], OFFT[:, sg, :], -1.0, None, ALU.mult, None, ["OFFT"], ["OFFT"])
                ts("dve", NOFF[:].rearrange("p a g -> p (a g)"), OFFT[:].rearrange("p a g -> p (a g)"), -1.0, None,
                   ALU.mult, None, ["OFFT"], ["NOFF"])
                ts("dve", SB2[:].rearrange("p a g -> p (a g)"), OFFT[:].rearrange("p a g -> p (a g)"),
                   TWO_PI, None, ALU.mult, None, ["OFFT"], ["SB2"])
                ts("dve", SB1[:].rearrange("p a g -> p (a g)"), SB2[:].rearrange("p a g -> p (a g)"), SHALF1, None,
                   ALU.mult, None, ["SB2", "CST"], ["SB1"])
                T.barrier()

            with ExitStack() as e2:
                def sb2(name, shape, dt=F32):
                    return e2.enter_context(nc.sbuf_tensor(_u(name), shape, dt))
                def ps2(name, shape, dt=F32):
                    return e2.enter_context(nc.psum_tensor(_u(name), shape, dt))
                UTb = sb2("UTb", [128, 8192], BF16)
                IOT = sb2("IOT", [128, SEG])
                UB = sb2("UB", [128, SEG])
                U2 = [sb2(f"U_{i}", [128, SEG]) for i in range(2)]; KK2 = [sb2(f"KK{i}", [128, SEG]) for i in range(2)]
                NF2 = [sb2(f"NF{i}", [128, SEG]) for i in range(2)]; AB2 = [sb2(f"AB{i}", [128, SEG]) for i in range(2)]
                CO2 = [sb2(f"COS2{i}", [128, SEG]) for i in range(2)]; SS2 = [sb2(f"SINS{i}", [128, SEG]) for i in range(2)]
                SN2 = [sb2(f"SIN2{i}", [128, SEG]) for i in range(2)]
                m2s = [sb2(f"sm2{i}", [128, 512]) for i in range(2)]
                W = sb2("W", [128, SEG]); RB = sb2("RB", [128, SEG]); Z = sb2("Z", [128, SEG])
                A1b = sb2("A1b", [128, SEG], BF16); A2b = sb2("A2b", [128, SEG], BF16)
                py = [ps2(f"py{i}", [128, 512]) for i in range(4)]
                p1 = [ps2(f"p1{i}", [128, 512]) for i in range(2)]
                p2 = [ps2(f"p2{i}", [128, 512]) for i in range(2)]
                dma("pool", IOT[:], iota_d.to_broadcast([128, SEG]), writes=["IOT"])
                ZT = sb2("ZT", [128, 4096], BF16)
                op("pool", lambda e: e.memset(ZT[:], 0.0), writes=["ZT"])
                xdv = XD.rearrange("(p i) d -> p (i d)", p=128)
                NOWN = 2048 // SEG
                items = [(g, seg) for g in range(32) for seg in range(NSEG)]
                pi_ = [0]

                def stageA(n):
                    g, seg = items[n]
                    b_ = n % 2
                    ownseg = seg >= NSEG - NOWN
                    if seg == 0:
                        ts("dve", UB[:], IOT[:], PHf[:, g:g + 1], None, ALU.mult, None, ["IOT", "PHf"], ["UB"])
                    act(U2[b_][:], UB[:], AF.Identity, ["UB", "OFFT"], [f"U_{b_}"], bias=OFFT[:, seg, g:g + 1])
                    act(KK2[b_][:], U2[b_][:], AF.Identity, [f"U_{b_}", "CST"], [f"KK{b_}"], bias=MAGICC)
                    stt("dve", NF2[b_][:], KK2[b_][:], -MAGIC, U2[b_][:], ALU.add, ALU.subtract,
                        [f"KK{b_}", f"U_{b_}"], [f"NF{b_}"])
                    act(AB2[b_][:], NF2[b_][:], AF.Abs, [f"NF{b_}"], [f"AB{b_}"])
                    act(CO2[b_][:], AB2[b_][:], AF.Sin, [f"AB{b_}", "CST"], [f"COS2{b_}"], scale=N2PI, bias=HALFPI)
                    act(SS2[b_][:], NF2[b_][:], AF.Sin, [f"NF{b_}", "CST"], [f"SINS{b_}"], scale=SC1)
                    if ownseg:
                        act(SN2[b_][:], NF2[b_][:], AF.Sin, [f"NF{b_}", "CST"], [f"SIN2{b_}"], scale=N2PI)

                def stageB(n):
                    g, seg = items[n]
                    b_ = n % 2
                    cbk, gg = g // 8, g % 8
                    ownseg = seg >= NSEG - NOWN
                    base = min((gg // 2) * 32, 64)
                    kr = 32 if gg // 2 < 3 else 64
                    COS2 = CO2[b_]; SINS = SS2[b_]; SIN2 = SN2[b_]
                    if seg == 0:
                        if gg == 0:
                            dma("sp", UTb[:], UT[cbk * 128:(cbk + 1) * 128, :], writes=["UTb"])
                            if cbk == 0:
                                for zi in range(64):
                                    dma("sp", xdv[:, zi * 4096:(zi + 1) * 4096], ZT[:], reads=["ZT"], writes=[f"XDz{zi}"])
                        cp("dve", RB[:], RHO[:, g:g + 1].to_broadcast([128, SEG]), ["RHO"], ["RB"])
                    for ch in range(SEG // 512):
                        tok0 = seg * SEG + ch * 512
                        q1 = p1[pi_[0] % 2]; q2 = p2[pi_[0] % 2]; k1 = f"p1{pi_[0] % 2}"; k2 = f"p2{pi_[0] % 2}"
                        pi_[0] += 1
                        mm(q1[:], BL1b[base:base + kr, g, :], UTb[base:base + kr, tok0:tok0 + 512], True, True,
                           ["BL1b", "UTb"], [k1])
                        mm(q2[:], BL2b[base:base + kr, g, :], UTb[base:base + kr, tok0:tok0 + 512], True, True,
                           ["BL2b", "UTb"], [k2])
                        wv = W[:, ch * 512:(ch + 1) * 512]
                        m2 = m2s[ch % 2]; km2 = f"m2{ch % 2}"
                        tt("dve", wv, q1[:], COS2[:, ch * 512:(ch + 1) * 512], ALU.mult, [k1, f"COS2{b_}"], [f"W{ch}"])
                        tt("dve", m2[:], q2[:], SINS[:, ch * 512:(ch + 1) * 512], ALU.mult, [k2, f"SINS{b_}"], [km2])
                        tt("dve", wv, wv, m2[:], ALU.add, [f"W{ch}", km2], [f"W{ch}"])
                    op("dve", lambda e: e.tensor_tensor_scan(out=Z[:], data0=RB[:], data1=W[:],
                                                              initial=CAR[:, g:g + 1], op0=ALU.mult, op1=ALU.add),
                       reads=["RB", "W0", "W1", "CAR"], writes=["Z"])
                    cp("dve", CAR[:, g:g + 1], Z[:, SEG - 1:SEG], ["Z"], ["CAR"])
                    if ownseg:
                        tt("dve", A1b[:], Z[:], COS2[:], ALU.mult, ["Z", f"COS2{b_}"], ["A1b"])
                        tt("dve", A2b[:], Z[:], SIN2[:], ALU.mult, ["Z", f"SIN2{b_}"], ["A2b"])
                        so = seg - (NSEG - NOWN)
                        for ch in range(SEG // 512):
                            yi = so * (SEG // 512) + ch
                            mm(py[yi][:], LA[:, g, :], A1b[:, ch * 512:(ch + 1) * 512], gg == 0, False,
                               ["LA", "A1b"], [f"py{yi}"])
                            mm(py[yi][:], LB[:, g, :], A2b[:, ch * 512:(ch + 1) * 512], False, gg == 7,
                               ["LB", "A2b"], [f"py{yi}"])
                    if gg == 7 and seg == NSEG - 1:
                        for yi in range(4):
                            stt("dve", Y[:, cbk, yi * 512:(yi + 1) * 512], UTb[:, 6144 + yi * 512:6144 + (yi + 1) * 512],
                                DSK[:, cbk:cbk + 1], py[yi][:], ALU.mult, ALU.add, ["UTb", "DSK", f"py{yi}"], ["Y"])

                stageA(0)
                for n in range(len(items)):
                    if n + 1 < len(items):
                        stageA(n + 1)
                    stageB(n)
                T.barrier()

            with ExitStack() as e2:
                def sb2(name, shape, dt=F32):
                    return e2.enter_context(nc.sbuf_tensor(_u(name), shape, dt))
                def ps2(name, shape, dt=F32):
                    return e2.enter_context(nc.psum_tensor(_u(name), shape, dt))
                WGb = sb2("WGb", [128, 4, 512], BF16)
                t1 = sb2("gt1", [128, 2048]); t2 = sb2("gt2", [128, 2048])
                YGb = sb2("YGb", [128, 4, 2048], BF16)
                sgl = sb2("sgl", [128, 512]); ysb = [sb2(f"ysb{i}", [128, 512], BF16) for i in range(2)]
                pg = [ps2(f"pgl{i}", [128, 512]) for i in range(2)]
                dma("pool", WGb[:], wglu_d.rearrange("(k p) n -> p k n", p=128), writes=["WGb"])
                for cbk in range(4):
                    yv = Y[:, cbk, :]
                    act(t1[:], yv, AF.Square, ["Y"], ["t1"])
                    ts("dve", t1[:], t1[:], 0.044715, 1.0, ALU.mult, ALU.add, ["t1"], ["t1"])
                    tt("dve", t1[:], t1[:], yv, ALU.mult, ["t1", "Y"], ["t1"])
                    act(t2[:], t1[:], AF.Sigmoid, ["t1"], ["t2"], scale=1.5957691216057308)
                    tt("dve", yv, yv, t2[:], ALU.mult, ["Y", "t2"], ["Y"])
                    act(YGb[:, cbk, :], yv, AF.Copy, ["Y"], ["YGb"])
                i = 0
                for cbo in range(4):
                    for ch in range(4):
                        p = pg[i % 2]; kp = f"pgl{i % 2}"; o = ysb[i % 2]; ko = f"ysb{i % 2}"
                        i += 1
                        for kc in range(4):
                            mm(p[:], WGb[:, kc, cbo * 128:(cbo + 1) * 128], YGb[:, kc, ch * 512:(ch + 1) * 512],
                               kc == 0, kc == 3, ["WGb", "YGb"], [kp])
                        act(sgl[:], p[:], AF.Sigmoid, [kp], ["sgl"])
                        tt("dve", o[:], Y[:, cbo, ch * 512:(ch + 1) * 512], sgl[:], ALU.mult, ["Y", "sgl"], [ko])
                        dma("sp", YST[cbo * 128:(cbo + 1) * 128, ch * 512:(ch + 1) * 512], o[:], reads=[ko])
                T.barrier()

        with ExitStack() as es:
            def sb(name, shape, dt=F32):
                return es.enter_context(nc.sbuf_tensor(_u(name), shape, dt))
            def ps(name, shape, dt=F32):
                return es.enter_context(nc.psum_tensor(_u(name), shape, dt))
            KA = sb("KA", [128, 8192], BF16)
            VA = sb("VA", [128, 64, 65], BF16)
            QA = sb("QA", [128, 2048], BF16)
            DM = sb("DM", [128, 4, 512], BF16)
            VB = sb("VB", [128, 8, 96]); VVd = sb("VVd", [128, 8, 96]); VO = sb("VO", [128, 8, 96])
            KM = sb("KM", [64, 32]); KMb = sb("KMb", [64, 32], BF16)
            gt_ = sb("gt", [128, 96]); top8 = sb("top8", [128, 8]); sel = sb("sel", [128, 96])
            pt = [sb(f"pt{i}", [128, 512], BF16) for i in range(3)]
            rrow = sb("rrow", [128, 512]); rhi = sb("rhi", [128, 512], BF16); rlo = sb("rlo", [128, 512], BF16)
            rtmp = sb("rtmp", [128, 512])
            onesb = sb("onesb", [128, 64], BF16)
            bc = sb("bc", [64, 512]); oab = sb("oab", [64, 512], BF16)
            pS = [ps(f"pS{i}", [128, 512]) for i in range(3)]
            pO = [ps(f"pO{i}", [128, 512]) for i in range(2)]
            pG = ps("pG", [128, 96]); pB = ps("pB", [128, 128]); pO2 = ps("pO2", [64, 512])
            dma("sp", KA[64:96, :], bonehot_d, writes=["KAoh"])
            dma("sp", DM[:].rearrange("p a n -> p (a n)"), dm_d, writes=["DM"])
            dma("pool", VB[:].rearrange("p a n -> p (a n)"), vb_d.to_broadcast([128, 768]), writes=["VB"])
            dma("pool", VVd[:].rearrange("p a n -> p (a n)"), vv_d.to_broadcast([128, 768]), writes=["VVd"])
            dma("pool", VO[:].rearrange("p a n -> p (a n)"), vo_d.to_broadcast([128, 768]), writes=["VO"])
            op("pool", lambda e: e.memset(VA[:, :, 64:65], 1.0), writes=["VA1"])
            op("pool", lambda e: e.memset(onesb[:], 1.0), writes=["onesb"])
            op("pool", lambda e: e.memset(gt_[:], 0.0), writes=["gt"])
            itb = [0]
            for h in range(8):
                dma("sp", KA[0:64, :], KT[h * 64:(h + 1) * 64, :], writes=["KA"])
                dma("sp", QA[0:64, :], QT[h * 64:(h + 1) * 64, :], writes=["QA"])
                dma("pool", VA[:, :, 0:64], VV[:, h * 64:(h + 1) * 64].rearrange("(t p) d -> p t d", p=128),
                    writes=["VA"])
                op("dve", lambda e: e.tensor_reduce(out=KM[:], in_=KA[0:64, :].rearrange("p (n l) -> p n l", l=256),
                                                    axis=AX.X, op=ALU.add), reads=["KA"], writes=["KM"])
                cp("dve", KMb[:], KM[:], ["KM"], ["KMb"])
                for qt in range(16):
                    qb = qt // 2
                    mm(pG[:, 64:96], QA[0:64, qt * 128:(qt + 1) * 128], KMb[:], True, True, ["QA", "KMb"], ["pG"])
                    tt("dve", gt_[:, 64:96], pG[:, 64:96], VB[:, qb, 64:96], ALU.add, ["pG", "VB"], ["gt"])
                    op("dve", lambda e: e.max(out=top8[:], in_=gt_[:, 64:96]), reads=["gt"], writes=["top8"])
                    ts("dve", sel[:, 64:96], gt_[:, 64:96], top8[:, 2:3], None, ALU.is_ge, None, ["gt", "top8"], ["sel"])
                    tt("dve", sel[:, 64:96], sel[:, 64:96], VVd[:, qb, 64:96], ALU.mult, ["sel", "VVd"], ["sel"])
                    tt("dve", sel[:, 64:96], sel[:, 64:96], VO[:, qb, 64:96], ALU.add, ["sel", "VO"], ["sel"])
                    ts("dve", gt_[:, 64:96], sel[:, 64:96], -1.0, -NEG, ALU.add, ALU.mult, ["sel"], ["gt"])
                    op("pe", lambda e: e.transpose(out=pB[0:96, :], in_=gt_[:, 0:96], identity=ident[:]),
                       reads=["gt", "ident"], writes=["pB"])
                    cp("dve", QA[64:96, qt * 128:(qt + 1) * 128], pB[64:96, :], ["pB"], ["QAb"])
                for G in range(4):
                    cnt = 52 + 4 * G
                    po = pO[G % 2]; kpo = f"pO{G % 2}"

                    def qk(kt):
                        i3 = (itb[0] + kt) % 3
                        mm(pS[i3][:], KA[0:96, kt * 128:(kt + 1) * 128], QA[0:96, G * 512:(G + 1) * 512], True, True,
                           ["KA", "KAoh", "QA", "QAb"], [f"pS{i3}"])
                    qk(0)
                    qk(1)
                    for kt in range(cnt):
                        i3 = (itb[0] + kt) % 3
                        s_ = pS[i3]; ks = f"pS{i3}"; p_ = pt[i3]; kp = f"pt{i3}"
                        if kt + 2 < cnt:
                            qk(kt + 2)
                        act(p_[:], s_[:], AF.Exp, [ks], [kp])
                        di = kt - (cnt - 4)
                        if di >= 0:
                            tt("dve", p_[:], p_[:], DM[:, di, :], ALU.mult, [kp, "DM"], [kp])
                        mm(po[0:65, :], VA[:, kt, 0:65], p_[:], kt == 0, kt == cnt - 1, ["VA", "VA1", kp], [kpo])
                    itb[0] += cnt
                    cp("dve", rrow[64:65, :], po[64:65, :], [kpo], ["rrow"])
                    op("dve", lambda e: e.reciprocal(out=rrow[64:65, :], in_=rrow[64:65, :]), reads=["rrow"], writes=["rrow"])
                    cp("dve", rhi[64:65, :], rrow[64:65, :], ["rrow"], ["rhi"])
                    cp("dve", rtmp[64:65, :], rhi[64:65, :], ["rhi"], ["rtmp"])
                    tt("dve", rtmp[64:65, :], rrow[64:65, :], rtmp[64:65, :], ALU.subtract, ["rrow", "rtmp"], ["rtmp"])
                    cp("dve", rlo[64:65, :], rtmp[64:65, :], ["rtmp"], ["rlo"])
                    mm(pO2[:], onesb[64:65, 0:64], rhi[64:65, :], True, False, ["onesb", "rhi"], ["pO2"])
                    mm(pO2[:], onesb[64:65, 0:64], rlo[64:65, :], False, True, ["onesb", "rlo"], ["pO2"])
                    cp("dve", bc[:], pO2[:], ["pO2"], ["bc"])
                    tt("dve", oab[:], po[0:64, :], bc[:], ALU.mult, [kpo, "bc"], ["oab"])
                    dma("sp", OAT[h * 64:(h + 1) * 64, G * 512:(G + 1) * 512], oab[:], reads=["oab"])
            T.barrier()

        with ExitStack() as es:
            def sb(name, shape, dt=F32):
                return es.enter_context(nc.sbuf_tensor(_u(name), shape, dt))
            def ps(name, shape, dt=F32):
                return es.enter_context(nc.psum_tensor(_u(name), shape, dt))
            Wsb = sb("Wsb", [128, 4, 1024], BF16); Wab = sb("Wab", [128, 4, 1024], BF16)
            Wo = sb("Wo", [128, 8, 1024], BF16)
            ys = sb("ys", [128, 4, 512], BF16); oa = sb("oa", [128, 4, 512], BF16)
            gsT = sb("gsT", [128, 8, 512], BF16); gaT = sb("gaT", [128, 8, 512], BF16)
            MT = sb("MT", [128, 8, 512], BF16)
            b1 = sb("b1", [128, 512]); b2 = sb("b2", [128, 512])
            xt = sb("xt2", [128, 1024]); x1 = sb("x1", [128, 1024]); tmp = sb("tmpE", [128, 1024]); tmp2 = sb("tmp2E", [128, 1024])
            sq = sb("sqE", [128, 1024], BF16); ss = sb("ssE", [128, 1]); ss2 = sb("ss2E", [128, 1]); rs = sb("rsE", [128, 1])
            hbs = [sb(f"hbE{i}", [128, 1024], BF16) for i in range(2)]; h2T = sb("h2T", [128, 8, 512], BF16)
            RWb = sb("RWb", [128, 8, 32], BF16); RBb = sb("RBb", [128, 32]); LT = sb("LT", [128, 128], BF16)
            ONESb = sb("ONESb", [128, 128], BF16); CNT = sb("CNT", [128, 32]); EOFF = sb("EOFF", [128, 32])
            mb = sb("mb", [128, 32], BF16); posf = sb("posfE", [128, 32]); idxf = sb("idxf", [128, 32])
            lg = sb("lg", [128, 32]); top8 = sb("top8F", [128, 8]); msk = sb("msk", [128, 32]); ex = sb("ex", [128, 32])
            nmx = sb("nmx", [128, 1]); sm = sb("sm", [128, 1])
            pl = ps("pl", [128, 32]); pp = ps("pp", [128, 32]); pc = ps("pc", [128, 32])
            dma("pool", RWb[:], rw_d.rearrange("(k p) n -> p k n", p=128), writes=["RWb"])
            dma("pool", RBb[:], rb_d.to_broadcast([128, 32]), writes=["RBb"])
            dma("pool", EOFF[:], eoff_d.to_broadcast([128, 32]), writes=["EOFF"])
            dma("sp", LT[:], ltri_d, writes=["LT"])
            op("pool", lambda e: e.memset(ONESb[:], 1.0), writes=["ONESb"])
            op("pool", lambda e: e.memset(CNT[:], 0.0), writes=["CNT"])
            pb1 = ps("pb1", [128, 512]); pb2 = ps("pb2", [128, 512])
            pmx = ps("pmx", [128, 1024]); pT = ps("pTE", [128, 8, 128], BF16)
            for (src, dst, kd, nk) in ((wsb_d, Wsb, "Wsb", 4), (wab_d, Wab, "Wab", 4), (wout_d, Wo, "Wo", 8)):
                dma("pool", dst[:], src.rearrange("(k p) n -> p k n", p=128), writes=[kd])
            for c in range(4):
                cs_ = slice(c * 512, (c + 1) * 512)
                dma("sp", ys[:], YST[:, cs_].rearrange("(k p) n -> p k n", p=128), writes=["ys"])
                dma("sp", oa[:], OAT[:, cs_].rearrange("(k p) n -> p k n", p=128), writes=["oa"])
                dma("pool", gsT[:], GST[:, cs_].rearrange("(k p) n -> p k n", p=128), writes=["gsT"])
                dma("pool", gaT[:], GAT[:, cs_].rearrange("(k p) n -> p k n", p=128), writes=["gaT"])
                for db in range(8):
                    for kc in range(4):
                        mm(pb1[:], Wsb[:, kc, db * 128:(db + 1) * 128], ys[:, kc, :], kc == 0, kc == 3, ["Wsb", "ys"], ["pb1"])
                    for kc in range(4):
                        mm(pb2[:], Wab[:, kc, db * 128:(db + 1) * 128], oa[:, kc, :], kc == 0, kc == 3, ["Wab", "oa"], ["pb2"])
                    tt("dve", b1[:], pb1[:], gsT[:, db, :], ALU.mult, ["pb1", "gsT"], ["b1"])
                    tt("dve", b2[:], pb2[:], gaT[:, db, :], ALU.mult, ["pb2", "gaT"], ["b2"])
                    tt("dve", MT[:, db, :], b1[:], b2[:], ALU.add, ["b1", "b2"], ["MT"])
                for j in range(4):
                    t = 4 * c + j
                    for half in range(2):
                        for kc in range(8):
                            mm(pmx[:, half * 512:(half + 1) * 512], MT[:, kc, j * 128:(j + 1) * 128],
                               Wo[:, kc, half * 512:(half + 1) * 512], kc == 0, kc == 7, ["MT", "Wo"], ["pmx"])
                    dma("sp", xt[:], x_loc[6144 + t * 128:6144 + (t + 1) * 128, :], writes=["xt"])
                    act(sq[:, 0:512], pmx[:, 0:512], AF.Square, ["pmx"], ["sq", "ss"], accum_out=ss[:])
                    act(sq[:, 512:1024], pmx[:, 512:1024], AF.Square, ["pmx"], ["sq", "ss2"], accum_out=ss2[:])
                    tt("dve", ss[:], ss[:], ss2[:], ALU.add, ["ss", "ss2"], ["ss"])
                    ts("dve", rs[:], ss[:], 1.0 / 1024, 1e-6, ALU.mult, ALU.add, ["ss"], ["rs"])
                    act(rs[:], rs[:], AF.Sqrt, ["rs"], ["rs"])
                    op("dve", lambda e: e.reciprocal(out=rs[:], in_=rs[:]), reads=["rs"], writes=["rs"])
                    cp("dve", tmp[:], pmx[:], ["pmx"], ["tmp"])
                    stt("dve", tmp[:], tmp[:], rs[:, 0:1], PRM[:, 2, :], ALU.mult, ALU.mult, ["tmp", "rs", "PRM"], ["tmp"])
                    tt("dve", x1[:], tmp[:], xt[:], ALU.add, ["tmp", "xt"], ["x1"])
                    dma("sp", X1[t * 128:(t + 1) * 128, :], x1[:], reads=["x1"])
                    hb = hbs[t % 2]; khb = f"hb{t % 2}"
                    norm_mod(sb, x1[:], "x1", 3, 4, None, hb[:], khb, tmp, tmp2, ss, rs, sq)
                    for kc in range(8):
                        op("pe", lambda e: e.transpose(out=pT[:, kc, :], in_=hb[:, kc * 128:(kc + 1) * 128],
                                                       identity=identb[:]),
                           reads=[khb, "identb"], writes=["pT"])
                    cp("dve", h2T[:, :, j * 128:(j + 1) * 128], pT[:], ["pT"], ["h2T"])
                    for kc in range(8):
                        mm(pl[:], h2T[:, kc, j * 128:(j + 1) * 128], RWb[:, kc, :], kc == 0, kc == 7, ["h2T", "RWb"], ["pl"])
                    tt("dve", lg[:], pl[:], RBb[:], ALU.add, ["pl", "RBb"], ["lg"])
                    op("dve", lambda e: e.max(out=top8[:], in_=lg[:]), reads=["lg"], writes=["top8"])
                    ts("dve", msk[:], lg[:], top8[:, 3:4], None, ALU.is_ge, None, ["lg", "top8"], ["msk"])
                    ts("dve", nmx[:], top8[:, 0:1], -1.0, None, ALU.mult, None, ["top8"], ["nmx"])
                    act(ex[:], lg[:], AF.Exp, ["lg", "nmx"], ["ex"], bias=nmx[:, 0:1])
                    tt("dve", ex[:], ex[:], msk[:], ALU.mult, ["ex", "msk"], ["ex"])
                    op("dve", lambda e: e.reduce_sum(out=sm[:], in_=ex[:], axis=AX.X), reads=["ex"], writes=["sm"])
                    op("dve", lambda e: e.reciprocal(out=sm[:], in_=sm[:]), reads=["sm"], writes=["sm"])
                    ts("dve", RWT[:, t, :], ex[:], sm[:, 0:1], None, ALU.mult, None, ["ex", "sm"], ["RWT"])
                    cp("dve", mb[:], msk[:], ["msk"], ["mb"])
                    mm(pp[:], LT[:], mb[:], True, True, ["LT", "mb"], ["pp"])
                    mm(pc[:], ONESb[:], mb[:], True, True, ["ONESb", "mb"], ["pc"])
                    tt("dve", posf[:], pp[:], CNT[:], ALU.add, ["pp", "CNT"], ["posf"])
                    tt("dve", CNT[:], CNT[:], pc[:], ALU.add, ["CNT", "pc"], ["CNT"])
                    ts("dve", idxf[:], posf[:], float(CAP), None, ALU.is_lt, None, ["posf"], ["idxf"])
                    tt("dve", msk[:], msk[:], idxf[:], ALU.mult, ["msk", "idxf"], ["msk"])
                    tt("dve", idxf[:], posf[:], EOFF[:], ALU.add, ["posf", "EOFF"], ["idxf"])
                    tt("dve", idxf[:], idxf[:], msk[:], ALU.mult, ["idxf", "msk"], ["idxf"])
                    ts("dve", idxf[:], idxf[:], 40000.0, None, ALU.add, None, ["idxf"], ["idxf"])
                    cp("dve", IDX[:, t * 32:(t + 1) * 32], idxf[:], ["idxf"], [f"IDX{t}"])
                    for e_ in range(32):
                        T.dma_fn("pool", lambda g, e_=e_, hb=hb: g.indirect_dma_start(
                            out=XD[:, :], out_offset=bass.IndirectOffsetOnAxis(ap=IDX[:, t * 32 + e_:t * 32 + e_ + 1], axis=0),
                            in_=hb[:, :], in_offset=None, bounds_check=BCREG, oob_is_err=False),
                            reads=[khb, f"IDX{t}"], writes=[f"XD{e_}"])
                dma("sp", H2T[:, cs_].rearrange("(k p) n -> p k n", p=128), h2T[:], reads=["h2T"])
            T.barrier()

        aes.close()
        with ExitStack() as es:
            def sb(name, shape, dt=F32):
                return es.enter_context(nc.sbuf_tensor(_u(name), shape, dt))
            def ps(name, shape, dt=F32):
                return es.enter_context(nc.psum_tensor(_u(name), shape, dt))
            acc = sb("acc", [128, 16, 1024])
            Wg = [sb(f"Wg{i}", [128, 8, 1024], BF16) for i in range(2)]
            Wu = [sb(f"Wu{i}", [128, 8, 1024], BF16) for i in range(2)]
            Wds = [sb(f"Wd{i}", [128, 8, 1024], BF16) for i in range(2)]
            XT1 = sb("XT0", [128, 8, 512], BF16)
            XT = [XT1, XT1]
            xs = [sb(f"xs{i}", [128, 1024], BF16) for i in range(2)]
            BG = sb("BG", [128, 32, 8]); BU = sb("BU", [128, 32, 8])
            BD = sb("BD", [128, 1024], BF16)
            aT = sb("aT", [128, 8, 512], BF16)
            gb = [sb("g_0", [128, 512])] * 2; ub = [sb("u_0", [128, 512])] * 2
            sb_ = [sb("s_0", [128, 512])] * 2; u0b = [sb("u0_0", [128, 512])] * 2
            yo = [sb(f"yo{i}", [128, 512], BF16) for i in range(2)]
            tg = [sb(f"tg{i}", [128, 1024], BF16) for i in range(2)]
            ssF = sb("ssF", [128, 1]); ss2F = sb("ss2F", [128, 1])
            pgt = [ps(f"pgt{i}", [128, 512]) for i in range(2)]
            put = [ps(f"put{i}", [128, 512]) for i in range(2)]
            pdn = [ps(f"pdn{i}", [128, 512]) for i in range(2)]
            pT = ps("pTF", [128, 8, 128], BF16)
            dma("sp", BG[:].rearrange("p e k -> p (e k)"), bg_d, writes=["BG"])
            dma("sp", BU[:].rearrange("p e k -> p (e k)"), bu_d, writes=["BU"])
            op("pool", lambda e: e.memset(acc[:], 0.0), writes=["acc"])
            for i in range(2):
                op("pool", lambda e, i=i: e.memset(tg[i][:], 0.0), writes=[f"tg{i}"])

            def load_gu(e):
                sl = e % 2
                dma("pool", Wg[sl][:], wg_d[e].rearrange("(k p) n -> p k n", p=128), writes=[f"Wg{sl}"])
                dma("pool", Wu[sl][:], wu_d[e].rearrange("(k p) n -> p k n", p=128), writes=[f"Wu{sl}"])

            def load_d(e):
                dma("pool", Wds[e % 2][:], wd_d[e].rearrange("(k p) n -> p k n", p=128), writes=[f"Wd{e % 2}"])

            def load_bd(e):
                dma("pool", BD[:], bd_d[e:e + 1, :].to_broadcast([128, 1024]), writes=["BD"])

            xi = [0]

            def build_xt(e, c):
                for st in range(4):
                    x_ = xs[xi[0] % 2]; kx = f"xs{xi[0] % 2}"
                    xi[0] += 1
                    r0 = e * CAP + c * 512 + st * 128
                    dma("sp", x_[:], XD[r0:r0 + 128, :], writes=[kx])
                    for kc in range(8):
                        op("pe", lambda en: en.transpose(out=pT[:, kc, :], in_=x_[:, kc * 128:(kc + 1) * 128],
                                                        identity=identb[:]),
                           reads=[kx, "identb"], writes=["pTF"])
                    act(XT[c][:, :, st * 128:(st + 1) * 128], pT[:], AF.Copy, ["pTF"], ["XT0"])

            def GU(e, c):
                sl = e % 2
                h_ = XT[c]; kh = "XT0"

                def tail(fc):
                    i2 = 0
                    ts("dve", ub[i2][:], u0b[i2][:], -7.0, 7.0, ALU.max, ALU.min, [f"u0_{i2}"], [f"u_{i2}"])
                    stt("dve", aT[:, fc, :], ub[i2][:], 1.0, sb_[i2][:], ALU.add, ALU.mult,
                        [f"u_{i2}", f"s_{i2}"], ["aT"])

                for fc in range(8):
                    i2 = fc % 2
                    pg_ = pgt[i2]; kg = f"pgt{i2}"; pu_ = put[i2]; ku = f"put{i2}"
                    for kc in range(8):
                        mm(pg_[:], Wg[sl][:, kc, fc * 128:(fc + 1) * 128], h_[:, kc, :], kc == 0, kc == 7,
                           [f"Wg{sl}", kh], [kg])
                    for kc in range(8):
                        mm(pu_[:], Wu[sl][:, kc, fc * 128:(fc + 1) * 128], h_[:, kc, :], kc == 0, kc == 7,
                           [f"Wu{sl}", kh], [ku])
                    ts("dve", gb[0][:], pg_[:], BG[:, e, fc:fc + 1], 7.0, ALU.add, ALU.min, [kg, "BG"], ["g_0"])
                    act(u0b[0][:], pu_[:], AF.Identity, [ku, "BU"], ["u0_0"], bias=BU[:, e, fc:fc + 1])
                    act(sb_[0][:], gb[0][:], AF.Silu, ["g_0"], ["s_0"], scale=1.702)
                    tail(fc)
                    combine_step()

            yi = [0]

            def DN(e, c):
                for j in range(4):
                    r0 = e * CAP + c * 512 + j * 128
                    for half in range(2):
                        pd_ = pdn[half]; kd = f"pdn{half}"
                        for fc in range(8):
                            mm(pd_[:], aT[:, fc, j * 128:(j + 1) * 128], Wds[e % 2][:, fc, half * 512:(half + 1) * 512],
                               fc == 0, fc == 7, ["aT", f"Wd{e % 2}"], [kd])
                        y_ = yo[yi[0] % 2]; ky = f"yo{yi[0] % 2}"
                        yi[0] += 1
                        stt("dve", y_[:], pd_[:], 1.0 / 1.702, BD[:, half * 512:(half + 1) * 512], ALU.mult, ALU.add,
                            [kd, "BD"], [ky])
                        dma("sp", YD[r0:r0 + 128, half * 512:(half + 1) * 512], y_[:], reads=[ky],
                            writes=[f"YD{e}_{c * 8 + j * 2 + half}"])

            gi = [0]
            pend_g = []
            pend_a = []

            def combine(e):
                for i in range(16):
                    pend_g.append((e, i))

            def combine_step():
                if pend_a:
                    e, i, k = pend_a.pop(0)
                    stt("dve", acc[:, i, :], tg[k][:], RWT[:, i, e:e + 1], acc[:, i, :], ALU.mult, ALU.add,
                        [f"tg{k}", "acc"], ["acc"])
                if pend_g:
                    e, i = pend_g.pop(0)
                    k = gi[0] % 2
                    gi[0] += 1
                    t_ = tg[k]
                    T.dma_fn("pool", lambda g, t_=t_, i=i, e=e: g.indirect_dma_start(
                        out=t_[:, :], out_offset=None, in_=YD[:, :],
                        in_offset=bass.IndirectOffsetOnAxis(ap=IDX[:, i * 32 + e:i * 32 + e + 1], axis=0),
                        bounds_check=BCREG, oob_is_err=False),
                        reads=[f"YD{e}_{m}" for m in range(16)], writes=[f"tg{k}"])
                    pend_a.append((e, i, k))

            load_gu(0)
            load_d(0)
            load_bd(0)
            load_gu(1)
            load_d(1)
            build_xt(0, 0)
            for e in range(32):
                GU(e, 0)
                build_xt(e, 1)
                DN(e, 0)
                GU(e, 1)
                if e + 2 < 32:
                    load_gu(e + 2)
                if e + 1 < 32:
                    build_xt(e + 1, 0)
                DN(e, 1)
                if e + 2 < 32:
                    load_d(e + 2)
                if e + 1 < 32:
                    load_bd(e + 1)
                combine(e)
            while pend_g or pend_a:
                combine_step()
            T.barrier()
            xv = Wg[0][:].rearrange("p k n -> p (k n)").bitcast(F32)
            sqv = Wu[0][:].rearrange("p k n -> p (k n)")
            for t in range(16):
                x1v = xv[:, (t % 2) * 2048:(t % 2) * 2048 + 1024]; ov = xv[:, (t % 2) * 2048 + 1024:(t % 2) * 2048 + 2048]
                kx = f"x1v{t % 2}"; ko = f"ov{t % 2}"
                dma("sp", x1v, X1[t * 128:(t + 1) * 128, :], writes=[kx])
                act(sqv[:, 0:1024], acc[:, t, :], AF.Square, ["acc"], ["sqv", "ssF"], accum_out=ssF[:])
                ts("dve", ss2F[:], ssF[:], 1.0 / 1024, 1e-6, ALU.mult, ALU.add, ["ssF"], ["ss2F"])
                act(ss2F[:], ss2F[:], AF.Sqrt, ["ss2F"], ["ss2F"])
                op("dve", lambda e: e.reciprocal(out=ss2F[:], in_=ss2F[:]), reads=["ss2F"], writes=["ss2F"])
                stt("dve", ov, acc[:, t, :], ss2F[:, 0:1], G2[:], ALU.mult, ALU.mult, ["acc", "ss2F", "G2"], [ko])
                tt("dve", ov, ov, x1v, ALU.add, [ko, kx], [ko])
                dma("sp", out_d[t * 128:(t + 1) * 128, :], ov, reads=[ko])
            T.finish("sp")
    return nc


_NC = None


def _bf16(a):
    return a.astype(ml_dtypes.bfloat16)


def kernel(**inp):
    global _NC
    f32 = np.float32
    x = np.asarray(inp["x"], f32)
    c = np.asarray(inp["c"], f32)
    positions = np.asarray(inp["positions"]).astype(np.int32)
    sq = lambda k: np.asarray(inp[k])[0]
    if _NC is None:
        _NC = build_program()
    nc = _NC
    gains = np.concatenate([sq("mix_pre_g"), sq("mix_post_g"), sq("ffn_pre_g"), sq("ffn_post_g")])[None, :].astype(f32)
    p = np.arange(128)
    inv_freq = (10000.0 ** (-np.arange(32, dtype=np.float32) / np.float32(32))).astype(f32)
    cst = np.zeros((128, 16), f32)
    sgn_r = np.where((p % 64) < 32, -1.0, 1.0).astype(f32)
    shalf = np.where(p < 64, 1.0, -1.0).astype(f32)
    cst[:, 1] = sgn_r
    cst[:, 2] = np.pi / 2
    cst[:, 3] = inv_freq[p % 32]
    cst[:, 4] = (p < 64)
    cst[:, 5] = -(p >= 64).astype(f32)
    cst[:, 6] = shalf * 0.999999
    cst[:, 7] = -1.0
    cst[:, 8] = -(p < 64).astype(f32)
    cst[:, 9] = 1.0
    cst[:, 10] = -TWO_PI_LO
    cst[:, 11] = -TWO_PI_LO * shalf
    cst[:, 13] = 12582912.0
    cst[:, 12] = shalf
    ident = np.eye(128, dtype=f32)
    iota = np.arange(1024, dtype=f32)[None, :]
    lre = np.concatenate([sq("ssm_lam_re").T, sq("ssm_lam_re").T], 0).astype(f32)
    lim = np.concatenate([sq("ssm_lam_im").T, sq("ssm_lam_im").T], 0).astype(f32)
    ldt = sq("ssm_log_dt")[None, :].astype(f32)
    bre = sq("ssm_b_re"); bim = sq("ssm_b_im")
    bl1 = np.zeros((128, 32, 128), f32); bl2 = np.zeros((128, 32, 128), f32)
    for g in range(32):
        r0 = ((g % 8) // 2) * 32 + (g % 2) * 16
        bl1[r0:r0 + 16, g, 0:64] = bre[g].T; bl1[r0:r0 + 16, g, 64:128] = bim[g].T
        bl2[r0:r0 + 16, g, 0:64] = bim[g].T; bl2[r0:r0 + 16, g, 64:128] = bre[g].T
    cre = sq("ssm_c_re"); cim = sq("ssm_c_im")
    crp = np.transpose(cre, (2, 0, 1)); cip = np.transpose(cim, (2, 0, 1))
    cr = np.concatenate([crp, crp], 0).reshape(128, 512).astype(f32)
    ci = np.concatenate([cip, cip], 0).reshape(128, 512).astype(f32)
    dsk = sq("ssm_d").reshape(4, 128).T.copy().astype(f32)
    bonehot = np.zeros((32, 8192), f32)
    for n in range(32):
        bonehot[n, n * 256:(n + 1) * 256] = 1.0
    bonehot = _bf16(bonehot)
    dmask = np.ones((128, 4, 512), f32)
    kk = np.arange(128)[:, None]; qq = np.arange(128)[None, :]
    for i in range(4):
        for j in range(4):
            if i // 2 == j // 2:
                if i == j:
                    dmask[:, i, j * 128:(j + 1) * 128] = (kk <= qq)
                elif i > j:
                    dmask[:, i, j * 128:(j + 1) * 128] = 0.0
    dmask = _bf16(dmask.reshape(128, 2048))
    bg = np.transpose(sq("b_gate").reshape(32, 8, 128), (2, 0, 1)).reshape(128, 256).astype(f32)
    bu = np.transpose(sq("b_up").reshape(32, 8, 128), (2, 0, 1)).reshape(128, 256).astype(f32)
    ltri = _bf16((np.arange(128)[:, None] < np.arange(128)[None, :]).astype(f32))
    eoff = (np.arange(32, dtype=f32) * 1024.0 - 40000.0)[None, :]
    shared = {
        "ltri": ltri, "eoff": eoff,
        "ada_w": sq("ada_w"), "ada_b": sq("ada_b")[None, :], "gains": gains, "w_in": sq("w_in"),
        "cst": cst, "ident": ident, "iota": iota, "lre": lre, "lim": lim, "ldt": ldt,
        "bl1": bl1.reshape(128, 4096), "bl2": bl2.reshape(128, 4096), "cr": cr, "ci": ci, "dsk": dsk,
        "w_glu": sq("ssm_w_glu"), "w_sb": sq("w_ssm_branch"), "w_ab": sq("w_attn_branch"), "w_out": sq("w_out"),
        "bonehot": bonehot, "dmask": dmask, "router_w": sq("router_w"), "router_b": sq("router_b")[None, :],
        "w_gate": sq("w_gate"), "w_up": sq("w_up"), "w_down": sq("w_down"),
        "b_gate": bg, "b_up": bu, "b_down": sq("b_down"),
    }
    shared = {k: np.ascontiguousarray(v) for k, v in shared.items()}
    in_maps = []
    for core in range(8):
        b, j = core // 4, core % 4
        nprev = j * 2048
        x_loc = np.zeros((8192, 1024), f32)
        x_loc[6144 - nprev:] = x[b, :nprev + 2048]
        tm = np.zeros(8192, f32); tm[6144 - nprev:] = 1.0
        pos = np.zeros(8192, np.int32); pos[6144 - nprev:] = positions[b, :nprev + 2048]
        ninv = (3 - j) * 8
        vb = np.zeros((8, 96), f32); vv = np.zeros((8, 96), f32); vo = np.zeros((8, 96), f32)
        for qb in range(8):
            n = np.arange(32)
            valid = (n >= ninv) & (n < 24 + qb)
            vb[qb, 64:96] = np.where(valid, 0.0, -1e30)
            vv[qb, 64:96] = valid
            vo[qb, 64 + 24 + qb] = 1.0
        m = dict(shared)
        m.update({
            "x_loc": x_loc, "tmask": np.ascontiguousarray(tm.reshape(64, 128).T),
            "pos_loc": pos[None, :], "c_b": np.ascontiguousarray(c[b].reshape(8, 128).T),
            "vbias": vb.reshape(1, 768), "vvalid": vv.reshape(1, 768), "vown": vo.reshape(1, 768),
        })
        in_maps.append(m)
    res = run_bass_kernel_spmd(nc, in_maps, core_ids=list(range(8)))
    out = np.zeros((2, 8192, 1024), f32)
    for core in range(8):
        b, j = core // 4, core % 4
        out[b, j * 2048:(j + 1) * 2048] = res.results[core]["out"]
    return out
```

```python
import numpy as np
from contextlib import ExitStack
import ml_dtypes
import concourse.bass as bass
import concourse.mybir as mybir
from concourse.bass_utils import run_bass_kernel_spmd

F32 = mybir.dt.float32
BF16 = mybir.dt.bfloat16
I32 = mybir.dt.int32
ALU = mybir.AluOpType
AF = mybir.ActivationFunctionType
AX = mybir.AxisListType
PI = float(np.pi)
TWO_PI = float(2 * np.pi)
PI_LO = 3.1415925
TWO_PI_LO = 6.283185
SEM_CAP = 30000
NEG = -30000.0


class Tracker:
    def __init__(self, nc):
        self.nc = nc
        self.engs = {"pe": nc.tensor, "act": nc.scalar, "dve": nc.vector,
                     "pool": nc.gpsimd, "sp": nc.sync}
        self.cur_sem = {}
        self.cnt = {}
        self.nsem = 0
        for e in ("pe", "act", "dve", "pool"):
            self._new_epoch(e)
        self.seen = {e: {} for e in self.engs}
        self.lastw = {}
        self.reads = {}
        self.NS = 8
        self.dq = {}
        for q in ("sp", "pool"):
            sems = [self._alloc(f"dq_{q}_{i}") for i in range(self.NS)]
            self.dq[q] = {"sems": sems, "i": 0, "last": {}}

    def _alloc(self, name):
        self.nsem += 1
        return self.nc.alloc_semaphore(name)

    def _new_epoch(self, e):
        self.cur_sem[e] = self._alloc(f"c_{e}_{self.nsem}")
        self.cnt[e] = 0

    def _wait(self, e, ev):
        if ev is None:
            return
        sem, val, src = ev
        if src == "pe" and e == "pe":
            return
        k = id(sem)
        old = self.seen[e].get(k)
        if old is not None and old >= val:
            return
        self.seen[e][k] = val
        self.engs[e].wait_ge(sem, val)

    def _deps(self, e, reads, writes):
        for r in reads:
            self._wait(e, self.lastw.get(r))
        for w in writes:
            self._wait(e, self.lastw.get(w))
            for ev in self.reads.get(w, ()):
                self._wait(e, ev)

    def _commit(self, ev, reads, writes):
        for r in reads:
            lst = self.reads.setdefault(r, [])
            lst.append(ev)
            if len(lst) > 24:
                d = {}
                for s, v, src in lst:
                    if id(s) not in d or d[id(s)][1] < v:
                        d[id(s)] = (s, v, src)
                self.reads[r] = list(d.values())
        for w in writes:
            self.lastw[w] = ev
            self.reads[w] = []

    def op(self, e, fn, reads=(), writes=()):
        self._deps(e, reads, writes)
        if self.cnt[e] >= SEM_CAP:
            self._new_epoch(e)
        ins = fn(self.engs[e])
        self.cnt[e] += 1
        ins.then_inc(self.cur_sem[e], 1)
        ev = (self.cur_sem[e], self.cnt[e], e)
        self._commit(ev, reads, writes)
        return ev

    def dma(self, q, out, in_, reads=(), writes=(), **kw):
        d = self.dq[q]
        i = d["i"]
        d["i"] += 1
        slot = i % self.NS
        sem = d["sems"][slot]
        prev = 16 * (i // self.NS)
        if prev + 16 > 2 * SEM_CAP:
            d["sems"] = [self._alloc(f"dq_{q}_{i}_{k}") for k in range(self.NS)]
            d["i"] = 1
            i = 0
            slot = 0
            sem = d["sems"][0]
            prev = 0
        if prev > 0:
            self._wait(q, (sem, prev, "dma"))
        self._deps(q, reads, writes)
        ins = self.engs[q].dma_start(out=out, in_=in_, **kw)
        ins.then_inc(sem, 16)
        ev = (sem, prev + 16, "dma")
        self._commit(ev, reads, writes)
        d["last"][id(sem)] = ev
        return ev

    def dma_fn(self, q, fn, reads=(), writes=()):
        d = self.dq[q]
        i = d["i"]
        d["i"] += 1
        slot = i % self.NS
        sem = d["sems"][slot]
        prev = 16 * (i // self.NS)
        if prev + 16 > 2 * SEM_CAP:
            d["sems"] = [self._alloc(f"dq_{q}_{i}_{k}") for k in range(self.NS)]
            d["i"] = 1
            slot = 0
            sem = d["sems"][0]
            prev = 0
        if prev > 0:
            self._wait(q, (sem, prev, "dma"))
        self._deps(q, reads, writes)
        ins = fn(self.engs[q])
        ins.then_inc(sem, 16)
        ev = (sem, prev + 16, "dma")
        self._commit(ev, reads, writes)
        d["last"][id(sem)] = ev
        return ev

    def _all_events(self):
        evs = []
        for e in ("pe", "act", "dve", "pool"):
            if self.cnt[e] > 0:
                evs.append((self.cur_sem[e], self.cnt[e], "bar"))
        for q, d in self.dq.items():
            for ev in d["last"].values():
                evs.append((ev[0], ev[1], "bar"))
        return evs

    def barrier(self):
        evs = self._all_events()
        for e in self.engs:
            for ev in evs:
                self._wait(e, ev)
        self.lastw.clear()
        self.reads.clear()

    def finish(self, eng="sp"):
        for ev in self._all_events():
            self._wait(eng, ev)


def build_program():
    nc = bass.Bass("TRN2", target_bir_lowering=False)

    def din(name, shape, dt=F32):
        return nc.dram_tensor(name, list(shape), dt, kind="ExternalInput").ap()

    def dscr(name, shape, dt):
        return nc.dram_tensor(name, list(shape), dt, kind="Internal").ap()

    x_loc = din("x_loc", [8192, 1024])
    tmask_d = din("tmask", [128, 64])
    pos_d = din("pos_loc", [1, 8192], I32)
    c_b = din("c_b", [128, 8])
    ada_w = din("ada_w", [1024, 6144])
    ada_b = din("ada_b", [1, 6144])
    gains = din("gains", [1, 4096])
    w_in = din("w_in", [1024, 4096])
    cst_d = din("cst", [128, 16])
    ident_d = din("ident", [128, 128])
    iota_d = din("iota", [1, 1024])
    lre_d = din("lre", [128, 32])
    lim_d = din("lim", [128, 32])
    ldt_d = din("ldt", [1, 32])
    bl1_d = din("bl1", [128, 32 * 128])
    bl2_d = din("bl2", [128, 32 * 128])
    cr_d = din("cr", [128, 512])
    ci_d = din("ci", [128, 512])
    dsk_d = din("dsk", [128, 4])
    wglu_d = din("w_glu", [512, 512])
    wsb_d = din("w_sb", [512, 1024])
    wab_d = din("w_ab", [512, 1024])
    wout_d = din("w_out", [1024, 1024])
    bonehot_d = din("bonehot", [32, 8192], BF16)
    dm_d = din("dmask", [128, 2048], BF16)
    vb_d = din("vbias", [1, 8 * 96])
    vv_d = din("vvalid", [1, 8 * 96])
    vo_d = din("vown", [1, 8 * 96])
    rw_d = din("router_w", [1024, 32])
    rb_d = din("router_b", [1, 32])
    wg_d = din("w_gate", [32, 1024, 1024])
    wu_d = din("w_up", [32, 1024, 1024])
    wd_d = din("w_down", [32, 1024, 1024])
    bg_d = din("b_gate", [128, 32 * 8])
    bu_d = din("b_up", [128, 32 * 8])
    bd_d = din("b_down", [32, 1024])
    ltri_d = din("ltri", [128, 128], BF16)
    eoff_d = din("eoff", [1, 32])
    out_d = nc.dram_tensor("out", [2048, 1024], F32, kind="ExternalOutput").ap()
    CAP = 1024
    XD = dscr("XD", [32 * CAP, 1024], BF16)
    YD = dscr("YD", [32 * CAP, 1024], BF16)

    UT = dscr("UT", [512, 8192], BF16)
    KT = dscr("KT", [512, 8192], BF16)
    VV = dscr("VV", [8192, 512], BF16)
    QT = dscr("QT", [512, 2048], BF16)
    GST = dscr("GST", [1024, 2048], BF16)
    GAT = dscr("GAT", [1024, 2048], BF16)
    YST = dscr("YST", [512, 2048], BF16)
    OAT = dscr("OAT", [512, 2048], BF16)
    X1 = dscr("X1", [2048, 1024], F32)
    H2T = dscr("H2T", [1024, 2048], BF16)

    T = Tracker(nc)
    BCREG = nc.gpsimd.to_reg(32 * 1024 - 1)
    _cnt = [0]

    def _u(n):
        _cnt[0] += 1
        return f'sb{_cnt[0]}_{n}'
    op = T.op
    dma = T.dma
    uid = [0]

    def mm(out, lhsT, rhs, start, stop, reads, writes):
        return op("pe", lambda e: e.matmul(out, lhsT=lhsT, rhs=rhs, start=start, stop=stop),
                  reads=reads, writes=writes)

    def act(out, in_, func, reads, writes, eng="act", **kw):
        return op(eng, lambda e: e.activation(out=out, in_=in_, func=func, **kw), reads=reads, writes=writes)

    def tt(eng, out, a, b, o, reads, writes):
        return op(eng, lambda e: e.tensor_tensor(out=out, in0=a, in1=b, op=o), reads=reads, writes=writes)

    def ts(eng, out, a, s1, s2, o0, o1, reads, writes):
        if o1 is None:
            return op(eng, lambda e: e.tensor_scalar(out=out, in0=a, scalar1=s1, scalar2=None, op0=o0),
                      reads=reads, writes=writes)
        return op(eng, lambda e: e.tensor_scalar(out=out, in0=a, scalar1=s1, scalar2=s2, op0=o0, op1=o1),
                  reads=reads, writes=writes)

    def stt(eng, out, a, s, b, o0, o1, reads, writes):
        return op(eng, lambda e: e.scalar_tensor_tensor(out=out, in0=a, scalar=s, in1=b, op0=o0, op1=o1),
                  reads=reads, writes=writes)

    def cp(eng, out, a, reads, writes):
        return op(eng, lambda e: e.tensor_copy(out=out, in_=a), reads=reads, writes=writes)


    MAGIC = 12582912.0
    INV2PI = float(1.0 / (2 * np.pi))

    def reduce_angle(x, kx, k, kk, r, kr, ab, kab):
        ts("dve", k, x, INV2PI, MAGIC, ALU.mult, ALU.add, [kx], [kk])
        ts("dve", k, k, -MAGIC, None, ALU.add, None, [kk], [kk])
        stt("dve", r, k, -TWO_PI, x, ALU.mult, ALU.add, [kk, kx], [kr])
        ts("dve", r, r, PI_LO, -PI_LO, ALU.min, ALU.max, [kr], [kr])
        act(ab, r, AF.Abs, [kr], [kab])

    with ExitStack() as gs:
        def gsb(name, shape, dt=F32):
            return gs.enter_context(nc.sbuf_tensor(_u(name), shape, dt))
        G2 = gsb("G2", [128, 1024])
        ident = gsb("ident", [128, 128])
        identb = gsb("identb", [128, 128], BF16)
        CST = gsb("CST", [128, 16])
        tmask = gsb("tmaskt", [128, 64])
        RWT = gsb("RWT", [128, 16, 32])
        IDX = gsb("IDX", [128, 512], I32)
        aes = ExitStack()
        PRM = aes.enter_context(nc.sbuf_tensor(_u("PRM"), [128, 5, 1024], F32))
        dma("sp", ident[:], ident_d, writes=["ident"])
        dma("sp", CST[:], cst_d, writes=["CST"])
        dma("sp", tmask[:], tmask_d, writes=["tmask"])
        cp("dve", identb[:], ident[:], ["ident"], ["identb"])
        SGNR = CST[:, 1:2]
        HALFPI = CST[:, 2:3]
        INVF = CST[:, 3:4]
        MLO = CST[:, 4:5]
        NMHI = CST[:, 5:6]
        SHALF = CST[:, 6:7]
        NEG1 = CST[:, 7:8]
        NMLO = CST[:, 8:9]
        ONE9 = CST[:, 9:10]
        N2PI = CST[:, 10:11]
        SC1 = CST[:, 11:12]
        SHALF1 = CST[:, 12:13]
        MAGICC = CST[:, 13:14]

        with ExitStack() as es:
            def sb(name, shape, dt=F32):
                return es.enter_context(nc.sbuf_tensor(_u(name), shape, dt))
            ct = sb("ct", [128, 8]); cs = sb("cs", [128, 8])
            CB = sb("CB", [128, 8, 128], BF16)
            AWb = sb("AWb", [128, 8, 512], BF16)
            ADAB = sb("ADAB", [128, 6144]); ADA = sb("ADA", [128, 6144])
            GB = sb("GB", [128, 4096])
            pa = es.enter_context(nc.psum_tensor(_u("pa"), [128, 512], F32))
            dma("sp", ct[:], c_b, writes=["ct"])
            dma("pool", ADAB[:], ada_b.to_broadcast([128, 6144]), writes=["ADAB"])
            dma("pool", GB[:], gains.to_broadcast([128, 4096]), writes=["GB"])
            act(cs[:], ct[:], AF.Silu, ["ct"], ["cs"])
            for kc in range(8):
                cp("dve", CB[:, kc, :], cs[:, kc:kc + 1].to_broadcast([128, 128]), ["cs"], ["CB"])
            for n in range(12):
                dma("pool", AWb[:], ada_w[:, n * 512:(n + 1) * 512].rearrange("(k p) n -> p k n", p=128),
                    writes=["AWb"])
                for kc in range(8):
                    mm(pa[:], CB[:, kc, :], AWb[:, kc, :], kc == 0, kc == 7, ["CB", "AWb"], ["pa"])
                tt("dve", ADA[:, n * 512:(n + 1) * 512], pa[:], ADAB[:, n * 512:(n + 1) * 512], ALU.add,
                   ["pa", "ADAB"], ["ADA"])
            stt("dve", PRM[:, 0, :], ADA[:, 1024:2048], 1.0, GB[:, 0:1024], ALU.add, ALU.mult, ["ADA", "GB"], ["PRM"])
            cp("dve", PRM[:, 1, :], ADA[:, 0:1024], ["ADA"], ["PRM"])
            tt("dve", PRM[:, 2, :], ADA[:, 2048:3072], GB[:, 1024:2048], ALU.mult, ["ADA", "GB"], ["PRM"])
            stt("dve", PRM[:, 3, :], ADA[:, 4096:5120], 1.0, GB[:, 2048:3072], ALU.add, ALU.mult, ["ADA", "GB"], ["PRM"])
            cp("dve", PRM[:, 4, :], ADA[:, 3072:4096], ["ADA"], ["PRM"])
            tt("dve", G2[:], ADA[:, 5120:6144], GB[:, 3072:4096], ALU.mult, ["ADA", "GB"], ["G2"])
            T.barrier()

        def norm_mod(es_sb, xt, key_x, a_idx, sh_idx, maskcol, hb, key_hb, tmp, tmp2, ss, rs, sq):
            act(sq[:], xt, AF.Square, [key_x], ["sq", "ss"], accum_out=ss[:])
            ts("dve", rs[:], ss[:], 1.0 / 1024, 1e-6, ALU.mult, ALU.add, ["ss"], ["rs"])
            act(rs[:], rs[:], AF.Sqrt, ["rs"], ["rs"])
            op("dve", lambda e: e.reciprocal(out=rs[:], in_=rs[:]), reads=["rs"], writes=["rs"])
            stt("dve", tmp[:], xt, rs[:, 0:1], PRM[:, a_idx, :], ALU.mult, ALU.mult, [key_x, "rs", "PRM"], ["tmp"])
            tt("dve", tmp2[:], tmp[:], PRM[:, sh_idx, :], ALU.add, ["tmp", "PRM"], ["tmp2"])
            if maskcol is None:
                act(hb, tmp2[:], AF.Copy, ["tmp2"], [key_hb])
            else:
                act(hb, tmp2[:], AF.Copy, ["tmp2", "tmask"], [key_hb], scale=maskcol)

        with ExitStack() as es:
            def sb(name, shape, dt=F32):
                return es.enter_context(nc.sbuf_tensor(_u(name), shape, dt))
            def ps(name, shape, dt=F32):
                return es.enter_context(nc.psum_tensor(_u(name), shape, dt))
            WB = sb("WB", [128, 8, 5120], BF16)
            xt = sb("xt", [128, 1024]); tmp = sb("tmp", [128, 1024]); tmp2 = sb("tmp2", [128, 1024])
            sq = sb("sq", [128, 1024], BF16)
            ss = sb("ss", [128, 1]); rs = sb("rs", [128, 1])
            hb = sb("hb", [128, 1024], BF16)
            hTs = [sb(f"hT{i}", [128, 8, 512], BF16) for i in range(2)]
            posi = sb("posi", [128, 512], I32); posf = sb("posf", [128, 512])
            a1 = sb("a1", [128, 512]); a2 = sb("a2", [128, 512])
            cosT = sb("cosT", [128, 512]); sinS = sb("sinS", [128, 512])
            m1 = sb("m1", [128, 512]); m2 = sb("m2", [128, 512])
            ob = [sb(f"ob{i}", [128, 512], BF16) for i in range(3)]
            pT = ps("pT", [128, 8, 128], BF16)
            pk = ps("pk", [128, 512]); pks = ps("pks", [128, 512])
            pm = [ps(f"pm{i}", [128, 512]) for i in range(2)]

            blocks = [(0, 0, False), (512, 1024, False), (1024, 1024, True), (1536, 1536, False),
                      (2048, 512, False), (2560, 512, True), (3072, 2048, False), (3584, 2560, False),
                      (4096, 3072, False), (4608, 3584, False)]
            for dst, src, swp in blocks:
                if not swp:
                    dma("pool", WB[:, :, dst:dst + 512], w_in[:, src:src + 512].rearrange("(k p) n -> p k n", p=128),
                        writes=["WB"])
                else:
                    srcv = w_in[:, src:src + 512].rearrange("(k p) (h t j) -> p k h t j", p=128, h=8, t=2, j=32)
                    dstv = WB[:, :, dst:dst + 512].rearrange("p k (h t j) -> p k h t j", h=8, t=2, j=32)
                    for kc in range(8):
                        dma("pool", dstv[:, kc, :, 0, :], srcv[:, kc, :, 1, :], writes=["WB"])
                        dma("pool", dstv[:, kc, :, 1, :], srcv[:, kc, :, 0, :], writes=["WB"])

            T.barrier()
            obi = [0]

            def emit(src_ps, key_ps, dst_ap, func=AF.Copy, **kw):
                o = ob[obi[0] % 3]
                k = f"ob{obi[0] % 3}"
                obi[0] += 1
                act(o[:], src_ps, func, [key_ps], [k], **kw)
                dma("sp", dst_ap, o[:], reads=[k])

            for c in range(16):
                own = c >= 12
                hT = hTs[c % 2]; khT = f"hT{c % 2}"
                for j in range(4):
                    t = 4 * c + j
                    dma("sp", xt[:], x_loc[t * 128:(t + 1) * 128, :], writes=["xt"])
                    norm_mod(sb, xt[:], "xt", 0, 1, tmask[:, t:t + 1], hb[:], "hb", tmp, tmp2, ss, rs, sq)
                    for kc in range(8):
                        op("pe", lambda e: e.transpose(out=pT[:, kc, :], in_=hb[:, kc * 128:(kc + 1) * 128],
                                                       identity=identb[:]),
                           reads=["hb", "identb"], writes=["pT"])
                    cp("dve", hT[:, :, j * 128:(j + 1) * 128], pT[:], ["pT"], [khT])
                dma("pool", posi[:], pos_d[0:1, c * 512:(c + 1) * 512].to_broadcast([128, 512]), writes=["posi"])
                cp("dve", posf[:], posi[:], ["posi"], ["posf"])
                ts("dve", a1[:], posf[:], INVF, None, ALU.mult, None, ["posf", "CST"], ["a1"])
                reduce_angle(a1[:], "a1", a2[:], "a2", m1[:], "m1", m2[:], "m2")
                act(sinS[:], m1[:], AF.Sin, ["m1", "CST"], ["sinS"], scale=SGNR)
                act(cosT[:], m2[:], AF.Sin, ["m2", "CST"], ["cosT"], scale=NEG1, bias=HALFPI)

                def rope_proj(c0, c0s, dst, scale):
                    for cb in range(4):
                        for kc in range(8):
                            mm(pk[:], WB[:, kc, c0 + cb * 128:c0 + (cb + 1) * 128], hT[:, kc, :], kc == 0, kc == 7,
                               ["WB", khT], ["pk"])
                        for kc in range(8):
                            mm(pks[:], WB[:, kc, c0s + cb * 128:c0s + (cb + 1) * 128], hT[:, kc, :], kc == 0, kc == 7,
                               ["WB", khT], ["pks"])
                        tt("dve", m1[:], pk[:], cosT[:], ALU.mult, ["pk", "cosT"], ["m1"])
                        tt("dve", m2[:], pks[:], sinS[:], ALU.mult, ["pks", "sinS"], ["m2"])
                        tt("dve", m1[:], m1[:], m2[:], ALU.add, ["m1", "m2"], ["m1"])
                        emit(m1[:], "m1", dst(cb), scale=scale)

                rope_proj(512, 1024, lambda cb: KT[cb * 128:(cb + 1) * 128, c * 512:(c + 1) * 512], 1.0)
                for cb in range(4):
                    p = pm[cb % 2]; kp = f"pm{cb % 2}"
                    for kc in range(8):
                        mm(p[:], WB[:, kc, cb * 128:(cb + 1) * 128], hT[:, kc, :], kc == 0, kc == 7, ["WB", khT], [kp])
                    emit(p[:], kp, UT[cb * 128:(cb + 1) * 128, c * 512:(c + 1) * 512])
                for j in range(4):
                    p = pm[j % 2]; kp = f"pm{j % 2}"
                    for kc in range(8):
                        mm(p[:], hT[:, kc, j * 128:(j + 1) * 128], WB[:, kc, 1536:2048], kc == 0, kc == 7, ["WB", khT], [kp])
                    emit(p[:], kp, VV[(4 * c + j) * 128:(4 * c + j + 1) * 128, :])
                if own:
                    co = c - 12
                    rope_proj(2048, 2560, lambda cb: QT[cb * 128:(cb + 1) * 128, co * 512:(co + 1) * 512], 0.125)
                    for gi, (c0, dstT) in enumerate(((3072, GST), (4096, GAT))):
                        for db in range(8):
                            p = pm[db % 2]; kp = f"pm{db % 2}"
                            for kc in range(8):
                                mm(p[:], WB[:, kc, c0 + db * 128:c0 + (db + 1) * 128], hT[:, kc, :], kc == 0, kc == 7,
                                   ["WB", khT], [kp])
                            emit(p[:], kp, dstT[db * 128:(db + 1) * 128, co * 512:(co + 1) * 512], func=AF.Sigmoid)
            T.barrier()

        SEG = 1024
        NSEG = 8192 // SEG
        with ExitStack() as es:
            def sb(name, shape, dt=F32):
                return es.enter_context(nc.sbuf_tensor(_u(name), shape, dt))
            Y = sb("Y", [128, 4, 2048])
            LA = sb("LA", [128, 32, 128], BF16); LB = sb("LB", [128, 32, 128], BF16)
            BL1b = sb("BL1b", [128, 32, 128], BF16); BL2b = sb("BL2b", [128, 32, 128], BF16)
            TH = sb("TH", [128, 32]); RHO = sb("RHO", [128, 32]); CAR = sb("CAR", [128, 32])
            PHf = sb("PHf", [128, 32]); OFFT = sb("OFFT", [128, 8, 32]); NOFF = sb("NOFF", [128, 8, 32])
            SB1 = sb("SB1", [128, 8, 32]); SB2 = sb("SB2", [128, 8, 32])
            DSK = sb("DSK", [128, 4])
            dma("sp", DSK[:], dsk_d, writes=["DSK"])
            with ExitStack() as e2:
                def sb2(name, shape, dt=F32):
                    return e2.enter_context(nc.sbuf_tensor(_u(name), shape, dt))
                LRE = sb2("LRE", [128, 32]); LIM = sb2("LIM", [128, 32]); LDT = sb2("LDT", [128, 32])
                CR = sb2("CR", [128, 32, 16]); CI = sb2("CI", [128, 32, 16])
                w = [sb2(f"w{i}", [128, 32]) for i in range(10)]
                c1 = sb2("c1", [128, 32, 16]); c2 = sb2("c2", [128, 32, 16])
                cpr = sb2("cpr", [128, 32, 16]); cpi = sb2("cpi", [128, 32, 16])
                for src, dstb, kb in ((bl1_d, BL1b, "BL1b"), (bl2_d, BL2b, "BL2b")):
                    dma("pool", dstb[:].rearrange("p g m -> p (g m)"), src, writes=[kb])
                dma("sp", LRE[:], lre_d, writes=["LRE"])
                dma("sp", LIM[:], lim_d, writes=["LIM"])
                dma("pool", LDT[:], ldt_d.to_broadcast([128, 32]), writes=["LDT"])
                dma("sp", CR[:].rearrange("p g c -> p (g c)"), cr_d, writes=["CR"])
                dma("sp", CI[:].rearrange("p g c -> p (g c)"), ci_d, writes=["CI"])
                K = "tb"
                dt_ = w[0]
                act(dt_[:], LDT[:], AF.Exp, ["LDT"], [K])
                tt("dve", TH[:], LIM[:], dt_[:], ALU.mult, ["LIM", K], ["TH"])
                tt("dve", w[1][:], LRE[:], dt_[:], ALU.mult, ["LRE", K], [K])
                act(RHO[:], w[1][:], AF.Exp, [K], ["RHO"])
                reduce_angle(TH[:], "TH", w[2][:], K, w[3][:], K, w[9][:], K)
                act(w[4][:], w[3][:], AF.Sin, [K, "CST"], [K], scale=ONE9)
                act(w[5][:], w[9][:], AF.Sin, [K, "CST"], [K], scale=NEG1, bias=HALFPI)
                tt("dve", w[6][:], RHO[:], w[5][:], ALU.mult, ["RHO", K], [K])
                ts("dve", w[6][:], w[6][:], -1.0, None, ALU.add, None, [K], [K])
                tt("dve", w[7][:], RHO[:], w[4][:], ALU.mult, ["RHO", K], [K])
                tt("dve", w[8][:], LRE[:], LRE[:], ALU.mult, ["LRE"], [K])
                tt("dve", w[9][:], LIM[:], LIM[:], ALU.mult, ["LIM"], [K])
                tt("dve", w[8][:], w[8][:], w[9][:], ALU.add, [K], [K])
                op("dve", lambda e: e.reciprocal(out=w[8][:], in_=w[8][:]), reads=[K], writes=[K])
                tt("dve", w[0][:], w[6][:], LRE[:], ALU.mult, [K, "LRE"], [K])
                tt("dve", w[1][:], w[7][:], LIM[:], ALU.mult, [K, "LIM"], [K])
                tt("dve", w[0][:], w[0][:], w[1][:], ALU.add, [K], [K])
                tt("dve", w[2][:], w[0][:], w[8][:], ALU.mult, [K], [K])
                tt("dve", w[0][:], w[7][:], LRE[:], ALU.mult, [K, "LRE"], [K])
                tt("dve", w[1][:], w[6][:], LIM[:], ALU.mult, [K, "LIM"], [K])
                tt("dve", w[0][:], w[0][:], w[1][:], ALU.subtract, [K], [K])
                tt("dve", w[3][:], w[0][:], w[8][:], ALU.mult, [K], [K])
                qrb = w[2][:, :].unsqueeze(2).to_broadcast([128, 32, 16])
                qib = w[3][:, :].unsqueeze(2).to_broadcast([128, 32, 16])
                tt("dve", c1[:], CR[:], qrb, ALU.mult, ["CR", K], [K])
                tt("dve", c2[:], CI[:], qib, ALU.mult, ["CI", K], [K])
                tt("dve", cpr[:], c1[:], c2[:], ALU.subtract, [K], [K])
                tt("dve", c1[:], CR[:], qib, ALU.mult, ["CR", K], [K])
                tt("dve", c2[:], CI[:], qrb, ALU.mult, ["CI", K], [K])
                tt("dve", cpi[:], c1[:], c2[:], ALU.add, [K], [K])
                ts("dve", c1[:], cpr[:], MLO, None, ALU.mult, None, [K, "CST"], [K])
                stt("dve", c1[:], cpi[:], NMHI, c1[:], ALU.mult, ALU.add, [K, "CST"], [K])
                ts("dve", c2[:], cpi[:], NMLO, None, ALU.mult, None, [K, "CST"], [K])
                stt("dve", c2[:], cpr[:], NMHI, c2[:], ALU.mult, ALU.add, [K, "CST"], [K])
                op("pool", lambda e: e.memset(LA[:], 0.0), writes=["LA"])
                op("pool", lambda e: e.memset(LB[:], 0.0), writes=["LB"])
                for src, dst, kd in ((c1, LA, "LA"), (c2, LB, "LB")):
                    dv = dst[:].rearrange("p (a r) (s c) -> p a r s c", r=8, s=8, c=16)
                    sv = src[:].rearrange("p (a r) c -> p a r c", r=8)
                    for r in range(8):
                        cp("dve", dv[:, :, r, r, :], sv[:, :, r, :], [K], [kd])
                op("dve", lambda e: e.memset(CAR[:], 0.0), writes=["CAR"])
                ts("dve", w[0][:], TH[:], INV2PI, MAGIC, ALU.mult, ALU.add, ["TH"], [K])
                ts("dve", w[0][:], w[0][:], -MAGIC, None, ALU.add, None, [K], [K])
                stt("dve", PHf[:], TH[:], INV2PI, w[0][:], ALU.mult, ALU.subtract, ["TH", K], ["PHf"])
                for sg in range(8):
                    ts("dve", w[1][:], PHf[:], float(sg * 1024), None, ALU.mult, None, ["PHf"], [K])
                    ts("dve", w[2][:], w[1][:], MAGIC, None, ALU.add, None, [K], [K])
                    stt("dve", OFFT[:, sg, :], w[2][:], -MAGIC, w[1][:], ALU.add, ALU.subtract, [K], ["OFFT"])
                    ts("dve", OFFT[:, sg, :], OFFT[:, sg, :], -1.0, None, ALU.mult, None, ["OFFT"], ["OFFT"])
                ts("dve", NOFF[:].rearrange("p a g -> p (a g)"), OFFT[:].rearrange("p a g -> p (a g)"), -1.0, None,
                   ALU.mult, None, ["OFFT"], ["NOFF"])
                ts("dve", SB2[:].rearrange("p a g -> p (a g)"), OFFT[:].rearrange("p a g -> p (a g)"),
                   TWO_PI, None, ALU.mult, None, ["OFFT"], ["SB2"])
                ts("dve", SB1[:].rearrange("p a g -> p (a g)"), SB2[:].rearrange("p a g -> p (a g)"), SHALF1, None,
                   ALU.mult, None, ["SB2", "CST"], ["SB1"])
                T.barrier()

            with ExitStack() as e2:
                def sb2(name, shape, dt=F32):
                    return e2.enter_context(nc.sbuf_tensor(_u(name), shape, dt))
                def ps2(name, shape, dt=F32):
                    return e2.enter_context(nc.psum_tensor(_u(name), shape, dt))
                UTb = sb2("UTb", [128, 8192], BF16)
                IOT = sb2("IOT", [128, SEG])
                UB = sb2("UB", [128, SEG])
                U2 = [sb2(f"U_{i}", [128, SEG]) for i in range(2)]; KK2 = [sb2(f"KK{i}", [128, SEG]) for i in range(2)]
                NF2 = [sb2(f"NF{i}", [128, SEG]) for i in range(2)]; AB2 = [sb2(f"AB{i}", [128, SEG]) for i in range(2)]
                CO2 = [sb2(f"COS2{i}", [128, SEG]) for i in range(2)]; SS2 = [sb2(f"SINS{i}", [128, SEG]) for i in range(2)]
                SN2 = [sb2(f"SIN2{i}", [128, SEG]) for i in range(2)]
                m2s = [sb2(f"sm2{i}", [128, 512]) for i in range(2)]
                W = sb2("W", [128, SEG]); RB = sb2("RB", [128, SEG]); Z = sb2("Z", [128, SEG])
                A1b = sb2("A1b", [128, SEG], BF16); A2b = sb2("A2b", [128, SEG], BF16)
                py = [ps2(f"py{i}", [128, 512]) for i in range(4)]
                p1 = [ps2(f"p1{i}", [128, 512]) for i in range(2)]
                p2 = [ps2(f"p2{i}", [128, 512]) for i in range(2)]
                dma("pool", IOT[:], iota_d.to_broadcast([128, SEG]), writes=["IOT"])
                ZT = sb2("ZT", [128, 4096], BF16)
                op("pool", lambda e: e.memset(ZT[:], 0.0), writes=["ZT"])
                xdv = XD.rearrange("(p i) d -> p (i d)", p=128)
                NOWN = 2048 // SEG
                items = [(g, seg) for g in range(32) for seg in range(NSEG)]
                pi_ = [0]

                def stageA(n):
                    g, seg = items[n]
                    b_ = n % 2
                    ownseg = seg >= NSEG - NOWN
                    if seg == 0:
                        ts("dve", UB[:], IOT[:], PHf[:, g:g + 1], None, ALU.mult, None, ["IOT", "PHf"], ["UB"])
                    act(U2[b_][:], UB[:], AF.Identity, ["UB", "OFFT"], [f"U_{b_}"], bias=OFFT[:, seg, g:g + 1])
                    act(KK2[b_][:], U2[b_][:], AF.Identity, [f"U_{b_}", "CST"], [f"KK{b_}"], bias=MAGICC)
                    stt("dve", NF2[b_][:], KK2[b_][:], -MAGIC, U2[b_][:], ALU.add, ALU.subtract,
                        [f"KK{b_}", f"U_{b_}"], [f"NF{b_}"])
                    act(AB2[b_][:], NF2[b_][:], AF.Abs, [f"NF{b_}"], [f"AB{b_}"])
                    act(CO2[b_][:], AB2[b_][:], AF.Sin, [f"AB{b_}", "CST"], [f"COS2{b_}"], scale=N2PI, bias=HALFPI)
                    act(SS2[b_][:], NF2[b_][:], AF.Sin, [f"NF{b_}", "CST"], [f"SINS{b_}"], scale=SC1)
                    if ownseg:
                        act(SN2[b_][:], NF2[b_][:], AF.Sin, [f"NF{b_}", "CST"], [f"SIN2{b_}"], scale=N2PI)

                def stageB(n):
                    g, seg = items[n]
                    b_ = n % 2
                    cbk, gg = g // 8, g % 8
                    ownseg = seg >= NSEG - NOWN
                    base = min((gg // 2) * 32, 64)
                    kr = 32 if gg // 2 < 3 else 64
                    COS2 = CO2[b_]; SINS = SS2[b_]; SIN2 = SN2[b_]
                    if seg == 0:
                        if gg == 0:
                            dma("sp", UTb[:], UT[cbk * 128:(cbk + 1) * 128, :], writes=["UTb"])
                            if cbk == 0:
                                for zi in range(64):
                                    dma("sp", xdv[:, zi * 4096:(zi + 1) * 4096], ZT[:], reads=["ZT"], writes=[f"XDz{zi}"])
                        cp("dve", RB[:], RHO[:, g:g + 1].to_broadcast([128, SEG]), ["RHO"], ["RB"])
                    for ch in range(SEG // 512):
                        tok0 = seg * SEG + ch * 512
                        q1 = p1[pi_[0] % 2]; q2 = p2[pi_[0] % 2]; k1 = f"p1{pi_[0] % 2}"; k2 = f"p2{pi_[0] % 2}"
                        pi_[0] += 1
                        mm(q1[:], BL1b[base:base + kr, g, :], UTb[base:base + kr, tok0:tok0 + 512], True, True,
                           ["BL1b", "UTb"], [k1])
                        mm(q2[:], BL2b[base:base + kr, g, :], UTb[base:base + kr, tok0:tok0 + 512], True, True,
                           ["BL2b", "UTb"], [k2])
                        wv = W[:, ch * 512:(ch + 1) * 512]
                        m2 = m2s[ch % 2]; km2 = f"m2{ch % 2}"
                        tt("dve", wv, q1[:], COS2[:, ch * 512:(ch + 1) * 512], ALU.mult, [k1, f"COS2{b_}"], [f"W{ch}"])
                        tt("dve", m2[:], q2[:], SINS[:, ch * 512:(ch + 1) * 512], ALU.mult, [k2, f"SINS{b_}"], [km2])
                        tt("dve", wv, wv, m2[:], ALU.add, [f"W{ch}", km2], [f"W{ch}"])
                    op("dve", lambda e: e.tensor_tensor_scan(out=Z[:], data0=RB[:], data1=W[:],
                                                              initial=CAR[:, g:g + 1], op0=ALU.mult, op1=ALU.add),
                       reads=["RB", "W0", "W1", "CAR"], writes=["Z"])
                    cp("dve", CAR[:, g:g + 1], Z[:, SEG - 1:SEG], ["Z"], ["CAR"])
                    if ownseg:
                        tt("dve", A1b[:], Z[:], COS2[:], ALU.mult, ["Z", f"COS2{b_}"], ["A1b"])
                        tt("dve", A2b[:], Z[:], SIN2[:], ALU.mult, ["Z", f"SIN2{b_}"], ["A2b"])
                        so = seg - (NSEG - NOWN)
                        for ch in range(SEG // 512):
                            yi = so * (SEG // 512) + ch
                            mm(py[yi][:], LA[:, g, :], A1b[:, ch * 512:(ch + 1) * 512], gg == 0, False,
                               ["LA", "A1b"], [f"py{yi}"])
                            mm(py[yi][:], LB[:, g, :], A2b[:, ch * 512:(ch + 1) * 512], False, gg == 7,
                               ["LB", "A2b"], [f"py{yi}"])
                    if gg == 7 and seg == NSEG - 1:
                        for yi in range(4):
                            stt("dve", Y[:, cbk, yi * 512:(yi + 1) * 512], UTb[:, 6144 + yi * 512:6144 + (yi + 1) * 512],
                                DSK[:, cbk:cbk + 1], py[yi][:], ALU.mult, ALU.add, ["UTb", "DSK", f"py{yi}"], ["Y"])

                stageA(0)
                for n in range(len(items)):
                    if n + 1 < len(items):
                        stageA(n + 1)
                    stageB(n)
                T.barrier()

            with ExitStack() as e2:
                def sb2(name, shape, dt=F32):
                    return e2.enter_context(nc.sbuf_tensor(_u(name), shape, dt))
                def ps2(name, shape, dt=F32):
                    return e2.enter_context(nc.psum_tensor(_u(name), shape, dt))
                WGb = sb2("WGb", [128, 4, 512], BF16)
                t1 = sb2("gt1", [128, 2048]); t2 = sb2("gt2", [128, 2048])
                YGb = sb2("YGb", [128, 4, 2048], BF16)
                sgl = sb2("sgl", [128, 512]); ysb = [sb2(f"ysb{i}", [128, 512], BF16) for i in range(2)]
                pg = [ps2(f"pgl{i}", [128, 512]) for i in range(2)]
                dma("pool", WGb[:], wglu_d.rearrange("(k p) n -> p k n", p=128), writes=["WGb"])
                for cbk in range(4):
                    yv = Y[:, cbk, :]
                    act(t1[:], yv, AF.Square, ["Y"], ["t1"])
                    ts("dve", t1[:], t1[:], 0.044715, 1.0, ALU.mult, ALU.add, ["t1"], ["t1"])
                    tt("dve", t1[:], t1[:], yv, ALU.mult, ["t1", "Y"], ["t1"])
                    act(t2[:], t1[:], AF.Sigmoid, ["t1"], ["t2"], scale=1.5957691216057308)
                    tt("dve", yv, yv, t2[:], ALU.mult, ["Y", "t2"], ["Y"])
                    act(YGb[:, cbk, :], yv, AF.Copy, ["Y"], ["YGb"])
                i = 0
                for cbo in range(4):
                    for ch in range(4):
                        p = pg[i % 2]; kp = f"pgl{i % 2}"; o = ysb[i % 2]; ko = f"ysb{i % 2}"
                        i += 1
                        for kc in range(4):
                            mm(p[:], WGb[:, kc, cbo * 128:(cbo + 1) * 128], YGb[:, kc, ch * 512:(ch + 1) * 512],
                               kc == 0, kc == 3, ["WGb", "YGb"], [kp])
                        act(sgl[:], p[:], AF.Sigmoid, [kp], ["sgl"])
                        tt("dve", o[:], Y[:, cbo, ch * 512:(ch + 1) * 512], sgl[:], ALU.mult, ["Y", "sgl"], [ko])
                        dma("sp", YST[cbo * 128:(cbo + 1) * 128, ch * 512:(ch + 1) * 512], o[:], reads=[ko])
                T.barrier()

        with ExitStack() as es:
            def sb(name, shape, dt=F32):
                return es.enter_context(nc.sbuf_tensor(_u(name), shape, dt))
            def ps(name, shape, dt=F32):
                return es.enter_context(nc.psum_tensor(_u(name), shape, dt))
            KA = sb("KA", [128, 8192], BF16)
            VA = sb("VA", [128, 64, 65], BF16)
            QA = sb("QA", [128, 2048], BF16)
            DM = sb("DM", [128, 4, 512], BF16)
            VB = sb("VB", [128, 8, 96]); VVd = sb("VVd", [128, 8, 96]); VO = sb("VO", [128, 8, 96])
            KM = sb("KM", [64, 32]); KMb = sb("KMb", [64, 32], BF16)
            gt_ = sb("gt", [128, 96]); top8 = sb("top8", [128, 8]); sel = sb("sel", [128, 96])
            pt = [sb(f"pt{i}", [128, 512], BF16) for i in range(3)]
            rrow = sb("rrow", [128, 512]); rhi = sb("rhi", [128, 512], BF16); rlo = sb("rlo", [128, 512], BF16)
            rtmp = sb("rtmp", [128, 512])
            onesb = sb("onesb", [128, 64], BF16)
            bc = sb("bc", [64, 512]); oab = sb("oab", [64, 512], BF16)
            pS = [ps(f"pS{i}", [128, 512]) for i in range(3)]
            pO = [ps(f"pO{i}", [128, 512]) for i in range(2)]
            pG = ps("pG", [128, 96]); pB = ps("pB", [128, 128]); pO2 = ps("pO2", [64, 512])
            dma("sp", KA[64:96, :], bonehot_d, writes=["KAoh"])
            dma("sp", DM[:].rearrange("p a n -> p (a n)"), dm_d, writes=["DM"])
            dma("pool", VB[:].rearrange("p a n -> p (a n)"), vb_d.to_broadcast([128, 768]), writes=["VB"])
            dma("pool", VVd[:].rearrange("p a n -> p (a n)"), vv_d.to_broadcast([128, 768]), writes=["VVd"])
            dma("pool", VO[:].rearrange("p a n -> p (a n)"), vo_d.to_broadcast([128, 768]), writes=["VO"])
            op("pool", lambda e: e.memset(VA[:, :, 64:65], 1.0), writes=["VA1"])
            op("pool", lambda e: e.memset(onesb[:], 1.0), writes=["onesb"])
            op("pool", lambda e: e.memset(gt_[:], 0.0), writes=["gt"])
            itb = [0]
            for h in range(8):
                dma("sp", KA[0:64, :], KT[h * 64:(h + 1) * 64, :], writes=["KA"])
                dma("sp", QA[0:64, :], QT[h * 64:(h + 1) * 64, :], writes=["QA"])
                dma("pool", VA[:, :, 0:64], VV[:, h * 64:(h + 1) * 64].rearrange("(t p) d -> p t d", p=128),
                    writes=["VA"])
                op("dve", lambda e: e.tensor_reduce(out=KM[:], in_=KA[0:64, :].rearrange("p (n l) -> p n l", l=256),
                                                    axis=AX.X, op=ALU.add), reads=["KA"], writes=["KM"])
                cp("dve", KMb[:], KM[:], ["KM"], ["KMb"])
                for qt in range(16):
                    qb = qt // 2
                    mm(pG[:, 64:96], QA[0:64, qt * 128:(qt + 1) * 128], KMb[:], True, True, ["QA", "KMb"], ["pG"])
                    tt("dve", gt_[:, 64:96], pG[:, 64:96], VB[:, qb, 64:96], ALU.add, ["pG", "VB"], ["gt"])
                    op("dve", lambda e: e.max(out=top8[:], in_=gt_[:, 64:96]), reads=["gt"], writes=["top8"])
                    ts("dve", sel[:, 64:96], gt_[:, 64:96], top8[:, 2:3], None, ALU.is_ge, None, ["gt", "top8"], ["sel"])
                    tt("dve", sel[:, 64:96], sel[:, 64:96], VVd[:, qb, 64:96], ALU.mult, ["sel", "VVd"], ["sel"])
                    tt("dve", sel[:, 64:96], sel[:, 64:96], VO[:, qb, 64:96], ALU.add, ["sel", "VO"], ["sel"])
                    ts("dve", gt_[:, 64:96], sel[:, 64:96], -1.0, -NEG, ALU.add, ALU.mult, ["sel"], ["gt"])
                    op("pe", lambda e: e.transpose(out=pB[0:96, :], in_=gt_[:, 0:96], identity=ident[:]),
                       reads=["gt", "ident"], writes=["pB"])
                    cp("dve", QA[64:96, qt * 128:(qt + 1) * 128], pB[64:96, :], ["pB"], ["QAb"])
                for G in range(4):
                    cnt = 52 + 4 * G
                    po = pO[G % 2]; kpo = f"pO{G % 2}"

                    def qk(kt):
                        i3 = (itb[0] + kt) % 3
                        mm(pS[i3][:], KA[0:96, kt * 128:(kt + 1) * 128], QA[0:96, G * 512:(G + 1) * 512], True, True,
                           ["KA", "KAoh", "QA", "QAb"], [f"pS{i3}"])
                    qk(0)
                    qk(1)
                    for kt in range(cnt):
                        i3 = (itb[0] + kt) % 3
                        s_ = pS[i3]; ks = f"pS{i3}"; p_ = pt[i3]; kp = f"pt{i3}"
                        if kt + 2 < cnt:
                            qk(kt + 2)
                        act(p_[:], s_[:], AF.Exp, [ks], [kp])
                        di = kt - (cnt - 4)
                        if di >= 0:
                            tt("dve", p_[:], p_[:], DM[:, di, :], ALU.mult, [kp, "DM"], [kp])
                        mm(po[0:65, :], VA[:, kt, 0:65], p_[:], kt == 0, kt == cnt - 1, ["VA", "VA1", kp], [kpo])
                    itb[0] += cnt
                    cp("dve", rrow[64:65, :], po[64:65, :], [kpo], ["rrow"])
                    op("dve", lambda e: e.reciprocal(out=rrow[64:65, :], in_=rrow[64:65, :]), reads=["rrow"], writes=["rrow"])
                    cp("dve", rhi[64:65, :], rrow[64:65, :], ["rrow"], ["rhi"])
                    cp("dve", rtmp[64:65, :], rhi[64:65, :], ["rhi"], ["rtmp"])
                    tt("dve", rtmp[64:65, :], rrow[64:65, :], rtmp[64:65, :], ALU.subtract, ["rrow", "rtmp"], ["rtmp"])
                    cp("dve", rlo[64:65, :], rtmp[64:65, :], ["rtmp"], ["rlo"])
                    mm(pO2[:], onesb[64:65, 0:64], rhi[64:65, :], True, False, ["onesb", "rhi"], ["pO2"])
                    mm(pO2[:], onesb[64:65, 0:64], rlo[64:65, :], False, True, ["onesb", "rlo"], ["pO2"])
                    cp("dve", bc[:], pO2[:], ["pO2"], ["bc"])
                    tt("dve", oab[:], po[0:64, :], bc[:], ALU.mult, [kpo, "bc"], ["oab"])
                    dma("sp", OAT[h * 64:(h + 1) * 64, G * 512:(G + 1) * 512], oab[:], reads=["oab"])
            T.barrier()

        with ExitStack() as es:
            def sb(name, shape, dt=F32):
                return es.enter_context(nc.sbuf_tensor(_u(name), shape, dt))
            def ps(name, shape, dt=F32):
                return es.enter_context(nc.psum_tensor(_u(name), shape, dt))
            Wsb = sb("Wsb", [128, 4, 1024], BF16); Wab = sb("Wab", [128, 4, 1024], BF16)
            Wo = sb("Wo", [128, 8, 1024], BF16)
            ys = sb("ys", [128, 4, 512], BF16); oa = sb("oa", [128, 4, 512], BF16)
            gsT = sb("gsT", [128, 8, 512], BF16); gaT = sb("gaT", [128, 8, 512], BF16)
            MT = sb("MT", [128, 8, 512], BF16)
            b1 = sb("b1", [128, 512]); b2 = sb("b2", [128, 512])
            xt = sb("xt2", [128, 1024]); x1 = sb("x1", [128, 1024]); tmp = sb("tmpE", [128, 1024]); tmp2 = sb("tmp2E", [128, 1024])
            sq = sb("sqE", [128, 1024], BF16); ss = sb("ssE", [128, 1]); ss2 = sb("ss2E", [128, 1]); rs = sb("rsE", [128, 1])
            hbs = [sb(f"hbE{i}", [128, 1024], BF16) for i in range(2)]; h2T = sb("h2T", [128, 8, 512], BF16)
            RWb = sb("RWb", [128, 8, 32], BF16); RBb = sb("RBb", [128, 32]); LT = sb("LT", [128, 128], BF16)
            ONESb = sb("ONESb", [128, 128], BF16); CNT = sb("CNT", [128, 32]); EOFF = sb("EOFF", [128, 32])
            mb = sb("mb", [128, 32], BF16); posf = sb("posfE", [128, 32]); idxf = sb("idxf", [128, 32])
            lg = sb("lg", [128, 32]); top8 = sb("top8F", [128, 8]); msk = sb("msk", [128, 32]); ex = sb("ex", [128, 32])
            nmx = sb("nmx", [128, 1]); sm = sb("sm", [128, 1])
            pl = ps("pl", [128, 32]); pp = ps("pp", [128, 32]); pc = ps("pc", [128, 32])
            dma("pool", RWb[:], rw_d.rearrange("(k p) n -> p k n", p=128), writes=["RWb"])
            dma("pool", RBb[:], rb_d.to_broadcast([128, 32]), writes=["RBb"])
            dma("pool", EOFF[:], eoff_d.to_broadcast([128, 32]), writes=["EOFF"])
            dma("sp", LT[:], ltri_d, writes=["LT"])
            op("pool", lambda e: e.memset(ONESb[:], 1.0), writes=["ONESb"])
            op("pool", lambda e: e.memset(CNT[:], 0.0), writes=["CNT"])
            pb1 = ps("pb1", [128, 512]); pb2 = ps("pb2", [128, 512])
            pmx = ps("pmx", [128, 1024]); pT = ps("pTE", [128, 8, 128], BF16)
            for (src, dst, kd, nk) in ((wsb_d, Wsb, "Wsb", 4), (wab_d, Wab, "Wab", 4), (wout_d, Wo, "Wo", 8)):
                dma("pool", dst[:], src.rearrange("(k p) n -> p k n", p=128), writes=[kd])
            for c in range(4):
                cs_ = slice(c * 512, (c + 1) * 512)
                dma("sp", ys[:], YST[:, cs_].rearrange("(k p) n -> p k n", p=128), writes=["ys"])
                dma("sp", oa[:], OAT[:, cs_].rearrange("(k p) n -> p k n", p=128), writes=["oa"])
                dma("pool", gsT[:], GST[:, cs_].rearrange("(k p) n -> p k n", p=128), writes=["gsT"])
                dma("pool", gaT[:], GAT[:, cs_].rearrange("(k p) n -> p k n", p=128), writes=["gaT"])
                for db in range(8):
                    for kc in range(4):
                        mm(pb1[:], Wsb[:, kc, db * 128:(db + 1) * 128], ys[:, kc, :], kc == 0, kc == 3, ["Wsb", "ys"], ["pb1"])
                    for kc in range(4):
                        mm(pb2[:], Wab[:, kc, db * 128:(db + 1) * 128], oa[:, kc, :], kc == 0, kc == 3, ["Wab", "oa"], ["pb2"])
                    tt("dve", b1[:], pb1[:], gsT[:, db, :], ALU.mult, ["pb1", "gsT"], ["b1"])
                    tt("dve", b2[:], pb2[:], gaT[:, db, :], ALU.mult, ["pb2", "gaT"], ["b2"])
                    tt("dve", MT[:, db, :], b1[:], b2[:], ALU.add, ["b1", "b2"], ["MT"])
                for j in range(4):
                    t = 4 * c + j
                    for half in range(2):
                        for kc in range(8):
                            mm(pmx[:, half * 512:(half + 1) * 512], MT[:, kc, j * 128:(j + 1) * 128],
                               Wo[:, kc, half * 512:(half + 1) * 512], kc == 0, kc == 7, ["MT", "Wo"], ["pmx"])
                    dma("sp", xt[:], x_loc[6144 + t * 128:6144 + (t + 1) * 128, :], writes=["xt"])
                    act(sq[:, 0:512], pmx[:, 0:512], AF.Square, ["pmx"], ["sq", "ss"], accum_out=ss[:])
                    act(sq[:, 512:1024], pmx[:, 512:1024], AF.Square, ["pmx"], ["sq", "ss2"], accum_out=ss2[:])
                    tt("dve", ss[:], ss[:], ss2[:], ALU.add, ["ss", "ss2"], ["ss"])
                    ts("dve", rs[:], ss[:], 1.0 / 1024, 1e-6, ALU.mult, ALU.add, ["ss"], ["rs"])
                    act(rs[:], rs[:], AF.Sqrt, ["rs"], ["rs"])
                    op("dve", lambda e: e.reciprocal(out=rs[:], in_=rs[:]), reads=["rs"], writes=["rs"])
                    cp("dve", tmp[:], pmx[:], ["pmx"], ["tmp"])
                    stt("dve", tmp[:], tmp[:], rs[:, 0:1], PRM[:, 2, :], ALU.mult, ALU.mult, ["tmp", "rs", "PRM"], ["tmp"])
                    tt("dve", x1[:], tmp[:], xt[:], ALU.add, ["tmp", "xt"], ["x1"])
                    dma("sp", X1[t * 128:(t + 1) * 128, :], x1[:], reads=["x1"])
                    hb = hbs[t % 2]; khb = f"hb{t % 2}"
                    norm_mod(sb, x1[:], "x1", 3, 4, None, hb[:], khb, tmp, tmp2, ss, rs, sq)
                    for kc in range(8):
                        op("pe", lambda e: e.transpose(out=pT[:, kc, :], in_=hb[:, kc * 128:(kc + 1) * 128],
                                                       identity=identb[:]),
                           reads=[khb, "identb"], writes=["pT"])
                    cp("dve", h2T[:, :, j * 128:(j + 1) * 128], pT[:], ["pT"], ["h2T"])
                    for kc in range(8):
                        mm(pl[:], h2T[:, kc, j * 128:(j + 1) * 128], RWb[:, kc, :], kc == 0, kc == 7, ["h2T", "RWb"], ["pl"])
                    tt("dve", lg[:], pl[:], RBb[:], ALU.add, ["pl", "RBb"], ["lg"])
                    op("dve", lambda e: e.max(out=top8[:], in_=lg[:]), reads=["lg"], writes=["top8"])
                    ts("dve", msk[:], lg[:], top8[:, 3:4], None, ALU.is_ge, None, ["lg", "top8"], ["msk"])
                    ts("dve", nmx[:], top8[:, 0:1], -1.0, None, ALU.mult, None, ["top8"], ["nmx"])
                    act(ex[:], lg[:], AF.Exp, ["lg", "nmx"], ["ex"], bias=nmx[:, 0:1])
                    tt("dve", ex[:], ex[:], msk[:], ALU.mult, ["ex", "msk"], ["ex"])
                    op("dve", lambda e: e.reduce_sum(out=sm[:], in_=ex[:], axis=AX.X), reads=["ex"], writes=["sm"])
                    op("dve", lambda e: e.reciprocal(out=sm[:], in_=sm[:]), reads=["sm"], writes=["sm"])
                    ts("dve", RWT[:, t, :], ex[:], sm[:, 0:1], None, ALU.mult, None, ["ex", "sm"], ["RWT"])
                    cp("dve", mb[:], msk[:], ["msk"], ["mb"])
                    mm(pp[:], LT[:], mb[:], True, True, ["LT", "mb"], ["pp"])
                    mm(pc[:], ONESb[:], mb[:], True, True, ["ONESb", "mb"], ["pc"])
                    tt("dve", posf[:], pp[:], CNT[:], ALU.add, ["pp", "CNT"], ["posf"])
                    tt("dve", CNT[:], CNT[:], pc[:], ALU.add, ["CNT", "pc"], ["CNT"])
                    ts("dve", idxf[:], posf[:], float(CAP), None, ALU.is_lt, None, ["posf"], ["idxf"])
                    tt("dve", msk[:], msk[:], idxf[:], ALU.mult, ["msk", "idxf"], ["msk"])
                    tt("dve", idxf[:], posf[:], EOFF[:], ALU.add, ["posf", "EOFF"], ["idxf"])
                    tt("dve", idxf[:], idxf[:], msk[:], ALU.mult, ["idxf", "msk"], ["idxf"])
                    ts("dve", idxf[:], idxf[:], 40000.0, None, ALU.add, None, ["idxf"], ["idxf"])
                    cp("dve", IDX[:, t * 32:(t + 1) * 32], idxf[:], ["idxf"], [f"IDX{t}"])
                    for e_ in range(32):
                        T.dma_fn("pool", lambda g, e_=e_, hb=hb: g.indirect_dma_start(
                            out=XD[:, :], out_offset=bass.IndirectOffsetOnAxis(ap=IDX[:, t * 32 + e_:t * 32 + e_ + 1], axis=0),
                            in_=hb[:, :], in_offset=None, bounds_check=BCREG, oob_is_err=False),
                            reads=[khb, f"IDX{t}"], writes=[f"XD{e_}"])
                dma("sp", H2T[:, cs_].rearrange("(k p) n -> p k n", p=128), h2T[:], reads=["h2T"])
            T.barrier()

        aes.close()
        with ExitStack() as es:
            def sb(name, shape, dt=F32):
                return es.enter_context(nc.sbuf_tensor(_u(name), shape, dt))
            def ps(name, shape, dt=F32):
                return es.enter_context(nc.psum_tensor(_u(name), shape, dt))
            acc = sb("acc", [128, 16, 1024])
            Wg = [sb(f"Wg{i}", [128, 8, 1024], BF16) for i in range(2)]
            Wu = [sb(f"Wu{i}", [128, 8, 1024], BF16) for i in range(2)]
            Wds = [sb(f"Wd{i}", [128, 8, 1024], BF16) for i in range(2)]
            XT1 = sb("XT0", [128, 8, 512], BF16)
            XT = [XT1, XT1]
            xs = [sb(f"xs{i}", [128, 1024], BF16) for i in range(2)]
            BG = sb("BG", [128, 32, 8]); BU = sb("BU", [128, 32, 8])
            BD = sb("BD", [128, 1024], BF16)
            aT = sb("aT", [128, 8, 512], BF16)
            gb = [sb("g_0", [128, 512])] * 2; ub = [sb("u_0", [128, 512])] * 2
            sb_ = [sb("s_0", [128, 512])] * 2; u0b = [sb("u0_0", [128, 512])] * 2
            yo = [sb(f"yo{i}", [128, 512], BF16) for i in range(2)]
            tg = [sb(f"tg{i}", [128, 1024], BF16) for i in range(2)]
            ssF = sb("ssF", [128, 1]); ss2F = sb("ss2F", [128, 1])
            pgt = [ps(f"pgt{i}", [128, 512]) for i in range(2)]
            put = [ps(f"put{i}", [128, 512]) for i in range(2)]
            pdn = [ps(f"pdn{i}", [128, 512]) for i in range(2)]
            pT = ps("pTF", [128, 8, 128], BF16)
            dma("sp", BG[:].rearrange("p e k -> p (e k)"), bg_d, writes=["BG"])
            dma("sp", BU[:].rearrange("p e k -> p (e k)"), bu_d, writes=["BU"])
            op("pool", lambda e: e.memset(acc[:], 0.0), writes=["acc"])
            for i in range(2):
                op("pool", lambda e, i=i: e.memset(tg[i][:], 0.0), writes=[f"tg{i}"])

            def load_gu(e):
                sl = e % 2
                dma("pool", Wg[sl][:], wg_d[e].rearrange("(k p) n -> p k n", p=128), writes=[f"Wg{sl}"])
                dma("pool", Wu[sl][:], wu_d[e].rearrange("(k p) n -> p k n", p=128), writes=[f"Wu{sl}"])

            def load_d(e):
                dma("pool", Wds[e % 2][:], wd_d[e].rearrange("(k p) n -> p k n", p=128), writes=[f"Wd{e % 2}"])

            def load_bd(e):
                dma("pool", BD[:], bd_d[e:e + 1, :].to_broadcast([128, 1024]), writes=["BD"])

            xi = [0]

            def build_xt(e, c):
                for st in range(4):
                    x_ = xs[xi[0] % 2]; kx = f"xs{xi[0] % 2}"
                    xi[0] += 1
                    r0 = e * CAP + c * 512 + st * 128
                    dma("sp", x_[:], XD[r0:r0 + 128, :], writes=[kx])
                    for kc in range(8):
                        op("pe", lambda en: en.transpose(out=pT[:, kc, :], in_=x_[:, kc * 128:(kc + 1) * 128],
                                                        identity=identb[:]),
                           reads=[kx, "identb"], writes=["pTF"])
                    act(XT[c][:, :, st * 128:(st + 1) * 128], pT[:], AF.Copy, ["pTF"], ["XT0"])

            def GU(e, c):
                sl = e % 2
                h_ = XT[c]; kh = "XT0"

                def tail(fc):
                    i2 = 0
                    ts("dve", ub[i2][:], u0b[i2][:], -7.0, 7.0, ALU.max, ALU.min, [f"u0_{i2}"], [f"u_{i2}"])
                    stt("dve", aT[:, fc, :], ub[i2][:], 1.0, sb_[i2][:], ALU.add, ALU.mult,
                        [f"u_{i2}", f"s_{i2}"], ["aT"])

                for fc in range(8):
                    i2 = fc % 2
                    pg_ = pgt[i2]; kg = f"pgt{i2}"; pu_ = put[i2]; ku = f"put{i2}"
                    for kc in range(8):
                        mm(pg_[:], Wg[sl][:, kc, fc * 128:(fc + 1) * 128], h_[:, kc, :], kc == 0, kc == 7,
                           [f"Wg{sl}", kh], [kg])
                    for kc in range(8):
                        mm(pu_[:], Wu[sl][:, kc, fc * 128:(fc + 1) * 128], h_[:, kc, :], kc == 0, kc == 7,
                           [f"Wu{sl}", kh], [ku])
                    ts("dve", gb[0][:], pg_[:], BG[:, e, fc:fc + 1], 7.0, ALU.add, ALU.min, [kg, "BG"], ["g_0"])
                    act(u0b[0][:], pu_[:], AF.Identity, [ku, "BU"], ["u0_0"], bias=BU[:, e, fc:fc + 1])
                    act(sb_[0][:], gb[0][:], AF.Silu, ["g_0"], ["s_0"], scale=1.702)
                    tail(fc)
                    combine_step()

            yi = [0]

            def DN(e, c):
                for j in range(4):
                    r0 = e * CAP + c * 512 + j * 128
                    for half in range(2):
                        pd_ = pdn[half]; kd = f"pdn{half}"
                        for fc in range(8):
                            mm(pd_[:], aT[:, fc, j * 128:(j + 1) * 128], Wds[e % 2][:, fc, half * 512:(half + 1) * 512],
                               fc == 0, fc == 7, ["aT", f"Wd{e % 2}"], [kd])
                        y_ = yo[yi[0] % 2]; ky = f"yo{yi[0] % 2}"
                        yi[0] += 1
                        stt("dve", y_[:], pd_[:], 1.0 / 1.702, BD[:, half * 512:(half + 1) * 512], ALU.mult, ALU.add,
                            [kd, "BD"], [ky])
                        dma("sp", YD[r0:r0 + 128, half * 512:(half + 1) * 512], y_[:], reads=[ky],
                            writes=[f"YD{e}_{c * 8 + j * 2 + half}"])

            gi = [0]
            pend_g = []
            pend_a = []

            def combine(e):
                for i in range(16):
                    pend_g.append((e, i))

            def combine_step():
                if pend_a:
                    e, i, k = pend_a.pop(0)
                    stt("dve", acc[:, i, :], tg[k][:], RWT[:, i, e:e + 1], acc[:, i, :], ALU.mult, ALU.add,
                        [f"tg{k}", "acc"], ["acc"])
                if pend_g:
                    e, i = pend_g.pop(0)
                    k = gi[0] % 2
                    gi[0] += 1
                    t_ = tg[k]
                    T.dma_fn("pool", lambda g, t_=t_, i=i, e=e: g.indirect_dma_start(
                        out=t_[:, :], out_offset=None, in_=YD[:, :],
                        in_offset=bass.IndirectOffsetOnAxis(ap=IDX[:, i * 32 + e:i * 32 + e + 1], axis=0),
                        bounds_check=BCREG, oob_is_err=False),
                        reads=[f"YD{e}_{m}" for m in range(16)], writes=[f"tg{k}"])
                    pend_a.append((e, i, k))

            load_gu(0)
            load_d(0)
            load_bd(0)
            load_gu(1)
            load_d(1)
            build_xt(0, 0)
            for e in range(32):
                GU(e, 0)
                build_xt(e, 1)
                DN(e, 0)
                GU(e, 1)
                if e + 2 < 32:
                    load_gu(e + 2)
                if e + 1 < 32:
                    build_xt(e + 1, 0)
                DN(e, 1)
                if e + 2 < 32:
                    load_d(e + 2)
                if e + 1 < 32:
                    load_bd(e + 1)
                combine(e)
            while pend_g or pend_a:
                combine_step()
            T.barrier()
            xv = Wg[0][:].rearrange("p k n -> p (k n)").bitcast(F32)
            sqv = Wu[0][:].rearrange("p k n -> p (k n)")
            for t in range(16):
                x1v = xv[:, (t % 2) * 2048:(t % 2) * 2048 + 1024]; ov = xv[:, (t % 2) * 2048 + 1024:(t % 2) * 2048 + 2048]
                kx = f"x1v{t % 2}"; ko = f"ov{t % 2}"
                dma("sp", x1v, X1[t * 128:(t + 1) * 128, :], writes=[kx])
                act(sqv[:, 0:1024], acc[:, t, :], AF.Square, ["acc"], ["sqv", "ssF"], accum_out=ssF[:])
                ts("dve", ss2F[:], ssF[:], 1.0 / 1024, 1e-6, ALU.mult, ALU.add, ["ssF"], ["ss2F"])
                act(ss2F[:], ss2F[:], AF.Sqrt, ["ss2F"], ["ss2F"])
                op("dve", lambda e: e.reciprocal(out=ss2F[:], in_=ss2F[:]), reads=["ss2F"], writes=["ss2F"])
                stt("dve", ov, acc[:, t, :], ss2F[:, 0:1], G2[:], ALU.mult, ALU.mult, ["acc", "ss2F", "G2"], [ko])
                tt("dve", ov, ov, x1v, ALU.add, [ko, kx], [ko])
                dma("sp", out_d[t * 128:(t + 1) * 128, :], ov, reads=[ko])
            T.finish("sp")
    return nc


_NC = None


def _bf16(a):
    return a.astype(ml_dtypes.bfloat16)


def kernel(**inp):
    global _NC
    f32 = np.float32
    x = np.asarray(inp["x"], f32)
    c = np.asarray(inp["c"], f32)
    positions = np.asarray(inp["positions"]).astype(np.int32)
    sq = lambda k: np.asarray(inp[k])[0]
    if _NC is None:
        _NC = build_program()
    nc = _NC
    gains = np.concatenate([sq("mix_pre_g"), sq("mix_post_g"), sq("ffn_pre_g"), sq("ffn_post_g")])[None, :].astype(f32)
    p = np.arange(128)
    inv_freq = (10000.0 ** (-np.arange(32, dtype=np.float32) / np.float32(32))).astype(f32)
    cst = np.zeros((128, 16), f32)
    sgn_r = np.where((p % 64) < 32, -1.0, 1.0).astype(f32)
    shalf = np.where(p < 64, 1.0, -1.0).astype(f32)
    cst[:, 1] = sgn_r
    cst[:, 2] = np.pi / 2
    cst[:, 3] = inv_freq[p % 32]
    cst[:, 4] = (p < 64)
    cst[:, 5] = -(p >= 64).astype(f32)
    cst[:, 6] = shalf * 0.999999
    cst[:, 7] = -1.0
    cst[:, 8] = -(p < 64).astype(f32)
    cst[:, 9] = 1.0
    cst[:, 10] = -TWO_PI_LO
    cst[:, 11] = -TWO_PI_LO * shalf
    cst[:, 13] = 12582912.0
    cst[:, 12] = shalf
    ident = np.eye(128, dtype=f32)
    iota = np.arange(1024, dtype=f32)[None, :]
    lre = np.concatenate([sq("ssm_lam_re").T, sq("ssm_lam_re").T], 0).astype(f32)
    lim = np.concatenate([sq("ssm_lam_im").T, sq("ssm_lam_im").T], 0).astype(f32)
    ldt = sq("ssm_log_dt")[None, :].astype(f32)
    bre = sq("ssm_b_re"); bim = sq("ssm_b_im")
    bl1 = np.zeros((128, 32, 128), f32); bl2 = np.zeros((128, 32, 128), f32)
    for g in range(32):
        r0 = ((g % 8) // 2) * 32 + (g % 2) * 16
        bl1[r0:r0 + 16, g, 0:64] = bre[g].T; bl1[r0:r0 + 16, g, 64:128] = bim[g].T
        bl2[r0:r0 + 16, g, 0:64] = bim[g].T; bl2[r0:r0 + 16, g, 64:128] = bre[g].T
    cre = sq("ssm_c_re"); cim = sq("ssm_c_im")
    crp = np.transpose(cre, (2, 0, 1)); cip = np.transpose(cim, (2, 0, 1))
    cr = np.concatenate([crp, crp], 0).reshape(128, 512).astype(f32)
    ci = np.concatenate([cip, cip], 0).reshape(128, 512).astype(f32)
    dsk = sq("ssm_d").reshape(4, 128).T.copy().astype(f32)
    bonehot = np.zeros((32, 8192), f32)
    for n in range(32):
        bonehot[n, n * 256:(n + 1) * 256] = 1.0
    bonehot = _bf16(bonehot)
    dmask = np.ones((128, 4, 512), f32)
    kk = np.arange(128)[:, None]; qq = np.arange(128)[None, :]
    for i in range(4):
        for j in range(4):
            if i // 2 == j // 2:
                if i == j:
                    dmask[:, i, j * 128:(j + 1) * 128] = (kk <= qq)
                elif i > j:
                    dmask[:, i, j * 128:(j + 1) * 128] = 0.0
    dmask = _bf16(dmask.reshape(128, 2048))
    bg = np.transpose(sq("b_gate").reshape(32, 8, 128), (2, 0, 1)).reshape(128, 256).astype(f32)
    bu = np.transpose(sq("b_up").reshape(32, 8, 128), (2, 0, 1)).reshape(128, 256).astype(f32)
    ltri = _bf16((np.arange(128)[:, None] < np.arange(128)[None, :]).astype(f32))
    eoff = (np.arange(32, dtype=f32) * 1024.0 - 40000.0)[None, :]
    shared = {
        "ltri": ltri, "eoff": eoff,
        "ada_w": sq("ada_w"), "ada_b": sq("ada_b")[None, :], "gains": gains, "w_in": sq("w_in"),
        "cst": cst, "ident": ident, "iota": iota, "lre": lre, "lim": lim, "ldt": ldt,
        "bl1": bl1.reshape(128, 4096), "bl2": bl2.reshape(128, 4096), "cr": cr, "ci": ci, "dsk": dsk,
        "w_glu": sq("ssm_w_glu"), "w_sb": sq("w_ssm_branch"), "w_ab": sq("w_attn_branch"), "w_out": sq("w_out"),
        "bonehot": bonehot, "dmask": dmask, "router_w": sq("router_w"), "router_b": sq("router_b")[None, :],
        "w_gate": sq("w_gate"), "w_up": sq("w_up"), "w_down": sq("w_down"),
        "b_gate": bg, "b_up": bu, "b_down": sq("b_down"),
    }
    shared = {k: np.ascontiguousarray(v) for k, v in shared.items()}
    in_maps = []
    for core in range(8):
        b, j = core // 4, core % 4
        nprev = j * 2048
        x_loc = np.zeros((8192, 1024), f32)
        x_loc[6144 - nprev:] = x[b, :nprev + 2048]
        tm = np.zeros(8192, f32); tm[6144 - nprev:] = 1.0
        pos = np.zeros(8192, np.int32); pos[6144 - nprev:] = positions[b, :nprev + 2048]
        ninv = (3 - j) * 8
        vb = np.zeros((8, 96), f32); vv = np.zeros((8, 96), f32); vo = np.zeros((8, 96), f32)
        for qb in range(8):
            n = np.arange(32)
            valid = (n >= ninv) & (n < 24 + qb)
            vb[qb, 64:96] = np.where(valid, 0.0, -1e30)
            vv[qb, 64:96] = valid
            vo[qb, 64 + 24 + qb] = 1.0
        m = dict(shared)
        m.update({
            "x_loc": x_loc, "tmask": np.ascontiguousarray(tm.reshape(64, 128).T),
            "pos_loc": pos[None, :], "c_b": np.ascontiguousarray(c[b].reshape(8, 128).T),
            "vbias": vb.reshape(1, 768), "vvalid": vv.reshape(1, 768), "vown": vo.reshape(1, 768),
        })
        in_maps.append(m)
    res = run_bass_kernel_spmd(nc, in_maps, core_ids=list(range(8)))
    out = np.zeros((2, 8192, 1024), f32)
    for core in range(8):
        b, j = core // 4, core % 4
        out[b, j * 2048:(j + 1) * 2048] = res.results[core]["out"]
    return out
```
